# Optimizing a Trainium2 kernel written in Bass

```python
import math
import jax, jax.numpy as jnp
from jax import lax
import numpy as np

D_MODEL = 1024
BATCH = 8
SEQ = 4096
DEPTH = 4

ROPE_BASE = 10000.0
NORM_EPS = 1e-6
GN_EPS = 1e-5
N_BRANCH = 3
FFN_RES = 0.5
D_FF = 2816
N_SUBNORMS = 6
MAX_POS_OFFSET = 1024
RET_HEADS = 4
RET_DK = 128
RET_DV = 128
RET_CHUNK = 128
RET_QK_W = RET_HEADS * RET_DK
RET_V_W = RET_HEADS * RET_DV
S5_WIDTH = 512
S5_GROUP = 16
S5_STATE = 64
S5_GROUPS = S5_WIDTH // S5_GROUP
S5_DT_MIN = 0.001
S5_DT_MAX = 0.1
MLA_HEADS = 4
MLA_Q_RANK = 256
MLA_KV_RANK = 128
MLA_NOPE = 128
MLA_ROPE = 64
MLA_DV = 128
MLA_V_W = MLA_HEADS * MLA_DV
ATTN_BLOCK = 128
IN_SIZES = (RET_QK_W, RET_QK_W, RET_V_W, RET_V_W, S5_WIDTH, MLA_Q_RANK, MLA_KV_RANK, MLA_ROPE, N_BRANCH * D_MODEL)
IN_WIDTH = RET_QK_W * 2 + RET_V_W * 2 + S5_WIDTH + MLA_Q_RANK + MLA_KV_RANK + MLA_ROPE + N_BRANCH * D_MODEL

kernel_name = "hybrid_retention_s5_mla_macaron"


def rms_norm(x, g):
    xf = x.astype(jnp.float32)
    y = xf * lax.rsqrt(jnp.mean(xf * xf, axis=-1, keepdims=True) + NORM_EPS)
    return (y * g.astype(jnp.float32)).astype(x.dtype)


def swiglu(h, w_gate, w_up, w_down):
    return (jax.nn.silu(h @ w_gate) * (h @ w_up)) @ w_down


def rope_cos_sin(positions, dim):
    inv_freq = ROPE_BASE ** (-jnp.arange(0, dim, 2, dtype=jnp.float32) / dim)
    ang = positions.astype(jnp.float32)[..., None] * inv_freq
    return jnp.cos(ang), jnp.sin(ang)


def apply_rope(x, cos, sin):
    half = x.shape[-1] // 2
    x1 = x[..., :half]
    x2 = x[..., half:]
    c = cos[:, :, None, :]
    s = sin[:, :, None, :]
    return jnp.concatenate([x1 * c - x2 * s, x2 * c + x1 * s], axis=-1).astype(x.dtype)


def split_columns(p):
    outs = []
    start = 0
    for size in IN_SIZES:
        outs.append(p[..., start:start + size])
        start += size
    return outs


def retention(q, k, v, g, cos, sin, w_o):
    B, L, _ = q.shape
    C = RET_CHUNK
    nc = L // C
    f32 = jnp.float32
    q = apply_rope(q.reshape(B, L, RET_HEADS, RET_DK), cos, sin).astype(f32)
    k = apply_rope(k.reshape(B, L, RET_HEADS, RET_DK), cos, sin).astype(f32) * (RET_DK ** -0.5)
    v = v.reshape(B, L, RET_HEADS, RET_DV).astype(f32)
    qc = q.reshape(B, nc, C, RET_HEADS, RET_DK)
    kc = k.reshape(B, nc, C, RET_HEADS, RET_DK)
    vc = v.reshape(B, nc, C, RET_HEADS, RET_DV)
    log_gamma = jnp.log1p(-jnp.exp2(-5.0 - jnp.arange(RET_HEADS, dtype=f32)))
    pos = jnp.arange(C, dtype=f32)
    rel = pos[:, None] - pos[None, :]
    intra = jnp.where(rel[None] >= 0.0,
                      jnp.exp(jnp.maximum(rel, 0.0)[None] * log_gamma[:, None, None]), 0.0)
    scores = jnp.einsum('bnchk,bnmhk->bnhcm', qc, kc) * intra
    inner = jnp.einsum('bnhcm,bnmhv->bnchv', scores, vc)
    k_decay = jnp.exp((C - 1.0 - pos)[:, None] * log_gamma[None, :])
    q_decay = jnp.exp((pos + 1.0)[:, None] * log_gamma[None, :])
    chunk_kv = jnp.einsum('bnmhk,bnmhv->nbhkv', kc * k_decay[:, :, None], vc)
    chunk_decay = jnp.exp(C * log_gamma)[:, None, None]

    def step(state, kv):
        return chunk_decay * state + kv, state

    _, s_prev = lax.scan(step, jnp.zeros((B, RET_HEADS, RET_DK, RET_DV), f32), chunk_kv)
    cross = jnp.einsum('bnchk,nbhkv->bnchv', qc * q_decay[:, :, None], s_prev)
    o = (inner + cross).reshape(B, L, RET_HEADS, RET_DV)
    mu = jnp.mean(o, axis=-1, keepdims=True)
    var = jnp.mean(jnp.square(o - mu), axis=-1, keepdims=True)
    o = ((o - mu) * lax.rsqrt(var + GN_EPS)).reshape(B, L, RET_V_W).astype(g.dtype)
    return (jax.nn.silu(g) * o) @ w_o


def _complex_scan_combine(e1, e2):
    a1r, a1i, b1r, b1i = e1
    a2r, a2i, b2r, b2i = e2
    return (a2r * a1r - a2i * a1i,
            a2r * a1i + a2i * a1r,
            a2r * b1r - a2i * b1i + b2r,
            a2r * b1i + a2i * b1r + b2i)


def s5_mixer(u, a_re, a_im, log_dt, b_re, b_im, c_re, c_im, d, w_glu_a, w_glu_b):
    B, L, _ = u.shape
    f32 = jnp.float32
    a_re = a_re.astype(f32)
    a_im = a_im.astype(f32)
    dt = jnp.exp(log_dt.astype(f32))[:, None]
    mag = jnp.exp(a_re * dt)
    abar_re = mag * jnp.cos(a_im * dt)
    abar_im = mag * jnp.sin(a_im * dt)
    den = a_re * a_re + a_im * a_im
    nr = abar_re - 1.0
    f_re = (nr * a_re + abar_im * a_im) / den
    f_im = (abar_im * a_re - nr * a_im) / den
    b_re = b_re.astype(f32)
    b_im = b_im.astype(f32)
    bbar_re = f_re[..., None] * b_re - f_im[..., None] * b_im
    bbar_im = f_re[..., None] * b_im + f_im[..., None] * b_re
    ug = u.astype(f32).reshape(B, L, S5_GROUPS, S5_GROUP)
    bu_re = jnp.einsum('blgh,gph->blgp', ug, bbar_re)
    bu_im = jnp.einsum('blgh,gph->blgp', ug, bbar_im)
    shape_a = (1, L, S5_GROUPS, S5_STATE)
    elems = (jnp.broadcast_to(abar_re, shape_a), jnp.broadcast_to(abar_im, shape_a), bu_re, bu_im)
    _, _, s_re, s_im = lax.associative_scan(_complex_scan_combine, elems, axis=1)
    y = (jnp.einsum('blgp,ghp->blgh', s_re, c_re.astype(f32))
         - jnp.einsum('blgp,ghp->blgh', s_im, c_im.astype(f32))).reshape(B, L, S5_WIDTH)
    y = jax.nn.gelu(y + d.astype(f32) * u.astype(f32)).astype(u.dtype)
    return (y @ w_glu_a) * jax.nn.sigmoid(y @ w_glu_b)


def mla(c_q, c_kv, k_rope, cos, sin, q_norm, kv_norm, w_uq, w_ukv, w_o):
    B, L, _ = c_q.shape
    q = (rms_norm(c_q, q_norm) @ w_uq).reshape(B, L, MLA_HEADS, MLA_NOPE + MLA_ROPE)
    q = jnp.concatenate([q[..., :MLA_NOPE], apply_rope(q[..., MLA_NOPE:], cos, sin)], axis=-1)
    kv = (rms_norm(c_kv, kv_norm) @ w_ukv).reshape(B, L, MLA_HEADS, MLA_NOPE + MLA_DV)
    k_pe = apply_rope(k_rope[:, :, None, :], cos, sin)
    k = jnp.concatenate([kv[..., :MLA_NOPE],
                         jnp.broadcast_to(k_pe, (B, L, MLA_HEADS, MLA_ROPE)).astype(kv.dtype)], axis=-1)
    v = kv[..., MLA_NOPE:]
    scale = (MLA_NOPE + MLA_ROPE) ** -0.5
    outs = []
    for i in range(L // ATTN_BLOCK):
        q0 = i * ATTN_BLOCK
        kend = q0 + ATTN_BLOCK
        qs = q[:, q0:kend]
        s = jnp.einsum('bqhd,bkhd->bhqk', qs, k[:, :kend]).astype(jnp.float32) * scale
        qi = q0 + jnp.arange(ATTN_BLOCK)
        ki = jnp.arange(kend)
        s = jnp.where(ki[None, :] <= qi[:, None], s, -jnp.inf)
        p = jax.nn.softmax(s, axis=-1).astype(v.dtype)
        outs.append(jnp.einsum('bhqk,bkhd->bqhd', p, v[:, :kend]))
    o = jnp.concatenate(outs, axis=1).reshape(B, L, MLA_V_W)
    return o @ w_o


def setup_inputs(seed: int = 0) -> dict:
    key = jax.random.key(seed)
    ks = jax.random.split(key, 24)
    f32 = jnp.float32

    def nrm(k, shape, fan_in):
        return jax.random.normal(k, shape, f32) * (fan_in ** -0.5)

    x = jax.random.normal(ks[0], (BATCH, SEQ, D_MODEL), f32)
    offsets = jax.random.randint(ks[1], (BATCH, 1), 0, MAX_POS_OFFSET, dtype=jnp.int32)
    positions = offsets + jnp.arange(SEQ, dtype=jnp.int32)[None, :]
    norm_gains = 1.0 + 0.05 * jax.random.normal(ks[2], (DEPTH, N_SUBNORMS, D_MODEL), f32)
    ffn_w_gate = nrm(ks[3], (DEPTH, 2, D_MODEL, D_FF), D_MODEL)
    ffn_w_up = nrm(ks[4], (DEPTH, 2, D_MODEL, D_FF), D_MODEL)
    ffn_w_down = nrm(ks[5], (DEPTH, 2, D_FF, D_MODEL), D_FF)
    w_in = nrm(ks[6], (DEPTH, D_MODEL, IN_WIDTH), D_MODEL)
    ret_w_o = nrm(ks[7], (DEPTH, RET_V_W, D_MODEL), RET_V_W)
    s5_a_re = -0.5 + 0.01 * jax.random.normal(ks[8], (DEPTH, S5_GROUPS, S5_STATE), f32)
    s5_a_im = (jnp.pi * jnp.arange(S5_STATE, dtype=f32))[None, None, :] + 0.01 * jax.random.normal(ks[9], (DEPTH, S5_GROUPS, S5_STATE), f32)
    s5_log_dt = jax.random.uniform(ks[10], (DEPTH, S5_GROUPS), f32, math.log(S5_DT_MIN), math.log(S5_DT_MAX))
    s5_b_re = nrm(ks[11], (DEPTH, S5_GROUPS, S5_STATE, S5_GROUP), 2 * S5_GROUP)
    s5_b_im = nrm(ks[12], (DEPTH, S5_GROUPS, S5_STATE, S5_GROUP), 2 * S5_GROUP)
    s5_c_re = nrm(ks[13], (DEPTH, S5_GROUPS, S5_GROUP, S5_STATE), 2 * S5_STATE)
    s5_c_im = nrm(ks[14], (DEPTH, S5_GROUPS, S5_GROUP, S5_STATE), 2 * S5_STATE)
    s5_d = jax.random.normal(ks[15], (DEPTH, S5_WIDTH), f32)
    s5_glu_a = nrm(ks[16], (DEPTH, S5_WIDTH, D_MODEL), S5_WIDTH)
    s5_glu_b = nrm(ks[17], (DEPTH, S5_WIDTH, D_MODEL), S5_WIDTH)
    mla_q_norm = 1.0 + 0.05 * jax.random.normal(ks[18], (DEPTH, MLA_Q_RANK), f32)
    mla_kv_norm = 1.0 + 0.05 * jax.random.normal(ks[19], (DEPTH, MLA_KV_RANK), f32)
    mla_w_uq = nrm(ks[20], (DEPTH, MLA_Q_RANK, MLA_HEADS * (MLA_NOPE + MLA_ROPE)), MLA_Q_RANK)
    mla_w_ukv = nrm(ks[21], (DEPTH, MLA_KV_RANK, MLA_HEADS * (MLA_NOPE + MLA_DV)), MLA_KV_RANK)
    mla_w_o = nrm(ks[22], (DEPTH, MLA_V_W, D_MODEL), MLA_V_W)
    w_out = nrm(ks[23], (DEPTH, D_MODEL, D_MODEL), D_MODEL)
    return {"x": x, "positions": positions, "norm_gains": norm_gains,
            "ffn_w_gate": ffn_w_gate, "ffn_w_up": ffn_w_up, "ffn_w_down": ffn_w_down,
            "w_in": w_in, "ret_w_o": ret_w_o,
            "s5_a_re": s5_a_re, "s5_a_im": s5_a_im, "s5_log_dt": s5_log_dt,
            "s5_b_re": s5_b_re, "s5_b_im": s5_b_im, "s5_c_re": s5_c_re, "s5_c_im": s5_c_im,
            "s5_d": s5_d, "s5_glu_a": s5_glu_a, "s5_glu_b": s5_glu_b,
            "mla_q_norm": mla_q_norm, "mla_kv_norm": mla_kv_norm,
            "mla_w_uq": mla_w_uq, "mla_w_ukv": mla_w_ukv, "mla_w_o": mla_w_o,
            "w_out": w_out}


def reference(x, positions, norm_gains, ffn_w_gate, ffn_w_up, ffn_w_down, w_in, ret_w_o,
              s5_a_re, s5_a_im, s5_log_dt, s5_b_re, s5_b_im, s5_c_re, s5_c_im, s5_d,
              s5_glu_a, s5_glu_b, mla_q_norm, mla_kv_norm, mla_w_uq, mla_w_ukv, mla_w_o, w_out):
    B, L, _ = x.shape
    cos_r, sin_r = rope_cos_sin(positions, RET_DK)
    cos_m, sin_m = rope_cos_sin(positions, MLA_ROPE)
    for l in range(DEPTH):
        n = norm_gains[l]
        h = rms_norm(x, n[0])
        x = x + FFN_RES * rms_norm(swiglu(h, ffn_w_gate[l, 0], ffn_w_up[l, 0], ffn_w_down[l, 0]), n[1])
        h = rms_norm(x, n[2])
        q_r, k_r, v_r, g_r, u_s, c_q, c_kv, k_pe, gates = split_columns(h @ w_in[l])
        y_ret = retention(q_r, k_r, v_r, g_r, cos_r, sin_r, ret_w_o[l])
        y_s5 = s5_mixer(u_s, s5_a_re[l], s5_a_im[l], s5_log_dt[l], s5_b_re[l], s5_b_im[l],
                        s5_c_re[l], s5_c_im[l], s5_d[l], s5_glu_a[l], s5_glu_b[l])
        y_mla = mla(c_q, c_kv, k_pe, cos_m, sin_m, mla_q_norm[l], mla_kv_norm[l],
                    mla_w_uq[l], mla_w_ukv[l], mla_w_o[l])
        gt = jax.nn.sigmoid(gates.reshape(B, L, N_BRANCH, D_MODEL))
        merged = gt[:, :, 0] * y_ret + gt[:, :, 1] * y_s5 + gt[:, :, 2] * y_mla
        x = x + rms_norm(merged @ w_out[l], n[3])
        h = rms_norm(x, n[4])
        x = x + FFN_RES * rms_norm(swiglu(h, ffn_w_gate[l, 1], ffn_w_up[l, 1], ffn_w_down[l, 1]), n[5])
    return x
```

```python
import contextlib
import math
import numpy as np
import concourse.bass as bass
import concourse.mybir as mybir
from concourse.bass_utils import run_bass_kernel_spmd

F32 = mybir.dt.float32
BF16 = mybir.dt.bfloat16
I32 = mybir.dt.int32
AF = mybir.ActivationFunctionType
ALU = mybir.AluOpType

D = 1024
L = 4096
NB = 8
TB = 512
DFF = 2816
NFF = 22
INW = 6080
DEPTH = 4
NORM_EPS = 1e-6
GN_EPS = 1e-5
ST = 4
N2 = TB // ST

SAME_ENG_SYNC = True
DMA_SLOTS = {'sp': 24, 'act': 6, 'pool': 20}


class Buf:
    __slots__ = ('name', 'w', 'r')

    def __init__(self, name=''):
        self.name = name
        self.w = []
        self.r = []


class Op:
    __slots__ = ('eng', 'fn', 'deps', 'dma', 'slot', 'gen', 'sig', 'sigval', 'waits', 'idx')


class Prog:
    def __init__(self, nc):
        self.nc = nc
        self.ops = []
        self.emitted = 0
        self.stack = contextlib.ExitStack()
        self.esem = {}
        for e in ('pe', 'act', 'dve', 'pool', 'sp'):
            self.esem[e] = self.stack.enter_context(nc.semaphore('es_' + e))
        self.dsem = {}
        for q, k in DMA_SLOTS.items():
            self.dsem[q] = [self.stack.enter_context(nc.semaphore('ds_%s%d' % (q, i))) for i in range(k)]
        self.dcount = {q: 0 for q in DMA_SLOTS}
        self.dhist = {q: [] for q in DMA_SLOTS}
        self.clocks = {e: {} for e in self.esem}
        self.sigcnt = {e: 0 for e in self.esem}
        self.lastop = {}
        self.sb_off = 0
        self.SB_BYTES = 207 * 1024
        self.big = nc.alloc_sbuf_tensor('big', [128, self.SB_BYTES], mybir.dt.uint8)

    def sb_reset(self, off=0):
        self.sb_off = off

    def sb(self, shape, dtype, name=None, top=False):
        nbytes = int(np.prod(shape[1:])) * mybir.dt.size(dtype)
        if top:
            self.top_off = (getattr(self, 'top_off', 0) + nbytes + 63) // 64 * 64
            off = self.SB_BYTES - self.top_off
            assert off >= self.sb_off, ('SBUF overflow(top)', name)
        else:
            off = (self.sb_off + 63) // 64 * 64
            self.sb_off = off + nbytes
            assert self.sb_off <= self.SB_BYTES - getattr(self, 'top_off', 0), ('SBUF overflow', name, self.sb_off)
        v = self.big[:, off:off + nbytes].bitcast(dtype)
        if len(shape) == 3:
            v = v.rearrange("p (a b) -> p a b", a=shape[1])
        elif len(shape) == 4:
            v = v.rearrange("p (a b c) -> p a b c", a=shape[1], b=shape[2])
        if shape[0] < 128:
            v = v[0:shape[0]]
        return v

    def add(self, eng, fn, reads=(), writes=(), dma=False):
        op = Op()
        op.idx = len(self.ops)
        op.eng = eng
        op.fn = fn
        op.dma = dma
        op.sig = False
        op.sigval = None
        deps = set()
        for b in reads:
            deps.update(b.w)
            b.r.append(op.idx)
        for b in writes:
            deps.update(b.w)
            deps.update(b.r)
            b.w = [op.idx]
            b.r = []
        if dma:
            k = DMA_SLOTS[eng]
            n = self.dcount[eng]
            self.dcount[eng] = n + 1
            op.slot = n % k
            op.gen = n // k
            if n >= k:
                deps.add(self.dhist[eng][n - k])
            self.dhist[eng].append(op.idx)
        elif fn is not None:
            self.lastop[eng] = op.idx
        deps.discard(op.idx)
        op.deps = deps
        self.ops.append(op)
        return op.idx

    def barrier(self):
        deps = set(self.lastop.values())
        for q, k in DMA_SLOTS.items():
            deps.update(self.dhist[q][-k:])
        for e in self.esem:
            i = self.add(e, None)
            self.ops[i].deps.update(deps)

    def flush(self):
        nc = self.nc
        ops = self.ops
        new = ops[self.emitted:]
        for op in new:
            waits = []
            clk = self.clocks[op.eng]
            for d in sorted(op.deps):
                dop = ops[d]
                if dop.dma:
                    key = ('D', dop.eng, dop.slot)
                    val = dop.gen + 1
                else:
                    if dop.eng == op.eng and (op.eng in ('pe', 'sp') or not SAME_ENG_SYNC):
                        continue
                    key = ('E', dop.eng)
                    val = d
                if clk.get(key, -1) >= val:
                    continue
                clk[key] = val
                waits.append(d)
                if not dop.dma and d >= self.emitted:
                    dop.sig = True
            op.waits = waits
        last = {}
        for op in new:
            if not op.dma and op.fn is not None:
                last[op.eng] = op
        for op in last.values():
            op.sig = True
        for op in new:
            if not op.dma and op.sig:
                self.sigcnt[op.eng] += 1
                op.sigval = self.sigcnt[op.eng]
        per = {e: [] for e in self.esem}
        for op in new:
            per[op.eng].append(op)
        self.emitted = len(ops)

        def event(d):
            dop = ops[d]
            if dop.dma:
                return self.dsem[dop.eng][dop.slot], 16 * (dop.gen + 1)
            if dop.sigval is None:
                j = d
                while ops[j].eng != dop.eng or ops[j].dma or ops[j].sigval is None:
                    j += 1
                return self.esem[dop.eng], ops[j].sigval
            return self.esem[dop.eng], dop.sigval

        def runner(engname):
            def f(e):
                for op in per[engname]:
                    for d in op.waits:
                        s, v = event(d)
                        e.wait_ge(s, v)
                    if op.fn is None:
                        continue
                    ins = op.fn(e)
                    if op.dma:
                        ins.then_inc(self.dsem[op.eng][op.slot], 16)
                    elif op.sig:
                        ins.then_inc(self.esem[op.eng], 1)
            return f

        with nc.Block() as block:
            block.tensor(runner('pe'))
            block.scalar(runner('act'))
            block.vector(runner('dve'))
            block.gpsimd(runner('pool'))
            block.sync(runner('sp'))

    def close(self):
        self.stack.close()


class Pool_:
    def __init__(self, P, n, shape, dtype, name):
        self.tiles = [P.sb(shape, dtype, name) for _ in range(n)]
        self.bufs = [Buf('%s%d' % (name, i)) for i in range(n)]
        self.i = 0

    def get(self):
        k = self.i % len(self.tiles)
        self.i += 1
        return self.tiles[k], self.bufs[k]


class PsumPool:
    def __init__(self, tiles, names):
        self.tiles = tiles
        self.bufs = [Buf(n) for n in names]
        self.i = 0

    def get(self):
        k = self.i % len(self.tiles)
        self.i += 1
        return self.tiles[k], self.bufs[k]


class Builder:
    def __init__(self, n_layers=DEPTH, phases=None, dump=None):
        self.n_layers = n_layers
        self.phases = phases
        self.dump = dump
        nc = self.nc = bass.Bass("TRN2", target_bir_lowering=False)
        self.P = Prog(nc)
        self.inputs = {}
        self.scratch = {}
        self.dbufs = {}

    def dram_in(self, name, shape, dtype=F32):
        t = self.nc.dram_tensor(name, list(shape), dtype, kind="ExternalInput").ap()
        self.inputs[name] = t
        return t

    def dram_scratch(self, name, shape, dtype):
        t = self.nc.dram_tensor(name, list(shape), dtype).ap()
        self.scratch[name] = t
        return t

    def xb(self, name, b):
        return [self.dbuf(name, b, c) for c in range(8)]

    def dbuf(self, name, *idx):
        key = (name,) + idx
        b = self.dbufs.get(key)
        if b is None:
            b = self.dbufs[key] = Buf(str(key))
        return b

    def mm(self, out_ap, pairs, reads, writes):
        def fn(e):
            n = len(pairs)
            ins = None
            for i, (l, r) in enumerate(pairs):
                ins = e.matmul(out_ap, lhsT=l, rhs=r, start=(i == 0), stop=(i == n - 1))
            return ins
        self.P.add('pe', fn, reads=reads, writes=writes)

    def dma(self, q, out_ap, in_ap, reads, writes):
        self.P.add(q, lambda e: e.dma_start(out=out_ap, in_=in_ap), reads=reads, writes=writes, dma=True)

    def act(self, out_ap, in_ap, func, reads, writes, scale=1.0, bias=None):
        if bias is None:
            self.P.add('act', lambda e: e.activation(out=out_ap, in_=in_ap, func=func, scale=scale), reads=reads, writes=writes)
        else:
            self.P.add('act', lambda e: e.activation(out=out_ap, in_=in_ap, func=func, scale=scale, bias=bias), reads=reads, writes=writes)

    def tt(self, eng, out_ap, a, b, op, reads, writes):
        self.P.add(eng, lambda e: e.tensor_tensor(out=out_ap, in0=a, in1=b, op=op), reads=reads, writes=writes)

    def ts(self, eng, out_ap, a, s1, s2, op0, op1, reads, writes):
        if op1 is None:
            self.P.add(eng, lambda e: e.tensor_scalar(out=out_ap, in0=a, scalar1=s1, scalar2=None, op0=op0), reads=reads, writes=writes)
        else:
            self.P.add(eng, lambda e: e.tensor_scalar(out=out_ap, in0=a, scalar1=s1, scalar2=s2, op0=op0, op1=op1), reads=reads, writes=writes)

    def stt(self, out_ap, a, s, b, op0, op1, reads, writes):
        self.P.add('dve', lambda e: e.scalar_tensor_tensor(out=out_ap, in0=a, scalar=s, in1=b, op0=op0, op1=op1), reads=reads, writes=writes)

    def copy(self, eng, out_ap, in_ap, reads, writes):
        if eng == 'act':
            self.P.add('act', lambda e: e.activation(out=out_ap, in_=in_ap, func=AF.Copy), reads=reads, writes=writes)
        else:
            self.P.add(eng, lambda e: e.tensor_copy(out=out_ap, in_=in_ap), reads=reads, writes=writes)

    def memset(self, eng, ap, val, writes):
        self.P.add(eng, lambda e: e.memset(ap, val), writes=writes)

    IN_SHAPES = {
        'xT': ([D, L], F32), 'pos': ([1, L], I32), 'gains': ([128, DEPTH * 48], F32),
        'ffn_w_gate': ([DEPTH, 2, D, DFF], F32), 'ffn_w_up': ([DEPTH, 2, D, DFF], F32), 'ffn_w_down': ([DEPTH, 2, DFF, D], F32),
        'w_in': ([DEPTH, D, INW], F32), 'ret_w_o': ([DEPTH, 512, D], F32),
        's5_are': ([DEPTH, 128, 16], F32), 's5_aim': ([DEPTH, 128, 16], F32), 's5_ldt': ([DEPTH, 128, 16], F32),
        's5_bre': ([DEPTH, 128, 16, 16], F32), 's5_bim': ([DEPTH, 128, 16, 16], F32),
        's5_cre': ([DEPTH, 128, 16, 16], F32), 's5_cim': ([DEPTH, 128, 16, 16], F32), 's5_d': ([DEPTH, 128, 4], F32),
        's5_glu_a': ([DEPTH, 512, D], F32), 's5_glu_b': ([DEPTH, 512, D], F32),
        'mla_q_norm': ([DEPTH, 128, 2], F32), 'mla_kv_norm': ([DEPTH, 128, 1], F32),
        'mla_w_uq': ([DEPTH, 256, 768], F32), 'mla_w_ukv': ([DEPTH, 128, 1024], F32), 'mla_w_o': ([DEPTH, 512, D], F32),
        'w_out': ([DEPTH, D, D], F32),
        'c_ident': ([128, 128], F32), 'c_tri': ([128, 128], F32), 'c_dec': ([128, 4, 128], F32), 'c_qdec': ([128, 4, 128], F32),
        'c_kdec': ([128, 4], F32), 'c_freq': ([128, 2], F32), 'c_sgn': ([128, 2], F32), 'c_step': ([128, N2], F32),
    }

    def I(self, name):
        t = self.inputs.get(name)
        if t is None:
            shape, dt = self.IN_SHAPES[name]
            shape = list(shape)
            if shape[0] == DEPTH and len(shape) >= 3:
                shape[0] = self.n_layers
            t = self.dram_in(name, shape, dt)
        return t

    def declare(self):
        self.out = self.nc.dram_tensor('outT', [D, L], F32, kind="ExternalOutput").ap()
        ds = self.dram_scratch
        self.X = ds('X', [D, L], F32)
        self.CR = ds('CR', [128, L], F32)
        self.SR = ds('SR', [128, L], F32)
        self.CM = ds('CM', [64, L], F32)
        self.SM = ds('SM', [64, L], F32)
        self.QR = ds('QR', [512, L], BF16)
        self.KR = ds('KR', [512, L], BF16)
        self.VR = ds('VR', [L, 512], BF16)
        self.SG = ds('SG', [512, L], BF16)
        self.U = ds('U', [512, L], BF16)
        self.QN = ds('QN', [512, L], BF16)
        self.QP = ds('QP', [256, L], BF16)
        self.KN = ds('KN', [512, L], BF16)
        self.KP = ds('KP', [64, L], BF16)
        self.VM = ds('VM', [L, 512], BF16)
        self.GT = ds('GT', [3072, L], BF16)
        self.MR = ds('MR', [D, L], BF16)
        self.MS = ds('MS', [D, L], BF16)
        if self.dump:
            self.dbg = self.nc.dram_tensor('dbg', list(self.dump[1]), self.dump[2], kind="ExternalOutput").ap()

    def consts(self):
        P = self.P
        P.sb_reset(0)
        self.ones_bf = P.sb([128, 128], BF16, 'ones')
        self.b_ones = Buf('ones')
        self.G = P.sb([128, DEPTH * 48], F32, 'G')
        self.GH = P.sb([128, DEPTH * 48], F32, 'GH')
        self.b_G = Buf('G')
        self.ident = P.sb([128, 128], F32, 'ident')
        self.b_ident = Buf('ident')
        self.memset('dve', self.ones_bf, 1.0, [self.b_ones])
        self.dma('sp', self.G, self.I('gains'), [], [self.b_G])
        self.dma('sp', self.ident, self.I('c_ident'), [], [self.b_ident])
        self.ts('dve', self.GH, self.G, 0.5, None, ALU.mult, None, [self.b_G], [self.b_G])
        self.const_end = P.sb_off
        self.psum = [self.nc.alloc_psum_tensor('ps%d' % i, [128, 512], F32)[:, :] for i in range(8)]

    def gcol(self, l, n, c):
        k = (l * 6 + n) * 8 + c
        return self.G[:, k:k + 1]

    def ghcol(self, l, n, c):
        k = (l * 6 + n) * 8 + c
        return self.GH[:, k:k + 1]

    def load_weight_rows(self, dst, src, nrows_chunks, ncols, bufs):
        maxc = 2048
        nsplit = (ncols + maxc - 1) // maxc
        w = (ncols + nsplit - 1) // nsplit
        for c in range(nrows_chunks):
            for s in range(nsplit):
                c0 = s * w
                c1 = min(ncols, c0 + w)
                self.dma('pool', dst[:, c, c0:c1], src[c * 128:(c + 1) * 128, c0:c1], [], [bufs[c][s]])

    def wbufs(self, n, ncols):
        nsplit = (ncols + 2047) // 2048
        return [[Buf() for _ in range(nsplit)] for _ in range(n)]

    @staticmethod
    def flat(bl):
        return [b for row in bl for b in row]

    def rms_stats(self, chunks, bxs, width, ps_stat, b_ps, sqpool, inv_n, eps_ap, rstd, b_rstd, tmp, b_tmp, b_eps):
        n = len(chunks)
        for c in range(n):
            sq, bsq = sqpool.get()
            self.act(sq[:, 0:width], chunks[c], AF.Square, [bxs[c]], [bsq])
            self.P.add('pe', (lambda sq, c: lambda e: e.matmul(ps_stat[:, 0:width], lhsT=self.ones_bf, rhs=sq[:, 0:width], start=(c == 0), stop=(c == n - 1)))(sq, c),
                       reads=[bsq, self.b_ones], writes=[b_ps])
        self.act(tmp[:, 0:width], ps_stat[:, 0:width], AF.Sqrt, [b_ps, b_eps], [b_tmp], scale=inv_n, bias=eps_ap)
        self.P.add('dve', lambda e: e.reciprocal(out=rstd[:, 0:width], in_=tmp[:, 0:width]), reads=[b_tmp], writes=[b_rstd])

    def ffn_phase(self, l, j, src, srcname, dst, dstname):
        P = self.P
        P.barrier()
        P.sb_reset(self.const_end)
        FT = 256
        NBF = L // FT
        n_pre = 0 if j == 0 else 4
        n_post = 1 if j == 0 else 5
        Wg = P.sb([128, 8, DFF], BF16, 'Wg')
        Wu = P.sb([128, 8, DFF], BF16, 'Wu')
        Wd = P.sb([128, NFF, D], BF16, 'Wd')
        bWg = self.wbufs(8, DFF)
        bWu = self.wbufs(8, DFF)
        bWd = self.wbufs(NFF, D)
        xts = [P.sb([128, 8, FT], F32, 'x') for _ in range(2)]
        bxs = [Buf('x0'), Buf('x1')]
        hs = [P.sb([128, 8, FT], BF16, 'h') for _ in range(2)]
        bhs = [[Buf('h%d_%d' % (i, c)) for c in range(8)] for i in range(2)]
        actb = P.sb([128, NFF, FT], BF16, 'act')
        bact = [Buf('act%d' % f) for f in range(NFF)]
        y = P.sb([128, 8, FT], F32, 'y')
        by = [Buf('y%d' % c) for c in range(8)]
        sqpool = Pool_(P, 4, [128, FT], BF16, 'sq')
        silp = Pool_(P, 4, [128, FT], BF16, 'sil')
        rstd = P.sb([128, FT], F32, 'rstd')
        b_rstd = Buf('rstd')
        rstd2 = P.sb([128, FT], F32, 'rstd2')
        b_rstd2 = Buf('rstd2')
        tmp = P.sb([128, FT], F32, 'tmp')
        b_tmp = Buf('tmp')
        eps = P.sb([128, 1], F32, 'eps')
        b_eps = Buf('eps')
        self.memset('dve', eps, NORM_EPS, [b_eps])
        ps = self.psum
        pg = PsumPool(ps[0:2], ['pg0', 'pg1'])
        pu = PsumPool(ps[2:4], ['pu0', 'pu1'])
        pd = PsumPool(ps[4:6], ['pd0', 'pd1'])
        ps_st, b_st = ps[6], Buf('pst')
        ps_yst, b_yst = ps[7], Buf('pyst')

        self.load_weight_rows(Wg, self.I('ffn_w_gate')[l, j], 8, DFF, bWg)
        self.load_weight_rows(Wu, self.I('ffn_w_up')[l, j], 8, DFF, bWu)
        self.load_weight_rows(Wd, self.I('ffn_w_down')[l, j], NFF, D, bWd)
        rWg, rWu, rWd = self.flat(bWg), self.flat(bWu), self.flat(bWd)

        srcv = src.rearrange("(c p) t -> p c t", p=128)
        dstv = dst.rearrange("(c p) t -> p c t", p=128)

        def stage_a(b):
            xt, bx, h, bh = xts[b % 2], bxs[b % 2], hs[b % 2], bhs[b % 2]
            self.dma('sp', xt, srcv[:, :, b * FT:(b + 1) * FT], self.xb(srcname, b * FT // TB), [bx])
            self.rms_stats([xt[:, c, :] for c in range(8)], [bx] * 8, FT, ps_st, b_st, sqpool, 1.0 / D, eps[:, 0:1], rstd, b_rstd, tmp, b_tmp, b_eps)
            for c in range(8):
                self.stt(h[:, c, :], xt[:, c, :], self.gcol(l, n_pre, c), rstd, ALU.mult, ALU.mult, [bx, b_rstd, self.b_G], [bh[c]])

        def stage_b(b):
            h, bh = hs[b % 2], bhs[b % 2]
            for f in range(NFF):
                g_ps, bg = pg.get()
                u_ps, bu = pu.get()
                self.mm(g_ps[:, 0:FT], [(Wg[:, k, f * 128:(f + 1) * 128], h[:, k, :]) for k in range(8)], bh + rWg, [bg])
                self.mm(u_ps[:, 0:FT], [(Wu[:, k, f * 128:(f + 1) * 128], h[:, k, :]) for k in range(8)], bh + rWu, [bu])
                sl, bsl = silp.get()
                self.act(sl, g_ps[:, 0:FT], AF.Silu, [bg], [bsl])
                self.tt('dve', actb[:, f, :], sl, u_ps[:, 0:FT], ALU.mult, [bsl, bu], [bact[f]])

        def stage_c(b):
            pend = []
            for c in range(8):
                d_ps, bd = pd.get()
                self.mm(d_ps[:, 0:FT], [(Wd[:, f, c * 128:(c + 1) * 128], actb[:, f, :]) for f in range(NFF)], bact + rWd, [bd])
                self.copy('dve', y[:, c, :], d_ps[:, 0:FT], [bd], [by[c]])
                sq, bsq = sqpool.get()
                self.act(sq, y[:, c, :], AF.Square, [by[c]], [bsq])
                pend.append((sq, bsq, c))
                if len(pend) > 2:
                    sq_, bsq_, c_ = pend.pop(0)
                    self.P.add('pe', (lambda sq, c: lambda e: e.matmul(ps_yst[:, 0:FT], lhsT=self.ones_bf, rhs=sq, start=(c == 0), stop=(c == 7)))(sq_, c_),
                               reads=[bsq_, self.b_ones], writes=[b_yst])
            for sq_, bsq_, c_ in pend:
                self.P.add('pe', (lambda sq, c: lambda e: e.matmul(ps_yst[:, 0:FT], lhsT=self.ones_bf, rhs=sq, start=(c == 0), stop=(c == 7)))(sq_, c_),
                           reads=[bsq_, self.b_ones], writes=[b_yst])

        def stage_d(b):
            xt, bx = xts[b % 2], bxs[b % 2]
            self.act(tmp, ps_yst[:, 0:FT], AF.Sqrt, [b_yst, b_eps], [b_tmp], scale=1.0 / D, bias=eps[:, 0:1])
            self.P.add('dve', lambda e: e.reciprocal(out=rstd2, in_=tmp), reads=[b_tmp], writes=[b_rstd2])
            for c in range(8):
                self.tt('dve', y[:, c, :], y[:, c, :], rstd2, ALU.mult, [by[c], b_rstd2], [by[c]])
                self.stt(xt[:, c, :], y[:, c, :], self.ghcol(l, n_post, c), xt[:, c, :], ALU.mult, ALU.add, [by[c], bx, self.b_G], [bx])
            self.dma('sp', dstv[:, :, b * FT:(b + 1) * FT], xt, [bx], self.xb(dstname, b * FT // TB))

        import os
        dbg_st = os.environ.get('FFN_STAGES', 'abcd')
        dbg_nb = int(os.environ.get('FFN_NB', NBF))
        stage_a(0)
        for b in range(dbg_nb):
            if 'b' in dbg_st:
                stage_b(b)
            if b + 1 < dbg_nb:
                stage_a(b + 1)
            if 'c' in dbg_st:
                stage_c(b)
            if 'd' in dbg_st:
                stage_d(b)

    def rope_tables(self):
        P = self.P
        P.barrier()
        P.sb_reset(self.const_end)
        posi = P.sb([128, L], I32, 'posi')
        posf = P.sb([128, L], F32, 'posf')
        ang = P.sb([128, L], F32, 'ang')
        kk = P.sb([128, L], F32, 'kk')
        res = P.sb([128, L], F32, 'res')
        fr = P.sb([128, 2], F32, 'fr')
        sg = P.sb([128, 2], F32, 'sg')
        b_pos, b_ang, b_kk, b_res, b_c = Buf(), Buf(), Buf(), Buf(), Buf()
        self.dma('sp', posi, self.I('pos').partition_broadcast(128), [], [b_pos])
        self.dma('sp', fr, self.I('c_freq'), [], [b_c])
        self.dma('sp', sg, self.I('c_sgn'), [], [b_c])
        self.copy('dve', posf, posi, [b_pos], [b_pos])
        MAG = 12582912.0
        C1 = 6.28125
        C2 = 2.0 * math.pi - 6.28125
        for col, rows, dc, dsn in ((0, 128, self.CR, self.SR), (1, 64, self.CM, self.SM)):
            for is_cos in (False, True):
                a = ang[0:rows]
                k = kk[0:rows]
                r = res[0:rows]
                self.ts('dve', a, posf[0:rows], fr[0:rows, col:col + 1], (math.pi / 2 if is_cos else 0.0), ALU.mult, ALU.add, [b_pos, b_c], [b_ang])
                self.ts('dve', k, a, 1.0 / (2.0 * math.pi), MAG, ALU.mult, ALU.add, [b_ang], [b_kk])
                self.ts('dve', k, k, -MAG, None, ALU.add, None, [b_kk], [b_kk])
                self.stt(a, k, -C1, a, ALU.mult, ALU.add, [b_kk, b_ang], [b_ang])
                self.stt(a, k, -C2, a, ALU.mult, ALU.add, [b_kk, b_ang], [b_ang])
                self.ts('dve', a, a, 3.141592, -3.141592, ALU.min, ALU.max, [b_ang], [b_ang])
                self.act(r, a, AF.Sin, [b_ang], [b_res])
                if not is_cos:
                    self.ts('dve', r, r, sg[0:rows, col:col + 1], None, ALU.mult, None, [b_res, b_c], [b_res])
                self.dma('sp', dc if is_cos else dsn, r, [b_res], [self.dbuf('ropetab')])

    def rope_evac(self, ps, rows, Ct, St, b_tab, bps, scale, out_ap, b_out, t1p, t2p):
        hf = rows // 2
        t1, bt1 = t1p.get()
        t2, bt2 = t2p.get()
        self.stt(t1[0:rows], ps[0:rows], scale, Ct[0:rows], ALU.mult, ALU.mult, [bps, b_tab], [bt1])
        self.stt(t2[0:hf], ps[hf:rows], scale, St[0:hf], ALU.mult, ALU.mult, [bps, b_tab], [bt2])
        self.stt(t2[hf:rows], ps[0:hf], scale, St[hf:rows], ALU.mult, ALU.mult, [bps, b_tab, bt2], [bt2])
        self.tt('pool', out_ap, t1[0:rows], t2[0:rows], ALU.add, [bt1, bt2], [b_out])

    def b1_phase(self, l):
        P = self.P
        P.barrier()
        P.sb_reset(self.const_end)
        Win = P.sb([128, 8, INW], BF16, 'Win')
        Wuq = P.sb([128, 2, 768], BF16, 'Wuq')
        Wukv = P.sb([128, 1, 1024], BF16, 'Wukv')
        bWin = self.wbufs(8, INW)
        bWuq = self.wbufs(2, 768)
        bWukv = self.wbufs(1, 1024)
        gq = P.sb([128, 2], F32, 'gq')
        gkv = P.sb([128, 1], F32, 'gkv')
        eps = P.sb([128, 1], F32, 'eps')
        b_small = Buf('small')
        self.memset('dve', eps, NORM_EPS, [b_small])
        self.dma('sp', gq, self.I('mla_q_norm')[l], [], [b_small])
        self.dma('sp', gkv, self.I('mla_kv_norm')[l], [], [b_small])
        xt = P.sb([128, 8, TB], F32, 'x')
        bx = Buf('x')
        h = P.sb([128, 8, TB], BF16, 'h')
        bh = [Buf('h%d' % c) for c in range(8)]
        CRt = P.sb([128, TB], F32, 'CRt')
        SRt = P.sb([128, TB], F32, 'SRt')
        CMt = P.sb([64, TB], F32, 'CMt')
        SMt = P.sb([64, TB], F32, 'SMt')
        b_tab = Buf('tab')
        stq = P.sb([128, 4, TB], BF16, 'stq'); b_stq = [Buf() for _ in range(4)]
        stk = P.sb([128, 4, TB], BF16, 'stk'); b_stk = [Buf() for _ in range(4)]
        stsg = P.sb([128, 4, TB], BF16, 'stsg'); b_stsg = [Buf() for _ in range(4)]
        stu = P.sb([128, 4, TB], BF16, 'stu'); b_stu = [Buf() for _ in range(4)]
        stv = P.sb([128, 4, 512], BF16, 'stv'); b_stv = [Buf() for _ in range(4)]
        stqn = P.sb([128, 4, TB], BF16, 'stqn'); b_stqn = [Buf() for _ in range(4)]
        stqp = P.sb([64, 4, TB], BF16, 'stqp'); b_stqp = [Buf() for _ in range(4)]
        stkn = P.sb([128, 4, TB], BF16, 'stkn'); b_stkn = [Buf() for _ in range(4)]
        stvm = P.sb([128, 4, 512], BF16, 'stvm'); b_stvm = [Buf() for _ in range(4)]
        stkp = P.sb([64, TB], BF16, 'stkp'); b_stkp = Buf()
        gtp = Pool_(P, 4, [128, TB], BF16, 'gt')
        cq = P.sb([128, 2, TB], F32, 'cq'); b_cq = [Buf(), Buf()]
        cqn = P.sb([128, 2, TB], BF16, 'cqn'); b_cqn = [Buf(), Buf()]
        ckv = P.sb([128, TB], F32, 'ckv'); b_ckv = Buf()
        ckvn = P.sb([128, TB], BF16, 'ckvn'); b_ckvn = Buf()
        t1p = Pool_(P, 2, [128, TB], F32, 't1')
        t2p = Pool_(P, 2, [128, TB], F32, 't2')
        sqpool = Pool_(P, 4, [128, TB], BF16, 'sq')
        rstd = P.sb([128, TB], F32, 'rstd'); b_rstd = Buf()
        tmp = P.sb([128, TB], F32, 'tmp'); b_tmp = Buf()
        ps = self.psum
        pp = PsumPool(ps[0:6], ['pp%d' % i for i in range(6)])
        ps_st, b_st = ps[6], Buf('pst')
        ps_st2, b_st2 = ps[7], Buf('pst2')

        self.load_weight_rows(Win, self.I('w_in')[l], 8, INW, bWin)
        self.load_weight_rows(Wuq, self.I('mla_w_uq')[l], 2, 768, bWuq)
        self.load_weight_rows(Wukv, self.I('mla_w_ukv')[l], 1, 1024, bWukv)
        rWin, rWuq, rWukv = self.flat(bWin), self.flat(bWuq), self.flat(bWukv)
        Xv = self.X.rearrange("(c p) t -> p c t", p=128)
        RSC = 128 ** -0.5
        MSC = 192 ** -0.5

        def proj(col0, ncols):
            pt, bp = pp.get()
            self.mm(pt[0:ncols, :], [(Win[:, k, col0:col0 + ncols], h[:, k, :]) for k in range(8)], bh + rWin, [bp])
            return pt, bp

        for b in range(NB):
            sl = slice(b * TB, (b + 1) * TB)
            self.dma('sp', xt, Xv[:, :, sl], self.xb('X', b), [bx])
            self.dma('sp', CRt, self.CR[:, sl], [self.dbuf('ropetab')], [b_tab])
            self.dma('sp', SRt, self.SR[:, sl], [self.dbuf('ropetab')], [b_tab])
            self.dma('sp', CMt, self.CM[:, sl], [self.dbuf('ropetab')], [b_tab])
            self.dma('sp', SMt, self.SM[:, sl], [self.dbuf('ropetab')], [b_tab])
            self.rms_stats([xt[:, c, :] for c in range(8)], [bx] * 8, TB, ps_st, b_st, sqpool, 1.0 / D, eps[:, 0:1], rstd, b_rstd, tmp, b_tmp, b_small)
            for c in range(8):
                self.stt(h[:, c, :], xt[:, c, :], self.gcol(l, 2, c), rstd, ALU.mult, ALU.mult, [bx, b_rstd, self.b_G], [bh[c]])
            for hh in range(4):
                pt, bp = proj(hh * 128, 128)
                self.rope_evac(pt, 128, CRt, SRt, b_tab, bp, 1.0, stq[:, hh, :], b_stq[hh], t1p, t2p)
                pt, bp = proj(512 + hh * 128, 128)
                self.rope_evac(pt, 128, CRt, SRt, b_tab, bp, RSC, stk[:, hh, :], b_stk[hh], t1p, t2p)
            self.dma('sp', self.QR.rearrange("(h p) t -> p h t", p=128)[:, :, sl], stq, b_stq, [self.dbuf('QR', b)])
            self.dma('sp', self.KR.rearrange("(h p) t -> p h t", p=128)[:, :, sl], stk, b_stk, [self.dbuf('KR', b)])
            for tt_ in range(4):
                pt, bp = pp.get()
                self.mm(pt, [(h[:, k, tt_ * 128:(tt_ + 1) * 128], Win[:, k, 1024:1536]) for k in range(8)], bh + rWin, [bp])
                self.copy('act', stv[:, tt_, :], pt, [bp], [b_stv[tt_]])
            self.dma('sp', self.VR[sl, :].rearrange("(t p) c -> p t c", p=128), stv, b_stv, [self.dbuf('VR', b)])
            for hh in range(4):
                pt, bp = proj(1536 + hh * 128, 128)
                self.act(stsg[:, hh, :], pt, AF.Silu, [bp], [b_stsg[hh]])
            self.dma('sp', self.SG.rearrange("(h p) t -> p h t", p=128)[:, :, sl], stsg, b_stsg, [self.dbuf('SG', b)])
            for hh in range(4):
                pt, bp = proj(2048 + hh * 128, 128)
                self.copy('act', stu[:, hh, :], pt, [bp], [b_stu[hh]])
            self.dma('sp', self.U.rearrange("(h p) t -> p h t", p=128)[:, :, sl], stu, b_stu, [self.dbuf('U', b)])
            for c2 in range(2):
                pt, bp = proj(2560 + c2 * 128, 128)
                self.copy('dve', cq[:, c2, :], pt, [bp], [b_cq[c2]])
            self.rms_stats([cq[:, 0, :], cq[:, 1, :]], b_cq, TB, ps_st2, b_st2, sqpool, 1.0 / 256, eps[:, 0:1], rstd, b_rstd, tmp, b_tmp, b_small)
            for c2 in range(2):
                self.stt(cqn[:, c2, :], cq[:, c2, :], gq[:, c2:c2 + 1], rstd, ALU.mult, ALU.mult, [b_cq[c2], b_rstd, b_small], [b_cqn[c2]])
            for hh in range(4):
                pt, bp = pp.get()
                self.mm(pt, [(Wuq[:, k2, hh * 192:hh * 192 + 128], cqn[:, k2, :]) for k2 in range(2)], b_cqn + rWuq, [bp])
                self.act(stqn[:, hh, :], pt, AF.Copy, [bp], [b_stqn[hh]], scale=MSC)
                pt, bp = pp.get()
                self.mm(pt[0:64, :], [(Wuq[:, k2, hh * 192 + 128:hh * 192 + 192], cqn[:, k2, :]) for k2 in range(2)], b_cqn + rWuq, [bp])
                self.rope_evac(pt, 64, CMt, SMt, b_tab, bp, MSC, stqp[:, hh, :], b_stqp[hh], t1p, t2p)
            self.dma('sp', self.QN.rearrange("(h p) t -> p h t", p=128)[:, :, sl], stqn, b_stqn, [self.dbuf('QN', b)])
            self.dma('sp', self.QP.rearrange("(h p) t -> p h t", p=64)[:, :, sl], stqp, b_stqp, [self.dbuf('QP', b)])
            pt, bp = proj(2816, 128)
            self.copy('dve', ckv, pt, [bp], [b_ckv])
            self.rms_stats([ckv], [b_ckv], TB, ps_st2, b_st2, sqpool, 1.0 / 128, eps[:, 0:1], rstd, b_rstd, tmp, b_tmp, b_small)
            self.stt(ckvn, ckv, gkv[:, 0:1], rstd, ALU.mult, ALU.mult, [b_ckv, b_rstd, b_small], [b_ckvn])
            for hh in range(4):
                pt, bp = pp.get()
                self.mm(pt, [(Wukv[:, 0, hh * 256:hh * 256 + 128], ckvn)], [b_ckvn] + rWukv, [bp])
                self.copy('act', stkn[:, hh, :], pt, [bp], [b_stkn[hh]])
            self.dma('sp', self.KN.rearrange("(h p) t -> p h t", p=128)[:, :, sl], stkn, b_stkn, [self.dbuf('KN', b)])
            wv = Wukv[:, 0, :].rearrange("p (h c) -> p h c", h=4)[:, :, 128:256]
            for tt_ in range(4):
                pt, bp = pp.get()
                self.mm(pt.rearrange("p (h c) -> p h c", h=4), [(ckvn[:, tt_ * 128:(tt_ + 1) * 128], wv)], [b_ckvn] + rWukv, [bp])
                self.copy('act', stvm[:, tt_, :], pt, [bp], [b_stvm[tt_]])
            self.dma('sp', self.VM[sl, :].rearrange("(t p) c -> p t c", p=128), stvm, b_stvm, [self.dbuf('VM', b)])
            pt, bp = proj(2944, 64)
            self.rope_evac(pt, 64, CMt, SMt, b_tab, bp, 1.0, stkp, b_stkp, t1p, t2p)
            self.dma('sp', self.KP[:, sl], stkp, [b_stkp], [self.dbuf('KP', b)])
            for jg in range(24):
                pt, bp = proj(3008 + jg * 128, 128)
                gt_, bgt = gtp.get()
                self.act(gt_, pt, AF.Sigmoid, [bp], [bgt])
                self.dma('sp', self.GT[jg * 128:(jg + 1) * 128, sl], gt_, [bgt], [self.dbuf('GT', b, jg)])

    def r_phase(self, l):
        P = self.P
        P.barrier()
        P.sb_reset(self.const_end)
        Wo = P.sb([128, 4, D], BF16, 'Wo')
        bWo = self.wbufs(4, D)
        self.load_weight_rows(Wo, self.I('ret_w_o')[l], 4, D, bWo)
        rWo = self.flat(bWo)
        DEC = P.sb([128, 4, 128], F32, 'DEC')
        QDEC = P.sb([128, 4, 128], F32, 'QDEC')
        KDEC = P.sb([128, 4], F32, 'KDEC')
        identb = P.sb([128, 128], BF16, 'identb')
        onesf = P.sb([128, 128], F32, 'onesf')
        epsg = P.sb([128, 1], F32, 'epsg')
        b_c = Buf('rc')
        self.dma('sp', DEC, self.I('c_dec'), [], [b_c])
        self.dma('sp', QDEC, self.I('c_qdec'), [], [b_c])
        self.dma('sp', KDEC, self.I('c_kdec'), [], [b_c])
        self.copy('dve', identb, self.ident, [self.b_ident], [b_c])
        self.memset('dve', onesf, 1.0 / 128, [b_c])
        self.memset('dve', epsg, GN_EPS, [b_c])
        S = P.sb([128, 4, 128], F32, 'S')
        Sb = P.sb([128, 4, 128], BF16, 'Sb')
        bS = [Buf('S%d' % i) for i in range(4)]
        bSb = [Buf('Sb%d' % i) for i in range(4)]
        for hh in range(4):
            self.memset('dve', S[:, hh, :], 0.0, [bS[hh]])
            self.memset('pool', Sb[:, hh, :], 0.0, [bSb[hh]])
        qt = P.sb([128, 4, TB], BF16, 'qt'); b_qt = Buf()
        kt = P.sb([128, 4, TB], BF16, 'kt'); b_kt = Buf()
        vt = P.sb([128, 4, 512], BF16, 'vt'); b_vt = Buf()
        sgt = P.sb([128, 4, TB], BF16, 'sgt'); b_sgt = Buf()
        GO = P.sb([128, 4, TB], BF16, 'GO'); bGO = [Buf() for _ in range(4)]
        ptp = Pool_(P, 8, [128, 128], BF16, 'PT')
        kdp = Pool_(P, 8, [128, 128], BF16, 'kd')
        qdp = Pool_(P, 8, [128, 128], BF16, 'qd')
        osb = [P.sb([128, TB], F32, 'osb') for _ in range(4)]; b_osb = [Buf() for _ in range(4)]
        osq = [P.sb([128, TB], F32, 'osq') for _ in range(4)]; b_osq = [Buf() for _ in range(4)]
        mean = [P.sb([128, TB], F32, 'mean') for _ in range(4)]; b_mean = [Buf() for _ in range(4)]
        var = [P.sb([128, TB], F32, 'var') for _ in range(4)]; b_var = [Buf() for _ in range(4)]
        rstd = [P.sb([128, TB], F32, 'rstd') for _ in range(4)]; b_rstd = [Buf() for _ in range(4)]
        gp = Pool_(P, 3, [128, TB], BF16, 'g0')
        mp = Pool_(P, 3, [128, TB], BF16, 'mr')
        ps = self.psum
        psc = PsumPool(ps[0:1], ['sc0'])
        pkt = PsumPool(ps[1:2], ['kt0'])
        psn = PsumPool(ps[2:3], ['sn'])
        pOs = [(ps[3 + i], Buf('O%d' % i)) for i in range(4)]
        pst = PsumPool([ps[7], ps[0], ps[1], ps[2]], ['st7', 'sc0', 'kt0', 'sn'])
        pst.bufs = [Buf('st7'), psc.bufs[0], pkt.bufs[0], psn.bufs[0]]
        G128 = [float(np.exp(128.0 * np.log1p(-np.exp2(-5.0 - hh)))) for hh in range(4)]

        for b in range(NB):
            sl = slice(b * TB, (b + 1) * TB)
            self.dma('sp', qt, self.QR.rearrange("(h p) t -> p h t", p=128)[:, :, sl], [self.dbuf('QR', b)], [b_qt])
            self.dma('sp', kt, self.KR.rearrange("(h p) t -> p h t", p=128)[:, :, sl], [self.dbuf('KR', b)], [b_kt])
            self.dma('sp', vt, self.VR[sl, :].rearrange("(t p) c -> p t c", p=128), [self.dbuf('VR', b)], [b_vt])
            self.dma('sp', sgt, self.SG.rearrange("(h p) t -> p h t", p=128)[:, :, sl], [self.dbuf('SG', b)], [b_sgt])
            def front(n, hh):
                cs = slice(n * 128, (n + 1) * 128)
                sc, bsc = psc.get()
                self.mm(sc[:, 0:128], [(kt[:, hh, cs], qt[:, hh, cs])], [b_kt, b_qt], [bsc])
                PT, bPT = ptp.get()
                self.tt('dve', PT, sc[:, 0:128], DEC[:, hh, :], ALU.mult, [bsc, b_c], [bPT])
                ktp, bktp = pkt.get()
                self.mm(ktp[:, 0:128], [(kt[:, hh, cs], identb)], [b_kt, b_c], [bktp])
                kd, bkd = kdp.get()
                self.act(kd, ktp[:, 0:128], AF.Copy, [bktp, b_c], [bkd], scale=KDEC[:, hh:hh + 1])
                qd, bqd = qdp.get()
                self.tt('pool', qd, qt[:, hh, cs], QDEC[:, hh, :], ALU.mult, [b_qt, b_c], [bqd])
                return (n, hh, PT, bPT, kd, bkd, qd, bqd)

            def back(fr):
                n, hh, PT, bPT, kd, bkd, qd, bqd = fr
                cs = slice(n * 128, (n + 1) * 128)
                O_ps, bO = pOs[hh]
                vs = vt[:, n, hh * 128:(hh + 1) * 128]
                self.mm(O_ps[:, cs], [(vs, PT), (Sb[:, hh, :], qd)], [b_vt, bPT, bSb[hh], bqd], [bO])
                sn, bsn = psn.get()
                self.mm(sn[:, 0:128], [(kd, vs)], [bkd, b_vt], [bsn])
                self.stt(S[:, hh, :], S[:, hh, :], G128[hh], sn[:, 0:128], ALU.mult, ALU.add, [bS[hh], bsn], [bS[hh]])
                self.copy('pool', Sb[:, hh, :], S[:, hh, :], [bS[hh]], [bSb[hh]])

            seq = [(n, hh) for n in range(4) for hh in range(4)]
            frs = [front(*seq[0]), front(*seq[1])]
            for i in range(len(seq)):
                if i + 2 < len(seq):
                    frs.append(front(*seq[i + 2]))
                back(frs.pop(0))
            H4 = range(4)
            for hh in H4:
                self.copy('act', osb[hh], pOs[hh][0], [pOs[hh][1]], [b_osb[hh]])
            for hh in H4:
                self.act(osq[hh], osb[hh], AF.Square, [b_osb[hh]], [b_osq[hh]])
            mpss, qpss = [], []
            for hh in H4:
                mps, bmps = pst.get()
                self.mm(mps, [(onesf, osb[hh])], [b_c, b_osb[hh]], [bmps])
                self.copy('act', mean[hh], mps, [bmps], [b_mean[hh]])
            for hh in H4:
                qps, bqps = pst.get()
                self.mm(qps, [(onesf, osq[hh])], [b_c, b_osq[hh]], [bqps])
                self.copy('act', var[hh], qps, [bqps], [b_var[hh]])
            for hh in H4:
                self.act(osq[hh], mean[hh], AF.Square, [b_mean[hh]], [b_osq[hh]])
            for hh in H4:
                self.tt('dve', var[hh], var[hh], osq[hh], ALU.subtract, [b_var[hh], b_osq[hh]], [b_var[hh]])
            for hh in H4:
                self.act(var[hh], var[hh], AF.Sqrt, [b_var[hh], b_c], [b_var[hh]], bias=epsg[:, 0:1])
            for hh in H4:
                self.P.add('dve', (lambda hh: lambda e: e.reciprocal(out=rstd[hh], in_=var[hh]))(hh), reads=[b_var[hh]], writes=[b_rstd[hh]])
            for hh in H4:
                self.tt('dve', osb[hh], osb[hh], mean[hh], ALU.subtract, [b_osb[hh], b_mean[hh]], [b_osb[hh]])
            for hh in H4:
                self.tt('pool', osb[hh], osb[hh], rstd[hh], ALU.mult, [b_osb[hh], b_rstd[hh]], [b_osb[hh]])
            for hh in H4:
                self.tt('pool', GO[:, hh, :], osb[hh], sgt[:, hh, :], ALU.mult, [b_osb[hh], b_sgt], [bGO[hh]])
            for c in range(8):
                yps, byps = pst.get()
                self.mm(yps, [(Wo[:, hh, c * 128:(c + 1) * 128], GO[:, hh, :]) for hh in range(4)], bGO + rWo, [byps])
                g0, bg0 = gp.get()
                self.dma('sp', g0, self.GT[c * 128:(c + 1) * 128, sl], [self.dbuf('GT', b, c)], [bg0])
                mr, bmr = mp.get()
                self.tt('dve', mr, yps, g0, ALU.mult, [byps, bg0], [bmr])
                self.dma('sp', self.MR[c * 128:(c + 1) * 128, sl], mr, [bmr], [self.dbuf('MR', b, c)])

    def m_phase(self, l):
        P = self.P
        P.barrier()
        P.sb_reset(self.const_end)
        KNs = P.sb([128, 4, L], BF16, 'KNs')
        KPs = P.sb([64, L], BF16, 'KPs')
        VMs = P.sb([128, 32, 512], BF16, 'VMs')
        bKV = [Buf('kv%d' % b) for b in range(NB)]
        Wmo = P.sb([128, 4, D], BF16, 'Wmo')
        Wout = P.sb([128, 8, D], BF16, 'Wout')
        bWmo = self.wbufs(4, D)
        bWout = self.wbufs(8, D)
        self.load_weight_rows(Wmo, self.I('mla_w_o')[l], 4, D, bWmo)
        self.load_weight_rows(Wout, self.I('w_out')[l], 8, D, bWout)
        rWmo, rWout = self.flat(bWmo), self.flat(bWout)
        trif = P.sb([128, 128], F32, 'trif')
        tri = P.sb([128, 128], BF16, 'tri')
        eps = P.sb([128, 1], F32, 'eps')
        b_c = Buf('mc')
        self.dma('sp', trif, self.I('c_tri'), [], [b_c])
        self.copy('dve', tri, trif, [b_c], [b_c])
        self.memset('dve', eps, NORM_EPS, [b_c])
        qn = P.sb([128, 4, TB], BF16, 'qn'); b_qn = Buf()
        qp = P.sb([64, 4, TB], BF16, 'qp'); b_qp = Buf()
        ptp = Pool_(P, 6, [128, TB], BF16, 'PT')
        omlas = [P.sb([128, 4, TB], BF16, 'omla') for _ in range(2)]; b_oms = [[Buf() for _ in range(4)] for _ in range(2)]
        rden = P.sb([128, TB], F32, 'rden'); b_rden = Buf()
        merged = P.sb([128, 8, TB], BF16, 'merged'); b_mg = [Buf() for _ in range(8)]
        g2p = Pool_(P, 3, [128, TB], BF16, 'g2')
        mrp = Pool_(P, 3, [128, TB], BF16, 'mrl')
        msp = Pool_(P, 3, [128, TB], BF16, 'msl')
        accp = Pool_(P, 2, [128, TB], F32, 'acc')
        y = P.sb([128, 8, TB], F32, 'y'); by = [Buf() for _ in range(8)]
        xcp = Pool_(P, 3, [128, TB], F32, 'xc')
        sqpool = Pool_(P, 4, [128, TB], BF16, 'sq')
        rstd = P.sb([128, TB], F32, 'rstd'); b_rstd = Buf()
        tmp = P.sb([128, TB], F32, 'tmp'); b_tmp = Buf()
        ps = self.psum
        pS = PsumPool(ps[0:2], ['S0', 'S1'])
        pO = PsumPool(ps[2:4], ['O0', 'O1'])
        pDn = PsumPool(ps[4:6], ['D0', 'D1'])
        pY = PsumPool(ps[6:7], ['Y0'])
        st_ps, b_st = ps[7], Buf('mst')

        def make_tail(b, omla, b_om):
            sl = slice(b * TB, (b + 1) * TB)
            steps = []

            def merge_step(c):
                yps, byps = pY.get()
                self.mm(yps, [(Wmo[:, hh, c * 128:(c + 1) * 128], omla[:, hh, :]) for hh in range(4)], b_om + rWmo, [byps])
                g2, bg2 = g2p.get()
                self.dma('sp', g2, self.GT[2048 + c * 128:2048 + (c + 1) * 128, sl], [self.dbuf('GT', b, 16 + c)], [bg2])
                mr, bmr = mrp.get()
                self.dma('sp', mr, self.MR[c * 128:(c + 1) * 128, sl], [self.dbuf('MR', b, c)], [bmr])
                ms, bms = msp.get()
                self.dma('sp', ms, self.MS[c * 128:(c + 1) * 128, sl], [self.dbuf('MS', b, c)], [bms])
                acc, bacc = accp.get()
                self.tt('dve', acc, yps, g2, ALU.mult, [byps, bg2], [bacc])
                self.tt('pool', acc, acc, mr, ALU.add, [bacc, bmr], [bacc])
                self.tt('pool', merged[:, c, :], acc, ms, ALU.add, [bacc, bms], [b_mg[c]])

            def wout_step(c2):
                yps, byps = pY.get()
                self.mm(yps, [(Wout[:, c, c2 * 128:(c2 + 1) * 128], merged[:, c, :]) for c in range(8)], b_mg + rWout, [byps])
                self.copy('dve', y[:, c2, :], yps, [byps], [by[c2]])
                sq, bsq = sqpool.get()
                self.act(sq, y[:, c2, :], AF.Square, [by[c2]], [bsq])
                self.P.add('pe', (lambda sq, c2: lambda e: e.matmul(st_ps, lhsT=self.ones_bf, rhs=sq, start=(c2 == 0), stop=(c2 == 7)))(sq, c2),
                           reads=[bsq, self.b_ones], writes=[b_st])

            def norm_step():
                self.act(tmp, st_ps, AF.Sqrt, [b_st, b_c], [b_tmp], scale=1.0 / D, bias=eps[:, 0:1])
                self.P.add('dve', lambda e: e.reciprocal(out=rstd, in_=tmp), reads=[b_tmp], writes=[b_rstd])

            def res_step(c2):
                xc, bxc = xcp.get()
                self.dma('sp', xc, self.X[c2 * 128:(c2 + 1) * 128, sl], [self.dbuf('X', b, c2)], [bxc])
                self.tt('dve', y[:, c2, :], y[:, c2, :], rstd, ALU.mult, [by[c2], b_rstd], [by[c2]])
                self.stt(xc, y[:, c2, :], self.gcol(l, 3, c2), xc, ALU.mult, ALU.add, [by[c2], bxc, self.b_G], [bxc])
                self.dma('sp', self.X[c2 * 128:(c2 + 1) * 128, sl], xc, [bxc], [self.dbuf('X', b, c2)])

            for c in range(8):
                steps.append((lambda c: lambda: merge_step(c))(c))
            for c2 in range(8):
                steps.append((lambda c2: lambda: wout_step(c2))(c2))
            steps.append(norm_step)
            for c2 in range(8):
                steps.append((lambda c2: lambda: res_step(c2))(c2))
            return steps

        pending_tail = []
        for b in range(NB):
            sl = slice(b * TB, (b + 1) * TB)
            self.dma('sp', KNs[:, :, sl], self.KN.rearrange("(h p) t -> p h t", p=128)[:, :, sl], [self.dbuf('KN', b)], [bKV[b]])
            self.dma('sp', KPs[:, sl], self.KP[:, sl], [self.dbuf('KP', b)], [bKV[b]])
            self.dma('sp', VMs[:, 4 * b:4 * b + 4, :], self.VM[sl, :].rearrange("(t p) c -> p t c", p=128), [self.dbuf('VM', b)], [bKV[b]])
            self.dma('sp', qn, self.QN.rearrange("(h p) t -> p h t", p=128)[:, :, sl], [self.dbuf('QN', b)], [b_qn])
            self.dma('sp', qp, self.QP.rearrange("(h p) t -> p h t", p=64)[:, :, sl], [self.dbuf('QP', b)], [b_qp])
            nkt = 4 * (b + 1)
            omla, b_om = omlas[b % 2], b_oms[b % 2]
            for hh in range(4):
                O_ps, bO = pO.get()
                Dn, bDn = pDn.get()
                def emit_S(kt, hh=hh):
                    c0 = 128 * (kt - 4 * b) if kt >= 4 * b else 0
                    ks = slice(kt * 128, (kt + 1) * 128)
                    kvb = bKV[kt // 4]
                    S_ps, bS_ = pS.get()
                    self.mm(S_ps[:, c0:TB], [(KNs[:, hh, ks], qn[:, hh, c0:TB]), (KPs[0:64, ks], qp[0:64, hh, c0:TB])], [kvb, b_qn, b_qp], [bS_])
                    return S_ps, bS_, c0, kvb
                nxt = emit_S(0)
                for kt in range(nkt):
                    S_ps, bS_, c0, kvb = nxt
                    if kt + 1 < nkt:
                        nxt = emit_S(kt + 1)
                    PT, bPT = ptp.get()
                    self.act(PT[:, c0:TB], S_ps[:, c0:TB], AF.Exp, [bS_], [bPT])
                    if kt >= 4 * b:
                        self.tt('pool', PT[:, c0:c0 + 128], PT[:, c0:c0 + 128], tri, ALU.mult, [bPT, b_c], [bPT])
                    first, lastk = (kt == 0), (kt == nkt - 1)
                    self.P.add('pe', (lambda O_ps, PT, c0, kt, hh, first, lastk: lambda e: e.matmul(
                        O_ps[:, c0:TB], lhsT=VMs[:, kt, hh * 128:(hh + 1) * 128], rhs=PT[:, c0:TB], start=first, stop=lastk))(O_ps, PT, c0, kt, hh, first, lastk),
                        reads=[kvb, bPT], writes=[bO])
                    self.P.add('pe', (lambda Dn, PT, c0, first, lastk: lambda e: e.matmul(
                        Dn[:, c0:TB], lhsT=self.ones_bf, rhs=PT[:, c0:TB], start=first, stop=lastk))(Dn, PT, c0, first, lastk),
                        reads=[self.b_ones, bPT], writes=[bDn])
                    if pending_tail:
                        pending_tail.pop(0)()
                self.P.add('dve', (lambda Dn: lambda e: e.reciprocal(out=rden, in_=Dn))(Dn), reads=[bDn], writes=[b_rden])
                self.tt('dve', omla[:, hh, :], O_ps, rden, ALU.mult, [bO, b_rden], [b_om[hh]])
            while pending_tail:
                pending_tail.pop(0)()
            pending_tail.extend(make_tail(b, omla, b_om))
        while pending_tail:
            pending_tail.pop(0)()

    def trig(self, ang, b_ang, n, out_sin, out_cos, b_out, wk):
        MAG = 12582912.0
        C1 = 6.28125
        C2 = 2.0 * math.pi - 6.28125
        a2, k, b_w = wk
        for dst, shift in ((out_sin, 0.0), (out_cos, math.pi / 2)):
            self.ts('dve', a2[:, 0:n], ang, shift, None, ALU.add, None, [b_ang], [b_w])
            self.ts('dve', k[:, 0:n], a2[:, 0:n], 1.0 / (2.0 * math.pi), MAG, ALU.mult, ALU.add, [b_w], [b_w])
            self.ts('dve', k[:, 0:n], k[:, 0:n], -MAG, None, ALU.add, None, [b_w], [b_w])
            self.stt(a2[:, 0:n], k[:, 0:n], -C1, a2[:, 0:n], ALU.mult, ALU.add, [b_w], [b_w])
            self.stt(a2[:, 0:n], k[:, 0:n], -C2, a2[:, 0:n], ALU.mult, ALU.add, [b_w], [b_w])
            self.ts('dve', a2[:, 0:n], a2[:, 0:n], 3.141592, -3.141592, ALU.min, ALU.max, [b_w], [b_w])
            self.act(dst, a2[:, 0:n], AF.Sin, [b_w], [b_out])

    def s_phase(self, l):
        P = self.P
        P.barrier()
        P.sb_reset(self.const_end)
        T = ST
        ps = self.psum
        Wa = P.sb([128, 4, D], BF16, 'Wa')
        Wb = P.sb([128, 4, D], BF16, 'Wb')
        bWa = self.wbufs(4, D)
        bWb = self.wbufs(4, D)
        self.load_weight_rows(Wa, self.I('s5_glu_a')[l], 4, D, bWa)
        self.load_weight_rows(Wb, self.I('s5_glu_b')[l], 4, D, bWb)
        rWa, rWb = self.flat(bWa), self.flat(bWb)
        are = P.sb([128, 16], F32); aim = P.sb([128, 16], F32); ldt = P.sb([128, 16], F32)
        bre = P.sb([128, 16, 16], F32); bim = P.sb([128, 16, 16], F32)
        cre = P.sb([128, 16, 16], F32); cim = P.sb([128, 16, 16], F32)
        dsk = P.sb([128, 4], F32)
        step = P.sb([128, N2], F32)
        b_par = Buf('s5par')
        for t_, nm in ((are, 's5_are'), (aim, 's5_aim'), (ldt, 's5_ldt'), (bre, 's5_bre'), (bim, 's5_bim'), (cre, 's5_cre'), (cim, 's5_cim'), (dsk, 's5_d')):
            self.dma('sp', t_, self.I(nm)[l], [], [b_par])
        self.dma('sp', step, self.I('c_step'), [], [b_par])
        adt = P.sb([128, 16], F32); th = P.sb([128, 16], F32); dtt = P.sb([128, 16], F32)
        b_adt = Buf('adt')
        self.act(dtt, ldt, AF.Exp, [b_par], [b_adt])
        self.tt('dve', adt, are, dtt, ALU.mult, [b_par, b_adt], [b_adt])
        self.tt('dve', th, aim, dtt, ALU.mult, [b_par, b_adt], [b_adt])
        wk = (P.sb([128, 4 * N2], F32, 'wk_a2'), P.sb([128, 4 * N2], F32, 'wk_k'), Buf('wk'))
        angs = P.sb([128, 16], F32); b_angs = Buf()
        mag = P.sb([128, 16], F32); b_mag = Buf()
        sn = P.sb([128, 16], F32); cs = P.sb([128, 16], F32); b_sc = Buf()

        def lam_pow(n, lr, li, nli, b_l):
            self.act(mag, adt, AF.Exp, [b_adt], [b_mag], scale=float(n))
            self.ts('dve', angs, th, float(n), None, ALU.mult, None, [b_adt], [b_angs])
            self.trig(angs, b_angs, 16, sn, cs, b_sc, wk)
            self.tt('dve', lr, mag, cs, ALU.mult, [b_mag, b_sc], [b_l])
            self.tt('dve', li, mag, sn, ALU.mult, [b_mag, b_sc], [b_l])
            self.ts('dve', nli, li, -1.0, None, ALU.mult, None, [b_l], [b_l])

        lr = P.sb([128, 16], F32); li = P.sb([128, 16], F32); nli = P.sb([128, 16], F32); b_l = Buf('lam')
        lam_pow(1, lr, li, nli, b_l)
        fre = P.sb([128, 16], F32); fim = P.sb([128, 16], F32); b_f = Buf('f')
        den = P.sb([128, 16], F32); t16 = P.sb([128, 16], F32); nr = P.sb([128, 16], F32)
        self.tt('dve', den, are, are, ALU.mult, [b_par], [b_f])
        self.tt('dve', t16, aim, aim, ALU.mult, [b_par], [b_f])
        self.tt('dve', den, den, t16, ALU.add, [b_f], [b_f])
        self.P.add('dve', lambda e: e.reciprocal(out=den, in_=den), reads=[b_f], writes=[b_f])
        self.ts('dve', nr, lr, -1.0, None, ALU.add, None, [b_l], [b_f])
        self.tt('dve', fre, nr, are, ALU.mult, [b_f, b_par], [b_f])
        self.tt('dve', t16, li, aim, ALU.mult, [b_l, b_par], [b_f])
        self.tt('dve', fre, fre, t16, ALU.add, [b_f], [b_f])
        self.tt('dve', fre, fre, den, ALU.mult, [b_f], [b_f])
        self.tt('dve', fim, li, are, ALU.mult, [b_l, b_par], [b_f])
        self.tt('dve', t16, nr, aim, ALU.mult, [b_f, b_par], [b_f])
        self.tt('dve', fim, fim, t16, ALU.subtract, [b_f], [b_f])
        self.tt('dve', fim, fim, den, ALU.mult, [b_f], [b_f])

        def bc(a):
            return a.unsqueeze(2).to_broadcast([128, 16, 16])

        def cmul(out_re, out_im, sr, si, xr, xi, reads, b_o, t_a, neg_im=False):
            self.tt('dve', out_re, xr, bc(sr), ALU.mult, reads, [b_o])
            self.tt('dve', t_a, xi, bc(si), ALU.mult, reads, [b_o])
            self.tt('dve', out_re, out_re, t_a, ALU.subtract, [b_o], [b_o])
            self.tt('dve', out_im, xi, bc(sr), ALU.mult, reads, [b_o])
            self.tt('dve', t_a, xr, bc(si), ALU.mult, reads, [b_o])
            self.tt('dve', out_im, out_im, t_a, ALU.add, [b_o], [b_o])
            if neg_im:
                self.ts('dve', out_im, out_im, -1.0, None, ALU.mult, None, [b_o], [b_o])

        bbre = P.sb([128, 16, 16], F32); bbim = P.sb([128, 16, 16], F32); b_bb = Buf('bb')
        t256 = P.sb([128, 16, 16], F32)
        cmul(bbre, bbim, fre, fim, bre, bim, [b_f, b_par], b_bb, t256)
        NatB = [P.sb([128, 16, 128], F32, 'NatBre', top=True), P.sb([128, 16, 128], F32, 'NatBim', top=True)]
        NatT = [P.sb([128, 16, 128], F32, 'NatTre', top=True), P.sb([128, 16, 128], F32, 'NatTim', top=True)]
        b_natB = Buf('natB'); b_natT = Buf('natT')
        for t_ in NatB:
            self.memset('pool', t_, 0.0, [b_natB])
        for t_ in NatT:
            self.memset('pool', t_, 0.0, [b_natT])

        def scatter(nat, src, b_src, b_nat):
            for m in range(4):
                for g2 in range(2):
                    rows = slice(64 * g2, 64 * g2 + 64)
                    dst = nat.rearrange("p (c m) x -> p c m x", m=4)[rows, :, m, m * 32 + g2 * 16:m * 32 + g2 * 16 + 16]
                    srcv = src.rearrange("p (c m) h -> p c m h", m=4)[rows, :, m, :]
                    self.copy('pool', dst, srcv, [b_src], [b_nat])

        scatter(NatB[0], bbre, b_bb, b_natB)
        scatter(NatB[1], bbim, b_bb, b_natB)
        identf = self.ident
        BfT = P.sb([128, 16 * T * 2, 128], BF16, 'BfT'); b_BfT = Buf('BfT')
        xr = P.sb([128, 16, 16], F32); xi = P.sb([128, 16, 16], F32); b_x = Buf('x')
        psetup = PsumPool(ps[0:4], ['su%d' % i for i in range(4)])
        for j in range(T):
            if j == T - 1:
                natre, natim, b_nat = NatB[0], NatB[1], b_natB
            else:
                lam_pow(T - 1 - j, lr, li, nli, b_l)
                cmul(xr, xi, lr, li, bbre, bbim, [b_l, b_bb], b_x, t256)
                scatter(NatT[0], xr, b_x, b_natT)
                scatter(NatT[1], xi, b_x, b_natT)
                natre, natim, b_nat = NatT[0], NatT[1], b_natT
            for ri, nat in ((0, natre), (1, natim)):
                for q in range(16):
                    pt, bp = psetup.get()
                    self.mm(pt[:, 0:128], [(nat[:, q, :], identf)], [b_nat, self.b_ident], [bp])
                    self.copy('act', BfT[:, (q * T + j) * 2 + ri, :], pt[:, 0:128], [bp], [b_BfT])
        Cf = P.sb([128, 16 * T * 2, 64], BF16, 'Cf'); b_Cf = Buf('Cf')
        self.memset('pool', Cf, 0.0, [b_Cf])
        Kd = P.sb([128, 4 * T, 128], BF16, 'Kd'); b_Kd = Buf('Kd')
        Cfv = Cf.rearrange("p (q j r) x -> p q j r x", q=16, j=T)
        for d in range(T + 1):
            if d == 0:
                self.copy('dve', xr, cre, [b_par], [b_x])
                self.ts('dve', xi, cim, -1.0, None, ALU.mult, None, [b_par], [b_x])
            else:
                lam_pow(d, lr, li, nli, b_l)
                cmul(xr, xi, lr, li, cre, cim, [b_l, b_par], b_x, t256, neg_im=True)
            if d >= 1:
                j = d - 1
                for ri, src in ((0, xr), (1, xi)):
                    for g2 in range(2):
                        rows = slice(64 * g2, 64 * g2 + 64)
                        for mm_ in range(2):
                            co = mm_ * 32 + g2 * 16
                            dstv = Cfv.rearrange("p (a b) j r x -> p a b j r x", b=2)[rows, :, mm_, j, ri, co:co + 16]
                            srcv = src.rearrange("p (a b) h -> p a b h", b=2)[rows, :, mm_, :]
                            self.copy('pool', dstv, srcv, [b_x], [b_Cf])
            if d <= T - 1:
                scatter(NatT[0], xr, b_x, b_natT)
                scatter(NatT[1], xi, b_x, b_natT)
                for c4 in range(4):
                    pt, bp = psetup.get()
                    pairs = []
                    for m in range(4):
                        pairs.append((NatB[0][:, 4 * c4 + m, :], NatT[0][:, 4 * c4 + m, :]))
                        pairs.append((NatB[1][:, 4 * c4 + m, :], NatT[1][:, 4 * c4 + m, :]))
                    self.mm(pt[:, 0:128], pairs, [b_natB, b_natT], [bp])
                    self.copy('act', Kd[:, c4 * T + d, :], pt[:, 0:128], [bp], [b_Kd])
        cosE = P.sb([128, 16, N2], F32, 'cosE'); sinE = P.sb([128, 16, N2], F32, 'sinE'); b_E = Buf('E')
        angE = P.sb([128, 16, N2], F32, 'angE', top=True); b_angE = Buf('angE')
        thT = P.sb([128, 16], F32)
        rho = P.sb([128, 16], F32); b_rho = Buf('rho')
        self.ts('dve', thT, th, float(T), None, ALU.mult, None, [b_adt], [b_rho])
        self.act(rho, adt, AF.Exp, [b_adt], [b_rho], scale=float(T))
        for q in range(16):
            self.ts('dve', angE[:, q, :], step, thT[:, q:q + 1], None, ALU.mult, None, [b_par, b_rho], [b_angE])
        for qq in range(4):
            self.trig(angE[:, 4 * qq:4 * qq + 4, :].rearrange("p q k -> p (q k)"), b_angE, 4 * N2, sinE[:, 4 * qq:4 * qq + 4, :].rearrange("p q k -> p (q k)"),
                      cosE[:, 4 * qq:4 * qq + 4, :].rearrange("p q k -> p (q k)"), b_E, wk)
        Zf = P.sb([128, 16, 2, N2 + 1], F32, 'Zf'); bZ = [Buf('Z%d' % q) for q in range(16)]
        bZc = [Buf('Zc%d' % c4) for c4 in range(4)]
        self.memset('pool', Zf, 0.0, bZc)
        P.barrier()
        P.top_off = 0
        ub = P.sb([128, 4, TB], BF16, 'ub'); b_ub = Buf('ub')
        tp = Pool_(P, 8, [128, 4, N2], F32, 'tS')
        Xp = Pool_(P, 4, [128, 4, N2], F32, 'XS')
        Wp = Pool_(P, 4, [128, 4, N2], F32, 'WS')
        Zbp = Pool_(P, 2, [128, 4, 2, N2], BF16, 'Zb')
        ysf = P.sb([128, 4, TB], BF16, 'ysf'); b_ysf = [Buf() for _ in range(4)]
        ysum = Pool_(P, 2, [128, TB], F32, 'ysum')
        sgp = Pool_(P, 2, [128, TB], F32, 'sgb')
        g1p = Pool_(P, 3, [128, TB], BF16, 'g1')
        mtp = Pool_(P, 2, [128, TB], F32, 'mt')
        msp = Pool_(P, 3, [128, TB], BF16, 'ms')
        pVr = PsumPool(ps[0:2], ['Vr0', 'Vr1'])
        pVi = PsumPool(ps[2:4], ['Vi0', 'Vi1'])
        pYs = PsumPool(ps[4:6], ['Ys0', 'Ys1'])
        pA = PsumPool(ps[6:7], ['A0'])
        pB = PsumPool(ps[7:8], ['B0'])

        def v4(t):
            return t.rearrange("p (m k) -> p m k", m=4)

        for b in range(NB):
            sl = slice(b * TB, (b + 1) * TB)
            self.dma('sp', ub, self.U.rearrange("(h p) t -> p h t", p=128)[:, :, sl], [self.dbuf('U', b)], [b_ub])
            for c4 in range(4):
                q0 = 4 * c4
                Vr, bVr = pVr.get()
                Vi, bVi = pVi.get()
                for m in range(4):
                    rs = slice(32 * m, 32 * m + 32)
                    for ri, (Vt, bVt) in enumerate(((Vr, bVr), (Vi, bVi))):
                        prs = [(BfT[:, ((q0 + m) * T + j) * 2 + ri, :], ub[:, c4, j:TB:T]) for j in range(T)]
                        self.mm(Vt[:, m * N2:(m + 1) * N2], prs, [b_BfT, b_ub], [bVt])
                cE, sE = cosE[:, q0:q0 + 4, :], sinE[:, q0:q0 + 4, :]
                (t1, bt1), (t2, bt2), (t3, bt3), (t4, bt4) = [tp.get() for _ in range(4)]
                self.tt('dve', t1, v4(Vr), cE, ALU.mult, [bVr, b_E], [bt1])
                self.tt('dve', t2, v4(Vi), sE, ALU.mult, [bVi, b_E], [bt2])
                self.tt('dve', t3, v4(Vi), cE, ALU.mult, [bVi, b_E], [bt3])
                self.tt('dve', t4, v4(Vr), sE, ALU.mult, [bVr, b_E], [bt4])
                (Xre, bXre), (Xim, bXim) = Xp.get(), Xp.get()
                self.tt('pool', Xre, t1, t2, ALU.add, [bt1, bt2], [bXre])
                self.tt('pool', Xim, t3, t4, ALU.subtract, [bt3, bt4], [bXim])
                self.copy('pool', Zf[:, q0:q0 + 4, :, 0:1], Zf[:, q0:q0 + 4, :, N2:N2 + 1], [bZc[c4]], [bZc[c4]])
                (Wre, bWre), (Wim, bWim) = Wp.get(), Wp.get()
                for m in range(4):
                    q = q0 + m
                    rb = rho[:, q:q + 1].to_broadcast([128, N2])
                    self.P.add('dve', (lambda Wre, rb, Xre, q, m: lambda e: e.tensor_tensor_scan(out=Wre[:, m, :], data0=rb, data1=Xre[:, m, :], initial=Zf[:, q, 0, 0:1], op0=ALU.mult, op1=ALU.add))(Wre, rb, Xre, q, m),
                               reads=[b_rho, bXre, bZc[c4]], writes=[bWre])
                    self.P.add('dve', (lambda Wim, rb, Xim, q, m: lambda e: e.tensor_tensor_scan(out=Wim[:, m, :], data0=rb, data1=Xim[:, m, :], initial=Zf[:, q, 1, 0:1], op0=ALU.mult, op1=ALU.add))(Wim, rb, Xim, q, m),
                               reads=[b_rho, bXim, bZc[c4]], writes=[bWim])
                (u1, bu1), (u2, bu2), (u3, bu3), (u4, bu4) = [tp.get() for _ in range(4)]
                self.tt('pool', u1, Wre, cE, ALU.mult, [bWre, b_E], [bu1])
                self.tt('pool', u2, Wim, sE, ALU.mult, [bWim, b_E], [bu2])
                self.tt('pool', u3, Wim, cE, ALU.mult, [bWim, b_E], [bu3])
                self.tt('pool', u4, Wre, sE, ALU.mult, [bWre, b_E], [bu4])
                self.tt('dve', Zf[:, q0:q0 + 4, 0, 1:N2 + 1], u1, u2, ALU.subtract, [bu1, bu2], [bZc[c4]])
                self.tt('dve', Zf[:, q0:q0 + 4, 1, 1:N2 + 1], u3, u4, ALU.add, [bu3, bu4], [bZc[c4]])
                Zb, bZb = Zbp.get()
                for m in range(4):
                    self.copy('act', Zb[:, m], Zf[:, q0 + m, :, 0:N2], [bZc[c4]], [bZb])
                Yp, bY = pYs.get()
                for j in range(T):
                    ops_ = []
                    for i in range(j + 1):
                        ops_.append((Yp[:, j:TB:T], Kd[:, c4 * T + (j - i), :], ub[:, c4, i:TB:T]))
                    for m in range(4):
                        q = q0 + m
                        for ri in range(2):
                            ops_.append((Yp[64 * (m // 2):64 * (m // 2) + 64, j:TB:T], Cfv[:, q, j, ri, :], Zb[:, m, ri, :]))

                    def fn(e, ops_=ops_):
                        ins = None
                        for ii, (o_, l_, r_) in enumerate(ops_):
                            ins = e.matmul(o_, lhsT=l_, rhs=r_, start=(ii == 0), stop=(ii == len(ops_) - 1))
                        return ins
                    self.P.add('pe', fn, reads=[b_Kd, b_Cf, b_ub, bZb], writes=[bY])
                ys_, bys = ysum.get()
                self.stt(ys_, ub[:, c4, :], dsk[:, c4:c4 + 1], Yp, ALU.mult, ALU.add, [b_ub, b_par, bY], [bys])
                self.act(ysf[:, c4, :], ys_, AF.Gelu, [bys], [b_ysf[c4]])
            for c in range(8):
                Ap, bA = pA.get()
                Bp, bB = pB.get()
                self.mm(Ap, [(Wa[:, c4, c * 128:(c + 1) * 128], ysf[:, c4, :]) for c4 in range(4)], b_ysf + rWa, [bA])
                self.mm(Bp, [(Wb[:, c4, c * 128:(c + 1) * 128], ysf[:, c4, :]) for c4 in range(4)], b_ysf + rWb, [bB])
                sgb, bsg = sgp.get()
                self.act(sgb, Bp, AF.Sigmoid, [bB], [bsg])
                g1, bg1 = g1p.get()
                self.dma('sp', g1, self.GT[1024 + c * 128:1024 + (c + 1) * 128, sl], [self.dbuf('GT', b, 8 + c)], [bg1])
                mt, bmt = mtp.get()
                self.tt('dve', mt, Ap, sgb, ALU.mult, [bA, bsg], [bmt])
                ms, bms = msp.get()
                self.tt('pool', ms, mt, g1, ALU.mult, [bmt, bg1], [bms])
                self.dma('sp', self.MS[c * 128:(c + 1) * 128, sl], ms, [bms], [self.dbuf('MS', b, c)])

    def finish(self):
        P = self.P
        if self.dump:
            name, shape, dt = self.dump
            src = self.scratch[name]
            P.barrier()
            self.dma('sp', self.dbg, src, [], [Buf()])
        P.barrier()
        P.flush()
        P.close()
        return self.nc

    def build(self):
        self.declare()
        self.consts()
        nl = self.n_layers
        ph = self.phases
        for l in range(nl):
            src, sname = (self.I('xT'), 'xT') if l == 0 else (self.X, 'X')
            if l == 0 and (ph is None or 'B1' in ph):
                self.rope_tables()
            if ph is None or 'F1' in ph:
                self.ffn_phase(l, 0, src, sname, self.X, 'X')
            if ph is None or 'B1' in ph:
                self.b1_phase(l)
            if ph is None or 'R' in ph:
                self.r_phase(l)
            if ph is None or 'S' in ph:
                self.s_phase(l)
            if ph is None or 'M' in ph:
                self.m_phase(l)
            if ph is None or 'F2' in ph:
                last = (l == nl - 1)
                self.ffn_phase(l, 1, self.X, 'X', self.out if last else self.X, 'out' if last else 'X')
        return self.finish()


def host_constants():
    c = {}
    c['c_ident'] = np.eye(128, dtype=np.float32)
    m = np.arange(128)
    c['c_tri'] = (m[None, :] >= m[:, None]).astype(np.float32)
    lg = np.log1p(-np.exp2(-5.0 - np.arange(4, dtype=np.float64)))
    rel = (m[None, :] - m[:, None]).astype(np.float64)
    dec = np.zeros((128, 4, 128), np.float32)
    for h in range(4):
        dec[:, h, :] = np.where(rel >= 0, np.exp(np.maximum(rel, 0) * lg[h]), 0.0)
    c['c_dec'] = dec
    qd = np.zeros((128, 4, 128), np.float32)
    for h in range(4):
        qd[:, h, :] = np.exp((m + 1.0) * lg[h])[None, :]
    c['c_qdec'] = qd
    c['c_kdec'] = np.stack([np.exp((127.0 - m) * lg[h]) for h in range(4)], axis=1).astype(np.float32)
    fr = np.zeros((128, 2), np.float32)
    inv_r = (10000.0 ** (-np.arange(0, 128, 2, dtype=np.float32) / 128)).astype(np.float32)
    inv_m = (10000.0 ** (-np.arange(0, 64, 2, dtype=np.float32) / 64)).astype(np.float32)
    fr[:, 0] = np.concatenate([inv_r, inv_r])
    fr[:64, 1] = np.concatenate([inv_m, inv_m])
    c['c_freq'] = fr
    sg = np.ones((128, 2), np.float32)
    sg[:64, 0] = -1.0
    sg[:32, 1] = -1.0
    c['c_sgn'] = sg
    c['c_step'] = np.broadcast_to(np.arange(1, N2 + 1, dtype=np.float32)[None, :], (128, N2)).copy()
    return c


def host_layout(inp, b):
    m = {}
    m['xT'] = np.ascontiguousarray(inp['x'][b].T)
    m['pos'] = np.ascontiguousarray(inp['positions'][b:b + 1]).astype(np.int32)
    g = np.asarray(inp['norm_gains'], np.float32)
    m['gains'] = np.ascontiguousarray(g.reshape(DEPTH, 6, 8, 128).transpose(3, 0, 1, 2).reshape(128, DEPTH * 48))
    for k in ('ffn_w_gate', 'ffn_w_up', 'ffn_w_down', 'w_in', 'ret_w_o', 's5_glu_a', 's5_glu_b', 'mla_w_uq', 'mla_w_ukv', 'mla_w_o', 'w_out'):
        m[k] = np.ascontiguousarray(inp[k], dtype=np.float32)

    def pair(a):
        return np.ascontiguousarray(a.reshape(DEPTH, 16, 2, 64).transpose(0, 2, 3, 1).reshape(DEPTH, 128, 16))
    m['s5_are'] = pair(np.asarray(inp['s5_a_re']))
    m['s5_aim'] = pair(np.asarray(inp['s5_a_im']))
    m['s5_ldt'] = pair(np.repeat(np.asarray(inp['s5_log_dt'])[:, :, None], 64, axis=2))
    m['s5_bre'] = np.ascontiguousarray(np.asarray(inp['s5_b_re']).reshape(DEPTH, 16, 2, 64, 16).transpose(0, 2, 3, 1, 4).reshape(DEPTH, 128, 16, 16))
    m['s5_bim'] = np.ascontiguousarray(np.asarray(inp['s5_b_im']).reshape(DEPTH, 16, 2, 64, 16).transpose(0, 2, 3, 1, 4).reshape(DEPTH, 128, 16, 16))
    m['s5_cre'] = np.ascontiguousarray(np.asarray(inp['s5_c_re']).reshape(DEPTH, 16, 2, 16, 64).transpose(0, 2, 4, 1, 3).reshape(DEPTH, 128, 16, 16))
    m['s5_cim'] = np.ascontiguousarray(np.asarray(inp['s5_c_im']).reshape(DEPTH, 16, 2, 16, 64).transpose(0, 2, 4, 1, 3).reshape(DEPTH, 128, 16, 16))
    m['s5_d'] = np.ascontiguousarray(np.asarray(inp['s5_d']).reshape(DEPTH, 4, 128).transpose(0, 2, 1))
    m['mla_q_norm'] = np.ascontiguousarray(np.asarray(inp['mla_q_norm']).reshape(DEPTH, 2, 128).transpose(0, 2, 1))
    m['mla_kv_norm'] = np.ascontiguousarray(np.asarray(inp['mla_kv_norm']).reshape(DEPTH, 128, 1))
    return m


_CACHE = {}


def kernel(**inputs):
    if 'nc' not in _CACHE:
        _CACHE['nc'] = Builder().build()
    nc = _CACHE['nc']
    consts = host_constants()
    in_maps = []
    shared = None
    for b in range(8):
        m = host_layout(inputs, b)
        if shared is None:
            shared = {k: v for k, v in m.items() if k not in ('xT', 'pos')}
        else:
            for k in shared:
                m[k] = shared[k]
        m.update(consts)
        in_maps.append(m)
    res = run_bass_kernel_spmd(nc, in_maps, core_ids=list(range(8)))
    out = np.stack([np.ascontiguousarray(res.results[b]['outT'].T) for b in range(8)], axis=0)
    return out.astype(np.float32)
```

```python
import contextlib
import math
import numpy as np
import concourse.bass as bass
import concourse.mybir as mybir
from concourse.bass_utils import run_bass_kernel_spmd

F32 = mybir.dt.float32
BF16 = mybir.dt.bfloat16
I32 = mybir.dt.int32
AF = mybir.ActivationFunctionType
ALU = mybir.AluOpType

D = 1024
L = 4096
NB = 8
TB = 512
DFF = 2816
NFF = 22
INW = 6080
DEPTH = 4
NORM_EPS = 1e-6
GN_EPS = 1e-5
ST = 4
N2 = TB // ST

SAME_ENG_SYNC = True
DMA_SLOTS = {'sp': 24, 'act': 6, 'pool': 20}


class Buf:
    __slots__ = ('name', 'w', 'r')

    def __init__(self, name=''):
        self.name = name
        self.w = []
        self.r = []


class Op:
    __slots__ = ('eng', 'fn', 'deps', 'dma', 'slot', 'gen', 'sig', 'sigval', 'waits', 'idx')


class Prog:
    def __init__(self, nc):
        self.nc = nc
        self.ops = []
        self.emitted = 0
        self.stack = contextlib.ExitStack()
        self.esem = {}
        for e in ('pe', 'act', 'dve', 'pool', 'sp'):
            self.esem[e] = self.stack.enter_context(nc.semaphore('es_' + e))
        self.dsem = {}
        for q, k in DMA_SLOTS.items():
            self.dsem[q] = [self.stack.enter_context(nc.semaphore('ds_%s%d' % (q, i))) for i in range(k)]
        self.dcount = {q: 0 for q in DMA_SLOTS}
        self.dhist = {q: [] for q in DMA_SLOTS}
        self.clocks = {e: {} for e in self.esem}
        self.sigcnt = {e: 0 for e in self.esem}
        self.lastop = {}
        self.sb_off = 0
        self.SB_BYTES = 207 * 1024
        self.big = nc.alloc_sbuf_tensor('big', [128, self.SB_BYTES], mybir.dt.uint8)

    def sb_reset(self, off=0):
        self.sb_off = off

    def sb(self, shape, dtype, name=None, top=False):
        nbytes = int(np.prod(shape[1:])) * mybir.dt.size(dtype)
        if top:
            self.top_off = (getattr(self, 'top_off', 0) + nbytes + 63) // 64 * 64
            off = self.SB_BYTES - self.top_off
            assert off >= self.sb_off, ('SBUF overflow(top)', name)
        else:
            off = (self.sb_off + 63) // 64 * 64
            self.sb_off = off + nbytes
            assert self.sb_off <= self.SB_BYTES - getattr(self, 'top_off', 0), ('SBUF overflow', name, self.sb_off)
        v = self.big[:, off:off + nbytes].bitcast(dtype)
        if len(shape) == 3:
            v = v.rearrange("p (a b) -> p a b", a=shape[1])
        elif len(shape) == 4:
            v = v.rearrange("p (a b c) -> p a b c", a=shape[1], b=shape[2])
        if shape[0] < 128:
            v = v[0:shape[0]]
        return v

    def add(self, eng, fn, reads=(), writes=(), dma=False):
        op = Op()
        op.idx = len(self.ops)
        op.eng = eng
        op.fn = fn
        op.dma = dma
        op.sig = False
        op.sigval = None
        deps = set()
        for b in reads:
            deps.update(b.w)
            b.r.append(op.idx)
        for b in writes:
            deps.update(b.w)
            deps.update(b.r)
            b.w = [op.idx]
            b.r = []
        if dma:
            k = DMA_SLOTS[eng]
            n = self.dcount[eng]
            self.dcount[eng] = n + 1
            op.slot = n % k
            op.gen = n // k
            if n >= k:
                deps.add(self.dhist[eng][n - k])
            self.dhist[eng].append(op.idx)
        elif fn is not None:
            self.lastop[eng] = op.idx
        deps.discard(op.idx)
        op.deps = deps
        self.ops.append(op)
        return op.idx

    def barrier(self):
        deps = set(self.lastop.values())
        for q, k in DMA_SLOTS.items():
            deps.update(self.dhist[q][-k:])
        for e in self.esem:
            i = self.add(e, None)
            self.ops[i].deps.update(deps)

    def flush(self):
        nc = self.nc
        ops = self.ops
        new = ops[self.emitted:]
        for op in new:
            waits = []
            clk = self.clocks[op.eng]
            for d in sorted(op.deps):
                dop = ops[d]
                if dop.dma:
                    key = ('D', dop.eng, dop.slot)
                    val = dop.gen + 1
                else:
                    if dop.eng == op.eng and (op.eng in ('pe', 'sp') or not SAME_ENG_SYNC):
                        continue
                    key = ('E', dop.eng)
                    val = d
                if clk.get(key, -1) >= val:
                    continue
                clk[key] = val
                waits.append(d)
                if not dop.dma and d >= self.emitted:
                    dop.sig = True
            op.waits = waits
        last = {}
        for op in new:
            if not op.dma and op.fn is not None:
                last[op.eng] = op
        for op in last.values():
            op.sig = True
        for op in new:
            if not op.dma and op.sig:
                self.sigcnt[op.eng] += 1
                op.sigval = self.sigcnt[op.eng]
        per = {e: [] for e in self.esem}
        for op in new:
            per[op.eng].append(op)
        self.emitted = len(ops)

        def event(d):
            dop = ops[d]
            if dop.dma:
                return self.dsem[dop.eng][dop.slot], 16 * (dop.gen + 1)
            if dop.sigval is None:
                j = d
                while ops[j].eng != dop.eng or ops[j].dma or ops[j].sigval is None:
                    j += 1
                return self.esem[dop.eng], ops[j].sigval
            return self.esem[dop.eng], dop.sigval

        def runner(engname):
            def f(e):
                for op in per[engname]:
                    for d in op.waits:
                        s, v = event(d)
                        e.wait_ge(s, v)
                    if op.fn is None:
                        continue
                    ins = op.fn(e)
                    if op.dma:
                        ins.then_inc(self.dsem[op.eng][op.slot], 16)
                    elif op.sig:
                        ins.then_inc(self.esem[op.eng], 1)
            return f

        with nc.Block() as block:
            block.tensor(runner('pe'))
            block.scalar(runner('act'))
            block.vector(runner('dve'))
            block.gpsimd(runner('pool'))
            block.sync(runner('sp'))

    def close(self):
        self.stack.close()


class Pool_:
    def __init__(self, P, n, shape, dtype, name):
        self.tiles = [P.sb(shape, dtype, name) for _ in range(n)]
        self.bufs = [Buf('%s%d' % (name, i)) for i in range(n)]
        self.i = 0

    def get(self):
        k = self.i % len(self.tiles)
        self.i += 1
        assert not self.bufs[k].w or self.bufs[k].r, ('pool slot reused before its consumer was recorded', self.bufs[k].name)
        return self.tiles[k], self.bufs[k]


class PsumPool:
    def __init__(self, tiles, names):
        self.tiles = tiles
        self.bufs = [Buf(n) for n in names]
        self.i = 0

    def get(self):
        k = self.i % len(self.tiles)
        self.i += 1
        assert not self.bufs[k].w or self.bufs[k].r, ('psum slot reused before its consumer was recorded', self.bufs[k].name)
        return self.tiles[k], self.bufs[k]


class Builder:
    def __init__(self, n_layers=DEPTH, phases=None, dump=None):
        self.n_layers = n_layers
        self.phases = phases
        self.dump = dump
        nc = self.nc = bass.Bass("TRN2", target_bir_lowering=False)
        self.P = Prog(nc)
        self.inputs = {}
        self.scratch = {}
        self.dbufs = {}

    def dram_in(self, name, shape, dtype=F32):
        t = self.nc.dram_tensor(name, list(shape), dtype, kind="ExternalInput").ap()
        self.inputs[name] = t
        return t

    def dram_scratch(self, name, shape, dtype):
        t = self.nc.dram_tensor(name, list(shape), dtype).ap()
        self.scratch[name] = t
        return t

    def xb(self, name, b):
        return [self.dbuf(name, b, c) for c in range(8)]

    def dbuf(self, name, *idx):
        key = (name,) + idx
        b = self.dbufs.get(key)
        if b is None:
            b = self.dbufs[key] = Buf(str(key))
        return b

    def mm(self, out_ap, pairs, reads, writes):
        def fn(e):
            n = len(pairs)
            ins = None
            for i, (l, r) in enumerate(pairs):
                ins = e.matmul(out_ap, lhsT=l, rhs=r, start=(i == 0), stop=(i == n - 1))
            return ins
        self.P.add('pe', fn, reads=reads, writes=writes)

    def dma(self, q, out_ap, in_ap, reads, writes):
        self.P.add(q, lambda e: e.dma_start(out=out_ap, in_=in_ap), reads=reads, writes=writes, dma=True)

    def act(self, out_ap, in_ap, func, reads, writes, scale=1.0, bias=None):
        if bias is None:
            self.P.add('act', lambda e: e.activation(out=out_ap, in_=in_ap, func=func, scale=scale), reads=reads, writes=writes)
        else:
            self.P.add('act', lambda e: e.activation(out=out_ap, in_=in_ap, func=func, scale=scale, bias=bias), reads=reads, writes=writes)

    def tt(self, eng, out_ap, a, b, op, reads, writes):
        self.P.add(eng, lambda e: e.tensor_tensor(out=out_ap, in0=a, in1=b, op=op), reads=reads, writes=writes)

    def ts(self, eng, out_ap, a, s1, s2, op0, op1, reads, writes):
        if op1 is None:
            self.P.add(eng, lambda e: e.tensor_scalar(out=out_ap, in0=a, scalar1=s1, scalar2=None, op0=op0), reads=reads, writes=writes)
        else:
            self.P.add(eng, lambda e: e.tensor_scalar(out=out_ap, in0=a, scalar1=s1, scalar2=s2, op0=op0, op1=op1), reads=reads, writes=writes)

    def stt(self, out_ap, a, s, b, op0, op1, reads, writes):
        self.P.add('dve', lambda e: e.scalar_tensor_tensor(out=out_ap, in0=a, scalar=s, in1=b, op0=op0, op1=op1), reads=reads, writes=writes)

    def copy(self, eng, out_ap, in_ap, reads, writes):
        if eng == 'act':
            self.P.add('act', lambda e: e.activation(out=out_ap, in_=in_ap, func=AF.Copy), reads=reads, writes=writes)
        else:
            self.P.add(eng, lambda e: e.tensor_copy(out=out_ap, in_=in_ap), reads=reads, writes=writes)

    def memset(self, eng, ap, val, writes):
        self.P.add(eng, lambda e: e.memset(ap, val), writes=writes)

    IN_SHAPES = {
        'xT': ([D, L], F32), 'pos': ([1, L], I32), 'gains': ([128, DEPTH * 48], F32),
        'ffn_w_gate': ([DEPTH, 2, D, DFF], F32), 'ffn_w_up': ([DEPTH, 2, D, DFF], F32), 'ffn_w_down': ([DEPTH, 2, DFF, D], F32),
        'w_in': ([DEPTH, D, INW], F32), 'ret_w_o': ([DEPTH, 512, D], F32),
        's5_are': ([DEPTH, 128, 16], F32), 's5_aim': ([DEPTH, 128, 16], F32), 's5_ldt': ([DEPTH, 128, 16], F32),
        's5_bre': ([DEPTH, 128, 16, 16], F32), 's5_bim': ([DEPTH, 128, 16, 16], F32),
        's5_cre': ([DEPTH, 128, 16, 16], F32), 's5_cim': ([DEPTH, 128, 16, 16], F32), 's5_d': ([DEPTH, 128, 4], F32),
        's5_glu_a': ([DEPTH, 512, D], F32), 's5_glu_b': ([DEPTH, 512, D], F32),
        'mla_q_norm': ([DEPTH, 128, 2], F32), 'mla_kv_norm': ([DEPTH, 128, 1], F32),
        'mla_w_uq': ([DEPTH, 256, 768], F32), 'mla_w_ukv': ([DEPTH, 128, 1024], F32), 'mla_w_o': ([DEPTH, 512, D], F32),
        'w_out': ([DEPTH, D, D], F32),
        'c_ident': ([128, 128], F32), 'c_tri': ([128, 128], F32), 'c_dec': ([128, 4, 128], F32), 'c_qdec': ([128, 4, 128], F32),
        'c_kdec': ([128, 4], F32), 'c_freq': ([128, 2], F32), 'c_sgn': ([128, 2], F32), 'c_step': ([128, N2], F32),
    }

    def I(self, name):
        t = self.inputs.get(name)
        if t is None:
            shape, dt = self.IN_SHAPES[name]
            shape = list(shape)
            if shape[0] == DEPTH and len(shape) >= 3:
                shape[0] = self.n_layers
            t = self.dram_in(name, shape, dt)
        return t

    def declare(self):
        self.out = self.nc.dram_tensor('outT', [D, L], F32, kind="ExternalOutput").ap()
        ds = self.dram_scratch
        self.X = ds('X', [D, L], F32)
        self.CR = ds('CR', [128, L], F32)
        self.SR = ds('SR', [128, L], F32)
        self.CM = ds('CM', [64, L], F32)
        self.SM = ds('SM', [64, L], F32)
        self.QR = ds('QR', [512, L], BF16)
        self.KR = ds('KR', [512, L], BF16)
        self.VR = ds('VR', [L, 512], BF16)
        self.SG = ds('SG', [512, L], BF16)
        self.U = ds('U', [512, L], BF16)
        self.QN = ds('QN', [512, L], BF16)
        self.QP = ds('QP', [256, L], BF16)
        self.KN = ds('KN', [512, L], BF16)
        self.KP = ds('KP', [64, L], BF16)
        self.VM = ds('VM', [L, 512], BF16)
        self.GT = ds('GT', [3072, L], BF16)
        self.MR = ds('MR', [D, L], BF16)
        self.MS = ds('MS', [D, L], BF16)
        if self.dump:
            self.dbg = self.nc.dram_tensor('dbg', list(self.dump[1]), self.dump[2], kind="ExternalOutput").ap()

    def consts(self):
        P = self.P
        P.sb_reset(0)
        self.ones_bf = P.sb([128, 128], BF16, 'ones')
        self.b_ones = Buf('ones')
        self.G = P.sb([128, DEPTH * 48], F32, 'G')
        self.GH = P.sb([128, DEPTH * 48], F32, 'GH')
        self.b_G = Buf('G')
        self.ident = P.sb([128, 128], F32, 'ident')
        self.b_ident = Buf('ident')
        self.memset('dve', self.ones_bf, 1.0, [self.b_ones])
        self.dma('sp', self.G, self.I('gains'), [], [self.b_G])
        self.dma('sp', self.ident, self.I('c_ident'), [], [self.b_ident])
        self.ts('dve', self.GH, self.G, 0.5, None, ALU.mult, None, [self.b_G], [self.b_G])
        self.const_end = P.sb_off
        self.psum = [self.nc.alloc_psum_tensor('ps%d' % i, [128, 512], F32)[:, :] for i in range(8)]

    def gcol(self, l, n, c):
        k = (l * 6 + n) * 8 + c
        return self.G[:, k:k + 1]

    def ghcol(self, l, n, c):
        k = (l * 6 + n) * 8 + c
        return self.GH[:, k:k + 1]

    def load_weight_rows(self, dst, src, nrows_chunks, ncols, bufs):
        maxc = 2048
        nsplit = (ncols + maxc - 1) // maxc
        w = (ncols + nsplit - 1) // nsplit
        for c in range(nrows_chunks):
            for s in range(nsplit):
                c0 = s * w
                c1 = min(ncols, c0 + w)
                self.dma('pool', dst[:, c, c0:c1], src[c * 128:(c + 1) * 128, c0:c1], [], [bufs[c][s]])

    def wbufs(self, n, ncols):
        nsplit = (ncols + 2047) // 2048
        return [[Buf() for _ in range(nsplit)] for _ in range(n)]

    @staticmethod
    def flat(bl):
        return [b for row in bl for b in row]

    def rms_stats(self, chunks, bxs, width, ps_stat, b_ps, sqpool, inv_n, eps_ap, rstd, b_rstd, tmp, b_tmp, b_eps):
        n = len(chunks)
        for c in range(n):
            sq, bsq = sqpool.get()
            self.act(sq[:, 0:width], chunks[c], AF.Square, [bxs[c]], [bsq])
            self.P.add('pe', (lambda sq, c: lambda e: e.matmul(ps_stat[:, 0:width], lhsT=self.ones_bf, rhs=sq[:, 0:width], start=(c == 0), stop=(c == n - 1)))(sq, c),
                       reads=[bsq, self.b_ones], writes=[b_ps])
        self.act(tmp[:, 0:width], ps_stat[:, 0:width], AF.Sqrt, [b_ps, b_eps], [b_tmp], scale=inv_n, bias=eps_ap)
        self.P.add('dve', lambda e: e.reciprocal(out=rstd[:, 0:width], in_=tmp[:, 0:width]), reads=[b_tmp], writes=[b_rstd])

    def ffn_phase(self, l, j, src, srcname, dst, dstname):
        P = self.P
        P.barrier()
        P.sb_reset(self.const_end)
        FT = 256
        NBF = L // FT
        n_pre = 0 if j == 0 else 4
        n_post = 1 if j == 0 else 5
        Wg = P.sb([128, 8, DFF], BF16, 'Wg')
        Wu = P.sb([128, 8, DFF], BF16, 'Wu')
        Wd = P.sb([128, NFF, D], BF16, 'Wd')
        bWg = self.wbufs(8, DFF)
        bWu = self.wbufs(8, DFF)
        bWd = self.wbufs(NFF, D)
        xts = [P.sb([128, 8, FT], F32, 'x') for _ in range(2)]
        bxs = [Buf('x0'), Buf('x1')]
        hs = [P.sb([128, 8, FT], BF16, 'h') for _ in range(2)]
        bhs = [[Buf('h%d_%d' % (i, c)) for c in range(8)] for i in range(2)]
        actb = P.sb([128, NFF, FT], BF16, 'act')
        bact = [Buf('act%d' % f) for f in range(NFF)]
        y = P.sb([128, 8, FT], F32, 'y')
        by = [Buf('y%d' % c) for c in range(8)]
        sqpool = Pool_(P, 4, [128, FT], BF16, 'sq')
        silp = Pool_(P, 4, [128, FT], BF16, 'sil')
        rstd = P.sb([128, FT], F32, 'rstd')
        b_rstd = Buf('rstd')
        rstd2 = P.sb([128, FT], F32, 'rstd2')
        b_rstd2 = Buf('rstd2')
        tmp = P.sb([128, FT], F32, 'tmp')
        b_tmp = Buf('tmp')
        eps = P.sb([128, 1], F32, 'eps')
        b_eps = Buf('eps')
        self.memset('dve', eps, NORM_EPS, [b_eps])
        ps = self.psum
        pg = PsumPool(ps[0:2], ['pg0', 'pg1'])
        pu = PsumPool(ps[2:4], ['pu0', 'pu1'])
        pd = PsumPool(ps[4:6], ['pd0', 'pd1'])
        ps_st, b_st = ps[6], Buf('pst')
        ps_yst, b_yst = ps[7], Buf('pyst')

        self.load_weight_rows(Wg, self.I('ffn_w_gate')[l, j], 8, DFF, bWg)
        self.load_weight_rows(Wu, self.I('ffn_w_up')[l, j], 8, DFF, bWu)
        self.load_weight_rows(Wd, self.I('ffn_w_down')[l, j], NFF, D, bWd)
        rWg, rWu, rWd = self.flat(bWg), self.flat(bWu), self.flat(bWd)

        srcv = src.rearrange("(c p) t -> p c t", p=128)
        dstv = dst.rearrange("(c p) t -> p c t", p=128)

        def stage_a(b):
            xt, bx, h, bh = xts[b % 2], bxs[b % 2], hs[b % 2], bhs[b % 2]
            self.dma('sp', xt, srcv[:, :, b * FT:(b + 1) * FT], self.xb(srcname, b * FT // TB), [bx])
            self.rms_stats([xt[:, c, :] for c in range(8)], [bx] * 8, FT, ps_st, b_st, sqpool, 1.0 / D, eps[:, 0:1], rstd, b_rstd, tmp, b_tmp, b_eps)
            for c in range(8):
                self.stt(h[:, c, :], xt[:, c, :], self.gcol(l, n_pre, c), rstd, ALU.mult, ALU.mult, [bx, b_rstd, self.b_G], [bh[c]])

        def stage_b(b):
            h, bh = hs[b % 2], bhs[b % 2]
            for f in range(NFF):
                g_ps, bg = pg.get()
                u_ps, bu = pu.get()
                self.mm(g_ps[:, 0:FT], [(Wg[:, k, f * 128:(f + 1) * 128], h[:, k, :]) for k in range(8)], bh + rWg, [bg])
                self.mm(u_ps[:, 0:FT], [(Wu[:, k, f * 128:(f + 1) * 128], h[:, k, :]) for k in range(8)], bh + rWu, [bu])
                sl, bsl = silp.get()
                self.act(sl, g_ps[:, 0:FT], AF.Silu, [bg], [bsl])
                self.tt('dve', actb[:, f, :], sl, u_ps[:, 0:FT], ALU.mult, [bsl, bu], [bact[f]])

        def stage_c(b):
            pend = []
            for c in range(8):
                d_ps, bd = pd.get()
                self.mm(d_ps[:, 0:FT], [(Wd[:, f, c * 128:(c + 1) * 128], actb[:, f, :]) for f in range(NFF)], bact + rWd, [bd])
                self.copy('dve', y[:, c, :], d_ps[:, 0:FT], [bd], [by[c]])
                sq, bsq = sqpool.get()
                self.act(sq, y[:, c, :], AF.Square, [by[c]], [bsq])
                pend.append((sq, bsq, c))
                if len(pend) > 2:
                    sq_, bsq_, c_ = pend.pop(0)
                    self.P.add('pe', (lambda sq, c: lambda e: e.matmul(ps_yst[:, 0:FT], lhsT=self.ones_bf, rhs=sq, start=(c == 0), stop=(c == 7)))(sq_, c_),
                               reads=[bsq_, self.b_ones], writes=[b_yst])
            for sq_, bsq_, c_ in pend:
                self.P.add('pe', (lambda sq, c: lambda e: e.matmul(ps_yst[:, 0:FT], lhsT=self.ones_bf, rhs=sq, start=(c == 0), stop=(c == 7)))(sq_, c_),
                           reads=[bsq_, self.b_ones], writes=[b_yst])

        def stage_d(b):
            xt, bx = xts[b % 2], bxs[b % 2]
            self.act(tmp, ps_yst[:, 0:FT], AF.Sqrt, [b_yst, b_eps], [b_tmp], scale=1.0 / D, bias=eps[:, 0:1])
            self.P.add('dve', lambda e: e.reciprocal(out=rstd2, in_=tmp), reads=[b_tmp], writes=[b_rstd2])
            for c in range(8):
                self.tt('dve', y[:, c, :], y[:, c, :], rstd2, ALU.mult, [by[c], b_rstd2], [by[c]])
                self.stt(xt[:, c, :], y[:, c, :], self.ghcol(l, n_post, c), xt[:, c, :], ALU.mult, ALU.add, [by[c], bx, self.b_G], [bx])
            self.dma('sp', dstv[:, :, b * FT:(b + 1) * FT], xt, [bx], self.xb(dstname, b * FT // TB))

        import os
        dbg_st = os.environ.get('FFN_STAGES', 'abcd')
        dbg_nb = int(os.environ.get('FFN_NB', NBF))
        stage_a(0)
        for b in range(dbg_nb):
            if 'b' in dbg_st:
                stage_b(b)
            if b + 1 < dbg_nb:
                stage_a(b + 1)
            if 'c' in dbg_st:
                stage_c(b)
            if 'd' in dbg_st:
                stage_d(b)

    def rope_tables(self):
        P = self.P
        P.barrier()
        P.sb_reset(self.const_end)
        posi = P.sb([128, L], I32, 'posi')
        posf = P.sb([128, L], F32, 'posf')
        ang = P.sb([128, L], F32, 'ang')
        kk = P.sb([128, L], F32, 'kk')
        res = P.sb([128, L], F32, 'res')
        fr = P.sb([128, 2], F32, 'fr')
        sg = P.sb([128, 2], F32, 'sg')
        b_pos, b_ang, b_kk, b_res, b_c = Buf(), Buf(), Buf(), Buf(), Buf()
        self.dma('sp', posi, self.I('pos').partition_broadcast(128), [], [b_pos])
        self.dma('sp', fr, self.I('c_freq'), [], [b_c])
        self.dma('sp', sg, self.I('c_sgn'), [], [b_c])
        self.copy('dve', posf, posi, [b_pos], [b_pos])
        MAG = 12582912.0
        C1 = 6.28125
        C2 = 2.0 * math.pi - 6.28125
        for col, rows, dc, dsn in ((0, 128, self.CR, self.SR), (1, 64, self.CM, self.SM)):
            for is_cos in (False, True):
                a = ang[0:rows]
                k = kk[0:rows]
                r = res[0:rows]
                self.ts('dve', a, posf[0:rows], fr[0:rows, col:col + 1], (math.pi / 2 if is_cos else 0.0), ALU.mult, ALU.add, [b_pos, b_c], [b_ang])
                self.ts('dve', k, a, 1.0 / (2.0 * math.pi), MAG, ALU.mult, ALU.add, [b_ang], [b_kk])
                self.ts('dve', k, k, -MAG, None, ALU.add, None, [b_kk], [b_kk])
                self.stt(a, k, -C1, a, ALU.mult, ALU.add, [b_kk, b_ang], [b_ang])
                self.stt(a, k, -C2, a, ALU.mult, ALU.add, [b_kk, b_ang], [b_ang])
                self.ts('dve', a, a, 3.141592, -3.141592, ALU.min, ALU.max, [b_ang], [b_ang])
                self.act(r, a, AF.Sin, [b_ang], [b_res])
                if not is_cos:
                    self.ts('dve', r, r, sg[0:rows, col:col + 1], None, ALU.mult, None, [b_res, b_c], [b_res])
                self.dma('sp', dc if is_cos else dsn, r, [b_res], [self.dbuf('ropetab')])

    def rope_evac(self, ps, rows, Ct, St, b_tab, bps, scale, out_ap, b_out, t1p, t2p):
        hf = rows // 2
        t1, bt1 = t1p.get()
        t2, bt2 = t2p.get()
        self.stt(t1[0:rows], ps[0:rows], scale, Ct[0:rows], ALU.mult, ALU.mult, [bps, b_tab], [bt1])
        self.stt(t2[0:hf], ps[hf:rows], scale, St[0:hf], ALU.mult, ALU.mult, [bps, b_tab], [bt2])
        self.stt(t2[hf:rows], ps[0:hf], scale, St[hf:rows], ALU.mult, ALU.mult, [bps, b_tab, bt2], [bt2])
        self.tt('pool', out_ap, t1[0:rows], t2[0:rows], ALU.add, [bt1, bt2], [b_out])

    def b1_phase(self, l):
        P = self.P
        P.barrier()
        P.sb_reset(self.const_end)
        Win = P.sb([128, 8, INW], BF16, 'Win')
        Wuq = P.sb([128, 2, 768], BF16, 'Wuq')
        Wukv = P.sb([128, 1, 1024], BF16, 'Wukv')
        bWin = self.wbufs(8, INW)
        bWuq = self.wbufs(2, 768)
        bWukv = self.wbufs(1, 1024)
        gq = P.sb([128, 2], F32, 'gq')
        gkv = P.sb([128, 1], F32, 'gkv')
        eps = P.sb([128, 1], F32, 'eps')
        b_small = Buf('small')
        self.memset('dve', eps, NORM_EPS, [b_small])
        self.dma('sp', gq, self.I('mla_q_norm')[l], [], [b_small])
        self.dma('sp', gkv, self.I('mla_kv_norm')[l], [], [b_small])
        xt = P.sb([128, 8, TB], F32, 'x')
        bx = Buf('x')
        h = P.sb([128, 8, TB], BF16, 'h')
        bh = [Buf('h%d' % c) for c in range(8)]
        CRt = P.sb([128, TB], F32, 'CRt')
        SRt = P.sb([128, TB], F32, 'SRt')
        CMt = P.sb([64, TB], F32, 'CMt')
        SMt = P.sb([64, TB], F32, 'SMt')
        b_tab = Buf('tab')
        stq = P.sb([128, 4, TB], BF16, 'stq'); b_stq = [Buf() for _ in range(4)]
        stk = P.sb([128, 4, TB], BF16, 'stk'); b_stk = [Buf() for _ in range(4)]
        stsg = P.sb([128, 4, TB], BF16, 'stsg'); b_stsg = [Buf() for _ in range(4)]
        stu = P.sb([128, 4, TB], BF16, 'stu'); b_stu = [Buf() for _ in range(4)]
        stv = P.sb([128, 4, 512], BF16, 'stv'); b_stv = [Buf() for _ in range(4)]
        stqn = P.sb([128, 4, TB], BF16, 'stqn'); b_stqn = [Buf() for _ in range(4)]
        stqp = P.sb([64, 4, TB], BF16, 'stqp'); b_stqp = [Buf() for _ in range(4)]
        stkn = P.sb([128, 4, TB], BF16, 'stkn'); b_stkn = [Buf() for _ in range(4)]
        stvm = P.sb([128, 4, 512], BF16, 'stvm'); b_stvm = [Buf() for _ in range(4)]
        stkp = P.sb([64, TB], BF16, 'stkp'); b_stkp = Buf()
        gtp = Pool_(P, 4, [128, TB], BF16, 'gt')
        cq = P.sb([128, 2, TB], F32, 'cq'); b_cq = [Buf(), Buf()]
        cqn = P.sb([128, 2, TB], BF16, 'cqn'); b_cqn = [Buf(), Buf()]
        ckv = P.sb([128, TB], F32, 'ckv'); b_ckv = Buf()
        ckvn = P.sb([128, TB], BF16, 'ckvn'); b_ckvn = Buf()
        t1p = Pool_(P, 2, [128, TB], F32, 't1')
        t2p = Pool_(P, 2, [128, TB], F32, 't2')
        sqpool = Pool_(P, 4, [128, TB], BF16, 'sq')
        rstd = P.sb([128, TB], F32, 'rstd'); b_rstd = Buf()
        tmp = P.sb([128, TB], F32, 'tmp'); b_tmp = Buf()
        ps = self.psum
        pp = PsumPool(ps[0:6], ['pp%d' % i for i in range(6)])
        ps_st, b_st = ps[6], Buf('pst')
        ps_st2, b_st2 = ps[7], Buf('pst2')

        self.load_weight_rows(Win, self.I('w_in')[l], 8, INW, bWin)
        self.load_weight_rows(Wuq, self.I('mla_w_uq')[l], 2, 768, bWuq)
        self.load_weight_rows(Wukv, self.I('mla_w_ukv')[l], 1, 1024, bWukv)
        rWin, rWuq, rWukv = self.flat(bWin), self.flat(bWuq), self.flat(bWukv)
        Xv = self.X.rearrange("(c p) t -> p c t", p=128)
        RSC = 128 ** -0.5
        MSC = 192 ** -0.5

        def proj(col0, ncols):
            pt, bp = pp.get()
            self.mm(pt[0:ncols, :], [(Win[:, k, col0:col0 + ncols], h[:, k, :]) for k in range(8)], bh + rWin, [bp])
            return pt, bp

        for b in range(NB):
            sl = slice(b * TB, (b + 1) * TB)
            self.dma('sp', xt, Xv[:, :, sl], self.xb('X', b), [bx])
            self.dma('sp', CRt, self.CR[:, sl], [self.dbuf('ropetab')], [b_tab])
            self.dma('sp', SRt, self.SR[:, sl], [self.dbuf('ropetab')], [b_tab])
            self.dma('sp', CMt, self.CM[:, sl], [self.dbuf('ropetab')], [b_tab])
            self.dma('sp', SMt, self.SM[:, sl], [self.dbuf('ropetab')], [b_tab])
            self.rms_stats([xt[:, c, :] for c in range(8)], [bx] * 8, TB, ps_st, b_st, sqpool, 1.0 / D, eps[:, 0:1], rstd, b_rstd, tmp, b_tmp, b_small)
            for c in range(8):
                self.stt(h[:, c, :], xt[:, c, :], self.gcol(l, 2, c), rstd, ALU.mult, ALU.mult, [bx, b_rstd, self.b_G], [bh[c]])
            for hh in range(4):
                pt, bp = proj(hh * 128, 128)
                self.rope_evac(pt, 128, CRt, SRt, b_tab, bp, 1.0, stq[:, hh, :], b_stq[hh], t1p, t2p)
                pt, bp = proj(512 + hh * 128, 128)
                self.rope_evac(pt, 128, CRt, SRt, b_tab, bp, RSC, stk[:, hh, :], b_stk[hh], t1p, t2p)
            self.dma('sp', self.QR.rearrange("(h p) t -> p h t", p=128)[:, :, sl], stq, b_stq, [self.dbuf('QR', b)])
            self.dma('sp', self.KR.rearrange("(h p) t -> p h t", p=128)[:, :, sl], stk, b_stk, [self.dbuf('KR', b)])
            for tt_ in range(4):
                pt, bp = pp.get()
                self.mm(pt, [(h[:, k, tt_ * 128:(tt_ + 1) * 128], Win[:, k, 1024:1536]) for k in range(8)], bh + rWin, [bp])
                self.copy('act', stv[:, tt_, :], pt, [bp], [b_stv[tt_]])
            self.dma('sp', self.VR[sl, :].rearrange("(t p) c -> p t c", p=128), stv, b_stv, [self.dbuf('VR', b)])
            for hh in range(4):
                pt, bp = proj(1536 + hh * 128, 128)
                self.act(stsg[:, hh, :], pt, AF.Silu, [bp], [b_stsg[hh]])
            self.dma('sp', self.SG.rearrange("(h p) t -> p h t", p=128)[:, :, sl], stsg, b_stsg, [self.dbuf('SG', b)])
            for hh in range(4):
                pt, bp = proj(2048 + hh * 128, 128)
                self.copy('act', stu[:, hh, :], pt, [bp], [b_stu[hh]])
            self.dma('sp', self.U.rearrange("(h p) t -> p h t", p=128)[:, :, sl], stu, b_stu, [self.dbuf('U', b)])
            for c2 in range(2):
                pt, bp = proj(2560 + c2 * 128, 128)
                self.copy('dve', cq[:, c2, :], pt, [bp], [b_cq[c2]])
            self.rms_stats([cq[:, 0, :], cq[:, 1, :]], b_cq, TB, ps_st2, b_st2, sqpool, 1.0 / 256, eps[:, 0:1], rstd, b_rstd, tmp, b_tmp, b_small)
            for c2 in range(2):
                self.stt(cqn[:, c2, :], cq[:, c2, :], gq[:, c2:c2 + 1], rstd, ALU.mult, ALU.mult, [b_cq[c2], b_rstd, b_small], [b_cqn[c2]])
            for hh in range(4):
                pt, bp = pp.get()
                self.mm(pt, [(Wuq[:, k2, hh * 192:hh * 192 + 128], cqn[:, k2, :]) for k2 in range(2)], b_cqn + rWuq, [bp])
                self.act(stqn[:, hh, :], pt, AF.Copy, [bp], [b_stqn[hh]], scale=MSC)
                pt, bp = pp.get()
                self.mm(pt[0:64, :], [(Wuq[:, k2, hh * 192 + 128:hh * 192 + 192], cqn[:, k2, :]) for k2 in range(2)], b_cqn + rWuq, [bp])
                self.rope_evac(pt, 64, CMt, SMt, b_tab, bp, MSC, stqp[:, hh, :], b_stqp[hh], t1p, t2p)
            self.dma('sp', self.QN.rearrange("(h p) t -> p h t", p=128)[:, :, sl], stqn, b_stqn, [self.dbuf('QN', b)])
            self.dma('sp', self.QP.rearrange("(h p) t -> p h t", p=64)[:, :, sl], stqp, b_stqp, [self.dbuf('QP', b)])
            pt, bp = proj(2816, 128)
            self.copy('dve', ckv, pt, [bp], [b_ckv])
            self.rms_stats([ckv], [b_ckv], TB, ps_st2, b_st2, sqpool, 1.0 / 128, eps[:, 0:1], rstd, b_rstd, tmp, b_tmp, b_small)
            self.stt(ckvn, ckv, gkv[:, 0:1], rstd, ALU.mult, ALU.mult, [b_ckv, b_rstd, b_small], [b_ckvn])
            for hh in range(4):
                pt, bp = pp.get()
                self.mm(pt, [(Wukv[:, 0, hh * 256:hh * 256 + 128], ckvn)], [b_ckvn] + rWukv, [bp])
                self.copy('act', stkn[:, hh, :], pt, [bp], [b_stkn[hh]])
            self.dma('sp', self.KN.rearrange("(h p) t -> p h t", p=128)[:, :, sl], stkn, b_stkn, [self.dbuf('KN', b)])
            wv = Wukv[:, 0, :].rearrange("p (h c) -> p h c", h=4)[:, :, 128:256]
            for tt_ in range(4):
                pt, bp = pp.get()
                self.mm(pt.rearrange("p (h c) -> p h c", h=4), [(ckvn[:, tt_ * 128:(tt_ + 1) * 128], wv)], [b_ckvn] + rWukv, [bp])
                self.copy('act', stvm[:, tt_, :], pt, [bp], [b_stvm[tt_]])
            self.dma('sp', self.VM[sl, :].rearrange("(t p) c -> p t c", p=128), stvm, b_stvm, [self.dbuf('VM', b)])
            pt, bp = proj(2944, 64)
            self.rope_evac(pt, 64, CMt, SMt, b_tab, bp, 1.0, stkp, b_stkp, t1p, t2p)
            self.dma('sp', self.KP[:, sl], stkp, [b_stkp], [self.dbuf('KP', b)])
            for jg in range(24):
                pt, bp = proj(3008 + jg * 128, 128)
                gt_, bgt = gtp.get()
                self.act(gt_, pt, AF.Sigmoid, [bp], [bgt])
                self.dma('sp', self.GT[jg * 128:(jg + 1) * 128, sl], gt_, [bgt], [self.dbuf('GT', b, jg)])

    def r_phase(self, l):
        P = self.P
        P.barrier()
        P.sb_reset(self.const_end)
        Wo = P.sb([128, 4, D], BF16, 'Wo')
        bWo = self.wbufs(4, D)
        self.load_weight_rows(Wo, self.I('ret_w_o')[l], 4, D, bWo)
        rWo = self.flat(bWo)
        DEC = P.sb([128, 4, 128], F32, 'DEC')
        QDEC = P.sb([128, 4, 128], F32, 'QDEC')
        KDEC = P.sb([128, 4], F32, 'KDEC')
        identb = P.sb([128, 128], BF16, 'identb')
        onesf = P.sb([128, 128], F32, 'onesf')
        epsg = P.sb([128, 1], F32, 'epsg')
        b_c = Buf('rc')
        self.dma('sp', DEC, self.I('c_dec'), [], [b_c])
        self.dma('sp', QDEC, self.I('c_qdec'), [], [b_c])
        self.dma('sp', KDEC, self.I('c_kdec'), [], [b_c])
        self.copy('dve', identb, self.ident, [self.b_ident], [b_c])
        self.memset('dve', onesf, 1.0 / 128, [b_c])
        self.memset('dve', epsg, GN_EPS, [b_c])
        S = P.sb([128, 4, 128], F32, 'S')
        Sb = P.sb([128, 4, 128], BF16, 'Sb')
        bS = [Buf('S%d' % i) for i in range(4)]
        bSb = [Buf('Sb%d' % i) for i in range(4)]
        for hh in range(4):
            self.memset('dve', S[:, hh, :], 0.0, [bS[hh]])
            self.memset('pool', Sb[:, hh, :], 0.0, [bSb[hh]])
        qt = P.sb([128, 4, TB], BF16, 'qt'); b_qt = Buf()
        kt = P.sb([128, 4, TB], BF16, 'kt'); b_kt = Buf()
        vt = P.sb([128, 4, 512], BF16, 'vt'); b_vt = Buf()
        sgt = P.sb([128, 4, TB], BF16, 'sgt'); b_sgt = Buf()
        GO = P.sb([128, 4, TB], BF16, 'GO'); bGO = [Buf() for _ in range(4)]
        ptp = Pool_(P, 8, [128, 128], BF16, 'PT')
        kdp = Pool_(P, 8, [128, 128], BF16, 'kd')
        qdp = Pool_(P, 8, [128, 128], BF16, 'qd')
        osb = [P.sb([128, TB], F32, 'osb') for _ in range(4)]; b_osb = [Buf() for _ in range(4)]
        osq = [P.sb([128, TB], F32, 'osq') for _ in range(4)]; b_osq = [Buf() for _ in range(4)]
        mean = [P.sb([128, TB], F32, 'mean') for _ in range(4)]; b_mean = [Buf() for _ in range(4)]
        var = [P.sb([128, TB], F32, 'var') for _ in range(4)]; b_var = [Buf() for _ in range(4)]
        rstd = [P.sb([128, TB], F32, 'rstd') for _ in range(4)]; b_rstd = [Buf() for _ in range(4)]
        gp = Pool_(P, 3, [128, TB], BF16, 'g0')
        mp = Pool_(P, 3, [128, TB], BF16, 'mr')
        ps = self.psum
        psc = PsumPool(ps[0:1], ['sc0'])
        pkt = PsumPool(ps[1:2], ['kt0'])
        psn = PsumPool(ps[2:3], ['sn'])
        pOs = [(ps[3 + i], Buf('O%d' % i)) for i in range(4)]
        pst = PsumPool([ps[7], ps[0], ps[1], ps[2]], ['st7', 'sc0', 'kt0', 'sn'])
        pst.bufs = [Buf('st7'), psc.bufs[0], pkt.bufs[0], psn.bufs[0]]
        G128 = [float(np.exp(128.0 * np.log1p(-np.exp2(-5.0 - hh)))) for hh in range(4)]

        for b in range(NB):
            sl = slice(b * TB, (b + 1) * TB)
            self.dma('sp', qt, self.QR.rearrange("(h p) t -> p h t", p=128)[:, :, sl], [self.dbuf('QR', b)], [b_qt])
            self.dma('sp', kt, self.KR.rearrange("(h p) t -> p h t", p=128)[:, :, sl], [self.dbuf('KR', b)], [b_kt])
            self.dma('sp', vt, self.VR[sl, :].rearrange("(t p) c -> p t c", p=128), [self.dbuf('VR', b)], [b_vt])
            self.dma('sp', sgt, self.SG.rearrange("(h p) t -> p h t", p=128)[:, :, sl], [self.dbuf('SG', b)], [b_sgt])
            def front(n, hh):
                cs = slice(n * 128, (n + 1) * 128)
                sc, bsc = psc.get()
                self.mm(sc[:, 0:128], [(kt[:, hh, cs], qt[:, hh, cs])], [b_kt, b_qt], [bsc])
                PT, bPT = ptp.get()
                self.tt('dve', PT, sc[:, 0:128], DEC[:, hh, :], ALU.mult, [bsc, b_c], [bPT])
                ktp, bktp = pkt.get()
                self.mm(ktp[:, 0:128], [(kt[:, hh, cs], identb)], [b_kt, b_c], [bktp])
                kd, bkd = kdp.get()
                self.act(kd, ktp[:, 0:128], AF.Copy, [bktp, b_c], [bkd], scale=KDEC[:, hh:hh + 1])
                qd, bqd = qdp.get()
                self.tt('pool', qd, qt[:, hh, cs], QDEC[:, hh, :], ALU.mult, [b_qt, b_c], [bqd])
                return (n, hh, PT, bPT, kd, bkd, qd, bqd)

            def back(fr):
                n, hh, PT, bPT, kd, bkd, qd, bqd = fr
                cs = slice(n * 128, (n + 1) * 128)
                O_ps, bO = pOs[hh]
                vs = vt[:, n, hh * 128:(hh + 1) * 128]
                self.mm(O_ps[:, cs], [(vs, PT), (Sb[:, hh, :], qd)], [b_vt, bPT, bSb[hh], bqd], [bO])
                sn, bsn = psn.get()
                self.mm(sn[:, 0:128], [(kd, vs)], [bkd, b_vt], [bsn])
                self.stt(S[:, hh, :], S[:, hh, :], G128[hh], sn[:, 0:128], ALU.mult, ALU.add, [bS[hh], bsn], [bS[hh]])
                self.copy('pool', Sb[:, hh, :], S[:, hh, :], [bS[hh]], [bSb[hh]])

            seq = [(n, hh) for n in range(4) for hh in range(4)]
            frs = [front(*seq[0]), front(*seq[1])]
            for i in range(len(seq)):
                if i + 2 < len(seq):
                    frs.append(front(*seq[i + 2]))
                back(frs.pop(0))
            H4 = range(4)
            for hh in H4:
                self.copy('act', osb[hh], pOs[hh][0], [pOs[hh][1]], [b_osb[hh]])
            for hh in H4:
                self.act(osq[hh], osb[hh], AF.Square, [b_osb[hh]], [b_osq[hh]])
            mpss, qpss = [], []
            for hh in H4:
                mps, bmps = pst.get()
                self.mm(mps, [(onesf, osb[hh])], [b_c, b_osb[hh]], [bmps])
                self.copy('act', mean[hh], mps, [bmps], [b_mean[hh]])
            for hh in H4:
                qps, bqps = pst.get()
                self.mm(qps, [(onesf, osq[hh])], [b_c, b_osq[hh]], [bqps])
                self.copy('act', var[hh], qps, [bqps], [b_var[hh]])
            for hh in H4:
                self.act(osq[hh], mean[hh], AF.Square, [b_mean[hh]], [b_osq[hh]])
            for hh in H4:
                self.tt('dve', var[hh], var[hh], osq[hh], ALU.subtract, [b_var[hh], b_osq[hh]], [b_var[hh]])
            for hh in H4:
                self.act(var[hh], var[hh], AF.Sqrt, [b_var[hh], b_c], [b_var[hh]], bias=epsg[:, 0:1])
            for hh in H4:
                self.P.add('dve', (lambda hh: lambda e: e.reciprocal(out=rstd[hh], in_=var[hh]))(hh), reads=[b_var[hh]], writes=[b_rstd[hh]])
            for hh in H4:
                self.tt('dve', osb[hh], osb[hh], mean[hh], ALU.subtract, [b_osb[hh], b_mean[hh]], [b_osb[hh]])
            for hh in H4:
                self.tt('pool', osb[hh], osb[hh], rstd[hh], ALU.mult, [b_osb[hh], b_rstd[hh]], [b_osb[hh]])
            for hh in H4:
                self.tt('pool', GO[:, hh, :], osb[hh], sgt[:, hh, :], ALU.mult, [b_osb[hh], b_sgt], [bGO[hh]])
            for c in range(8):
                yps, byps = pst.get()
                self.mm(yps, [(Wo[:, hh, c * 128:(c + 1) * 128], GO[:, hh, :]) for hh in range(4)], bGO + rWo, [byps])
                g0, bg0 = gp.get()
                self.dma('sp', g0, self.GT[c * 128:(c + 1) * 128, sl], [self.dbuf('GT', b, c)], [bg0])
                mr, bmr = mp.get()
                self.tt('dve', mr, yps, g0, ALU.mult, [byps, bg0], [bmr])
                self.dma('sp', self.MR[c * 128:(c + 1) * 128, sl], mr, [bmr], [self.dbuf('MR', b, c)])

    def m_phase(self, l):
        P = self.P
        P.barrier()
        P.sb_reset(self.const_end)
        KNs = P.sb([128, 4, L], BF16, 'KNs')
        KPs = P.sb([64, L], BF16, 'KPs')
        VMs = P.sb([128, 32, 512], BF16, 'VMs')
        bKV = [Buf('kv%d' % b) for b in range(NB)]
        Wmo = P.sb([128, 4, D], BF16, 'Wmo')
        Wout = P.sb([128, 8, D], BF16, 'Wout')
        bWmo = self.wbufs(4, D)
        bWout = self.wbufs(8, D)
        self.load_weight_rows(Wmo, self.I('mla_w_o')[l], 4, D, bWmo)
        self.load_weight_rows(Wout, self.I('w_out')[l], 8, D, bWout)
        rWmo, rWout = self.flat(bWmo), self.flat(bWout)
        trif = P.sb([128, 128], F32, 'trif')
        tri = P.sb([128, 128], BF16, 'tri')
        eps = P.sb([128, 1], F32, 'eps')
        b_c = Buf('mc')
        self.dma('sp', trif, self.I('c_tri'), [], [b_c])
        self.copy('dve', tri, trif, [b_c], [b_c])
        self.memset('dve', eps, NORM_EPS, [b_c])
        qn = P.sb([128, 4, TB], BF16, 'qn'); b_qn = Buf()
        qp = P.sb([64, 4, TB], BF16, 'qp'); b_qp = Buf()
        ptp = Pool_(P, 6, [128, TB], BF16, 'PT')
        omlas = [P.sb([128, 4, TB], BF16, 'omla') for _ in range(2)]; b_oms = [[Buf() for _ in range(4)] for _ in range(2)]
        rden = P.sb([128, TB], F32, 'rden'); b_rden = Buf()
        merged = P.sb([128, 8, TB], BF16, 'merged'); b_mg = [Buf() for _ in range(8)]
        g2p = Pool_(P, 3, [128, TB], BF16, 'g2')
        mrp = Pool_(P, 3, [128, TB], BF16, 'mrl')
        msp = Pool_(P, 3, [128, TB], BF16, 'msl')
        accp = Pool_(P, 2, [128, TB], F32, 'acc')
        y = P.sb([128, 8, TB], F32, 'y'); by = [Buf() for _ in range(8)]
        xcp = Pool_(P, 3, [128, TB], F32, 'xc')
        sqpool = Pool_(P, 4, [128, TB], BF16, 'sq')
        rstd = P.sb([128, TB], F32, 'rstd'); b_rstd = Buf()
        tmp = P.sb([128, TB], F32, 'tmp'); b_tmp = Buf()
        ps = self.psum
        pS = PsumPool(ps[0:2], ['S0', 'S1'])
        pO = PsumPool(ps[2:4], ['O0', 'O1'])
        pDn = PsumPool(ps[4:6], ['D0', 'D1'])
        pY = PsumPool(ps[6:7], ['Y0'])
        st_ps, b_st = ps[7], Buf('mst')

        def make_tail(b, omla, b_om):
            sl = slice(b * TB, (b + 1) * TB)
            steps = []

            def merge_step(c):
                yps, byps = pY.get()
                self.mm(yps, [(Wmo[:, hh, c * 128:(c + 1) * 128], omla[:, hh, :]) for hh in range(4)], b_om + rWmo, [byps])
                g2, bg2 = g2p.get()
                self.dma('sp', g2, self.GT[2048 + c * 128:2048 + (c + 1) * 128, sl], [self.dbuf('GT', b, 16 + c)], [bg2])
                mr, bmr = mrp.get()
                self.dma('sp', mr, self.MR[c * 128:(c + 1) * 128, sl], [self.dbuf('MR', b, c)], [bmr])
                ms, bms = msp.get()
                self.dma('sp', ms, self.MS[c * 128:(c + 1) * 128, sl], [self.dbuf('MS', b, c)], [bms])
                acc, bacc = accp.get()
                self.tt('dve', acc, yps, g2, ALU.mult, [byps, bg2], [bacc])
                self.tt('pool', acc, acc, mr, ALU.add, [bacc, bmr], [bacc])
                self.tt('pool', merged[:, c, :], acc, ms, ALU.add, [bacc, bms], [b_mg[c]])

            def wout_step(c2):
                yps, byps = pY.get()
                self.mm(yps, [(Wout[:, c, c2 * 128:(c2 + 1) * 128], merged[:, c, :]) for c in range(8)], b_mg + rWout, [byps])
                self.copy('dve', y[:, c2, :], yps, [byps], [by[c2]])
                sq, bsq = sqpool.get()
                self.act(sq, y[:, c2, :], AF.Square, [by[c2]], [bsq])
                self.P.add('pe', (lambda sq, c2: lambda e: e.matmul(st_ps, lhsT=self.ones_bf, rhs=sq, start=(c2 == 0), stop=(c2 == 7)))(sq, c2),
                           reads=[bsq, self.b_ones], writes=[b_st])

            def norm_step():
                self.act(tmp, st_ps, AF.Sqrt, [b_st, b_c], [b_tmp], scale=1.0 / D, bias=eps[:, 0:1])
                self.P.add('dve', lambda e: e.reciprocal(out=rstd, in_=tmp), reads=[b_tmp], writes=[b_rstd])

            def res_step(c2):
                xc, bxc = xcp.get()
                self.dma('sp', xc, self.X[c2 * 128:(c2 + 1) * 128, sl], [self.dbuf('X', b, c2)], [bxc])
                self.tt('dve', y[:, c2, :], y[:, c2, :], rstd, ALU.mult, [by[c2], b_rstd], [by[c2]])
                self.stt(xc, y[:, c2, :], self.gcol(l, 3, c2), xc, ALU.mult, ALU.add, [by[c2], bxc, self.b_G], [bxc])
                self.dma('sp', self.X[c2 * 128:(c2 + 1) * 128, sl], xc, [bxc], [self.dbuf('X', b, c2)])

            for c in range(8):
                steps.append((lambda c: lambda: merge_step(c))(c))
            for c2 in range(8):
                steps.append((lambda c2: lambda: wout_step(c2))(c2))
            steps.append(norm_step)
            for c2 in range(8):
                steps.append((lambda c2: lambda: res_step(c2))(c2))
            return steps

        pending_tail = []
        for b in range(NB):
            sl = slice(b * TB, (b + 1) * TB)
            self.dma('sp', KNs[:, :, sl], self.KN.rearrange("(h p) t -> p h t", p=128)[:, :, sl], [self.dbuf('KN', b)], [bKV[b]])
            self.dma('sp', KPs[:, sl], self.KP[:, sl], [self.dbuf('KP', b)], [bKV[b]])
            self.dma('sp', VMs[:, 4 * b:4 * b + 4, :], self.VM[sl, :].rearrange("(t p) c -> p t c", p=128), [self.dbuf('VM', b)], [bKV[b]])
            self.dma('sp', qn, self.QN.rearrange("(h p) t -> p h t", p=128)[:, :, sl], [self.dbuf('QN', b)], [b_qn])
            self.dma('sp', qp, self.QP.rearrange("(h p) t -> p h t", p=64)[:, :, sl], [self.dbuf('QP', b)], [b_qp])
            nkt = 4 * (b + 1)
            omla, b_om = omlas[b % 2], b_oms[b % 2]
            for hh in range(4):
                O_ps, bO = pO.get()
                Dn, bDn = pDn.get()
                def emit_S(kt, hh=hh):
                    c0 = 128 * (kt - 4 * b) if kt >= 4 * b else 0
                    ks = slice(kt * 128, (kt + 1) * 128)
                    kvb = bKV[kt // 4]
                    S_ps, bS_ = pS.get()
                    self.mm(S_ps[:, c0:TB], [(KNs[:, hh, ks], qn[:, hh, c0:TB]), (KPs[0:64, ks], qp[0:64, hh, c0:TB])], [kvb, b_qn, b_qp], [bS_])
                    return S_ps, bS_, c0, kvb
                nxt = emit_S(0)
                for kt in range(nkt):
                    S_ps, bS_, c0, kvb = nxt
                    if kt + 1 < nkt:
                        nxt = emit_S(kt + 1)
                    PT, bPT = ptp.get()
                    self.act(PT[:, c0:TB], S_ps[:, c0:TB], AF.Exp, [bS_], [bPT])
                    if kt >= 4 * b:
                        self.tt('pool', PT[:, c0:c0 + 128], PT[:, c0:c0 + 128], tri, ALU.mult, [bPT, b_c], [bPT])
                    first, lastk = (kt == 0), (kt == nkt - 1)
                    self.P.add('pe', (lambda O_ps, PT, c0, kt, hh, first, lastk: lambda e: e.matmul(
                        O_ps[:, c0:TB], lhsT=VMs[:, kt, hh * 128:(hh + 1) * 128], rhs=PT[:, c0:TB], start=first, stop=lastk))(O_ps, PT, c0, kt, hh, first, lastk),
                        reads=[kvb, bPT], writes=[bO])
                    self.P.add('pe', (lambda Dn, PT, c0, first, lastk: lambda e: e.matmul(
                        Dn[:, c0:TB], lhsT=self.ones_bf, rhs=PT[:, c0:TB], start=first, stop=lastk))(Dn, PT, c0, first, lastk),
                        reads=[self.b_ones, bPT], writes=[bDn])
                    if pending_tail:
                        pending_tail.pop(0)()
                self.P.add('dve', (lambda Dn: lambda e: e.reciprocal(out=rden, in_=Dn))(Dn), reads=[bDn], writes=[b_rden])
                self.tt('dve', omla[:, hh, :], O_ps, rden, ALU.mult, [bO, b_rden], [b_om[hh]])
            while pending_tail:
                pending_tail.pop(0)()
            pending_tail.extend(make_tail(b, omla, b_om))
        while pending_tail:
            pending_tail.pop(0)()

    def trig(self, ang, b_ang, n, out_sin, out_cos, b_out, wk):
        MAG = 12582912.0
        C1 = 6.28125
        C2 = 2.0 * math.pi - 6.28125
        a2, k, b_w = wk
        for dst, shift in ((out_sin, 0.0), (out_cos, math.pi / 2)):
            self.ts('dve', a2[:, 0:n], ang, shift, None, ALU.add, None, [b_ang], [b_w])
            self.ts('dve', k[:, 0:n], a2[:, 0:n], 1.0 / (2.0 * math.pi), MAG, ALU.mult, ALU.add, [b_w], [b_w])
            self.ts('dve', k[:, 0:n], k[:, 0:n], -MAG, None, ALU.add, None, [b_w], [b_w])
            self.stt(a2[:, 0:n], k[:, 0:n], -C1, a2[:, 0:n], ALU.mult, ALU.add, [b_w], [b_w])
            self.stt(a2[:, 0:n], k[:, 0:n], -C2, a2[:, 0:n], ALU.mult, ALU.add, [b_w], [b_w])
            self.ts('dve', a2[:, 0:n], a2[:, 0:n], 3.141592, -3.141592, ALU.min, ALU.max, [b_w], [b_w])
            self.act(dst, a2[:, 0:n], AF.Sin, [b_w], [b_out])

    def s_phase(self, l):
        P = self.P
        P.barrier()
        P.sb_reset(self.const_end)
        T = ST
        ps = self.psum
        Wa = P.sb([128, 4, D], BF16, 'Wa')
        Wb = P.sb([128, 4, D], BF16, 'Wb')
        bWa = self.wbufs(4, D)
        bWb = self.wbufs(4, D)
        self.load_weight_rows(Wa, self.I('s5_glu_a')[l], 4, D, bWa)
        self.load_weight_rows(Wb, self.I('s5_glu_b')[l], 4, D, bWb)
        rWa, rWb = self.flat(bWa), self.flat(bWb)
        are = P.sb([128, 16], F32); aim = P.sb([128, 16], F32); ldt = P.sb([128, 16], F32)
        bre = P.sb([128, 16, 16], F32); bim = P.sb([128, 16, 16], F32)
        cre = P.sb([128, 16, 16], F32); cim = P.sb([128, 16, 16], F32)
        dsk = P.sb([128, 4], F32)
        step = P.sb([128, N2], F32)
        b_par = Buf('s5par')
        for t_, nm in ((are, 's5_are'), (aim, 's5_aim'), (ldt, 's5_ldt'), (bre, 's5_bre'), (bim, 's5_bim'), (cre, 's5_cre'), (cim, 's5_cim'), (dsk, 's5_d')):
            self.dma('sp', t_, self.I(nm)[l], [], [b_par])
        self.dma('sp', step, self.I('c_step'), [], [b_par])
        adt = P.sb([128, 16], F32); th = P.sb([128, 16], F32); dtt = P.sb([128, 16], F32)
        b_adt = Buf('adt')
        self.act(dtt, ldt, AF.Exp, [b_par], [b_adt])
        self.tt('dve', adt, are, dtt, ALU.mult, [b_par, b_adt], [b_adt])
        self.tt('dve', th, aim, dtt, ALU.mult, [b_par, b_adt], [b_adt])
        wk = (P.sb([128, 4 * N2], F32, 'wk_a2'), P.sb([128, 4 * N2], F32, 'wk_k'), Buf('wk'))
        angs = P.sb([128, 16], F32); b_angs = Buf()
        mag = P.sb([128, 16], F32); b_mag = Buf()
        sn = P.sb([128, 16], F32); cs = P.sb([128, 16], F32); b_sc = Buf()

        def lam_pow(n, lr, li, nli, b_l):
            self.act(mag, adt, AF.Exp, [b_adt], [b_mag], scale=float(n))
            self.ts('dve', angs, th, float(n), None, ALU.mult, None, [b_adt], [b_angs])
            self.trig(angs, b_angs, 16, sn, cs, b_sc, wk)
            self.tt('dve', lr, mag, cs, ALU.mult, [b_mag, b_sc], [b_l])
            self.tt('dve', li, mag, sn, ALU.mult, [b_mag, b_sc], [b_l])
            self.ts('dve', nli, li, -1.0, None, ALU.mult, None, [b_l], [b_l])

        lr = P.sb([128, 16], F32); li = P.sb([128, 16], F32); nli = P.sb([128, 16], F32); b_l = Buf('lam')
        lam_pow(1, lr, li, nli, b_l)
        fre = P.sb([128, 16], F32); fim = P.sb([128, 16], F32); b_f = Buf('f')
        den = P.sb([128, 16], F32); t16 = P.sb([128, 16], F32); nr = P.sb([128, 16], F32)
        self.tt('dve', den, are, are, ALU.mult, [b_par], [b_f])
        self.tt('dve', t16, aim, aim, ALU.mult, [b_par], [b_f])
        self.tt('dve', den, den, t16, ALU.add, [b_f], [b_f])
        self.P.add('dve', lambda e: e.reciprocal(out=den, in_=den), reads=[b_f], writes=[b_f])
        self.ts('dve', nr, lr, -1.0, None, ALU.add, None, [b_l], [b_f])
        self.tt('dve', fre, nr, are, ALU.mult, [b_f, b_par], [b_f])
        self.tt('dve', t16, li, aim, ALU.mult, [b_l, b_par], [b_f])
        self.tt('dve', fre, fre, t16, ALU.add, [b_f], [b_f])
        self.tt('dve', fre, fre, den, ALU.mult, [b_f], [b_f])
        self.tt('dve', fim, li, are, ALU.mult, [b_l, b_par], [b_f])
        self.tt('dve', t16, nr, aim, ALU.mult, [b_f, b_par], [b_f])
        self.tt('dve', fim, fim, t16, ALU.subtract, [b_f], [b_f])
        self.tt('dve', fim, fim, den, ALU.mult, [b_f], [b_f])

        def bc(a):
            return a.unsqueeze(2).to_broadcast([128, 16, 16])

        def cmul(out_re, out_im, sr, si, xr, xi, reads, b_o, t_a, neg_im=False):
            self.tt('dve', out_re, xr, bc(sr), ALU.mult, reads, [b_o])
            self.tt('dve', t_a, xi, bc(si), ALU.mult, reads, [b_o])
            self.tt('dve', out_re, out_re, t_a, ALU.subtract, [b_o], [b_o])
            self.tt('dve', out_im, xi, bc(sr), ALU.mult, reads, [b_o])
            self.tt('dve', t_a, xr, bc(si), ALU.mult, reads, [b_o])
            self.tt('dve', out_im, out_im, t_a, ALU.add, [b_o], [b_o])
            if neg_im:
                self.ts('dve', out_im, out_im, -1.0, None, ALU.mult, None, [b_o], [b_o])

        bbre = P.sb([128, 16, 16], F32); bbim = P.sb([128, 16, 16], F32); b_bb = Buf('bb')
        t256 = P.sb([128, 16, 16], F32)
        cmul(bbre, bbim, fre, fim, bre, bim, [b_f, b_par], b_bb, t256)
        NatB = [P.sb([128, 16, 128], F32, 'NatBre', top=True), P.sb([128, 16, 128], F32, 'NatBim', top=True)]
        NatT = [P.sb([128, 16, 128], F32, 'NatTre', top=True), P.sb([128, 16, 128], F32, 'NatTim', top=True)]
        b_natB = Buf('natB'); b_natT = Buf('natT')
        for t_ in NatB:
            self.memset('pool', t_, 0.0, [b_natB])
        for t_ in NatT:
            self.memset('pool', t_, 0.0, [b_natT])

        def scatter(nat, src, b_src, b_nat):
            for m in range(4):
                for g2 in range(2):
                    rows = slice(64 * g2, 64 * g2 + 64)
                    dst = nat.rearrange("p (c m) x -> p c m x", m=4)[rows, :, m, m * 32 + g2 * 16:m * 32 + g2 * 16 + 16]
                    srcv = src.rearrange("p (c m) h -> p c m h", m=4)[rows, :, m, :]
                    self.copy('pool', dst, srcv, [b_src], [b_nat])

        scatter(NatB[0], bbre, b_bb, b_natB)
        scatter(NatB[1], bbim, b_bb, b_natB)
        identf = self.ident
        BfT = P.sb([128, 16 * T * 2, 128], BF16, 'BfT'); b_BfT = Buf('BfT')
        xr = P.sb([128, 16, 16], F32); xi = P.sb([128, 16, 16], F32); b_x = Buf('x')
        psetup = PsumPool(ps[0:4], ['su%d' % i for i in range(4)])
        for j in range(T):
            if j == T - 1:
                natre, natim, b_nat = NatB[0], NatB[1], b_natB
            else:
                lam_pow(T - 1 - j, lr, li, nli, b_l)
                cmul(xr, xi, lr, li, bbre, bbim, [b_l, b_bb], b_x, t256)
                scatter(NatT[0], xr, b_x, b_natT)
                scatter(NatT[1], xi, b_x, b_natT)
                natre, natim, b_nat = NatT[0], NatT[1], b_natT
            for ri, nat in ((0, natre), (1, natim)):
                for q in range(16):
                    pt, bp = psetup.get()
                    self.mm(pt[:, 0:128], [(nat[:, q, :], identf)], [b_nat, self.b_ident], [bp])
                    self.copy('act', BfT[:, (q * T + j) * 2 + ri, :], pt[:, 0:128], [bp], [b_BfT])
        Cf = P.sb([128, 16 * T * 2, 64], BF16, 'Cf'); b_Cf = Buf('Cf')
        self.memset('pool', Cf, 0.0, [b_Cf])
        Kd = P.sb([128, 4 * T, 128], BF16, 'Kd'); b_Kd = Buf('Kd')
        Cfv = Cf.rearrange("p (q j r) x -> p q j r x", q=16, j=T)
        for d in range(T + 1):
            if d == 0:
                self.copy('dve', xr, cre, [b_par], [b_x])
                self.ts('dve', xi, cim, -1.0, None, ALU.mult, None, [b_par], [b_x])
            else:
                lam_pow(d, lr, li, nli, b_l)
                cmul(xr, xi, lr, li, cre, cim, [b_l, b_par], b_x, t256, neg_im=True)
            if d >= 1:
                j = d - 1
                for ri, src in ((0, xr), (1, xi)):
                    for g2 in range(2):
                        rows = slice(64 * g2, 64 * g2 + 64)
                        for mm_ in range(2):
                            co = mm_ * 32 + g2 * 16
                            dstv = Cfv.rearrange("p (a b) j r x -> p a b j r x", b=2)[rows, :, mm_, j, ri, co:co + 16]
                            srcv = src.rearrange("p (a b) h -> p a b h", b=2)[rows, :, mm_, :]
                            self.copy('pool', dstv, srcv, [b_x], [b_Cf])
            if d <= T - 1:
                scatter(NatT[0], xr, b_x, b_natT)
                scatter(NatT[1], xi, b_x, b_natT)
                for c4 in range(4):
                    pt, bp = psetup.get()
                    pairs = []
                    for m in range(4):
                        pairs.append((NatB[0][:, 4 * c4 + m, :], NatT[0][:, 4 * c4 + m, :]))
                        pairs.append((NatB[1][:, 4 * c4 + m, :], NatT[1][:, 4 * c4 + m, :]))
                    self.mm(pt[:, 0:128], pairs, [b_natB, b_natT], [bp])
                    self.copy('act', Kd[:, c4 * T + d, :], pt[:, 0:128], [bp], [b_Kd])
        cosE = P.sb([128, 16, N2], F32, 'cosE'); sinE = P.sb([128, 16, N2], F32, 'sinE'); b_E = Buf('E')
        angE = P.sb([128, 16, N2], F32, 'angE', top=True); b_angE = Buf('angE')
        thT = P.sb([128, 16], F32)
        rho = P.sb([128, 16], F32); b_rho = Buf('rho')
        self.ts('dve', thT, th, float(T), None, ALU.mult, None, [b_adt], [b_rho])
        self.act(rho, adt, AF.Exp, [b_adt], [b_rho], scale=float(T))
        for q in range(16):
            self.ts('dve', angE[:, q, :], step, thT[:, q:q + 1], None, ALU.mult, None, [b_par, b_rho], [b_angE])
        for qq in range(4):
            self.trig(angE[:, 4 * qq:4 * qq + 4, :].rearrange("p q k -> p (q k)"), b_angE, 4 * N2, sinE[:, 4 * qq:4 * qq + 4, :].rearrange("p q k -> p (q k)"),
                      cosE[:, 4 * qq:4 * qq + 4, :].rearrange("p q k -> p (q k)"), b_E, wk)
        Zf = P.sb([128, 16, 2, N2 + 1], F32, 'Zf'); bZ = [Buf('Z%d' % q) for q in range(16)]
        bZc = [Buf('Zc%d' % c4) for c4 in range(4)]
        self.memset('pool', Zf, 0.0, bZc)
        P.barrier()
        P.top_off = 0
        ubs = [P.sb([128, 4, TB], BF16, 'ub') for _ in range(2)]; b_ubs = [Buf('ub0'), Buf('ub1')]
        tp = Pool_(P, 12, [128, 4, N2], F32, 'tS')
        Xp = Pool_(P, 4, [128, 4, N2], F32, 'XS')
        Wp = Pool_(P, 4, [128, 4, N2], F32, 'WS')
        Zbp = Pool_(P, 3, [128, 4, 2, N2], BF16, 'Zb')
        ysfs = [P.sb([128, 4, TB], BF16, 'ysf') for _ in range(2)]; b_ysfs = [[Buf() for _ in range(4)] for _ in range(2)]
        ysum = Pool_(P, 2, [128, TB], F32, 'ysum')
        sgp = Pool_(P, 2, [128, TB], F32, 'sgb')
        g1p = Pool_(P, 4, [128, TB], BF16, 'g1')
        mtp = Pool_(P, 2, [128, TB], F32, 'mt')
        msp = Pool_(P, 3, [128, TB], BF16, 'ms')
        pVr = PsumPool(ps[0:2], ['Vr0', 'Vr1'])
        pVi = PsumPool(ps[2:4], ['Vi0', 'Vi1'])
        pYs = PsumPool(ps[4:6], ['Ys0', 'Ys1'])
        pA = PsumPool(ps[6:7], ['A0'])
        pB = PsumPool(ps[7:8], ['B0'])

        def v4(t):
            return t.rearrange("p (m k) -> p m k", m=4)

        def chunk_item(b, c4):
            sl = slice(b * TB, (b + 1) * TB)
            ub, b_ub = ubs[b % 2], b_ubs[b % 2]
            ysf, b_ysf = ysfs[b % 2], b_ysfs[b % 2]
            q0 = 4 * c4
            cE, sE = cosE[:, q0:q0 + 4, :], sinE[:, q0:q0 + 4, :]
            d = {}

            def s1():
                if c4 == 0:
                    self.dma('sp', ub, self.U.rearrange("(h p) t -> p h t", p=128)[:, :, sl], [self.dbuf('U', b)], [b_ub])
                d['Vr'] = pVr.get()
                d['Vi'] = pVi.get()
                for m in range(4):
                    for ri, (Vt, bVt) in enumerate((d['Vr'], d['Vi'])):
                        prs = [(BfT[:, ((q0 + m) * T + j) * 2 + ri, :], ub[:, c4, j:TB:T]) for j in range(T)]
                        self.mm(Vt[:, m * N2:(m + 1) * N2], prs, [b_BfT, b_ub], [bVt])

            def s2():
                (Vr, bVr), (Vi, bVi) = d['Vr'], d['Vi']
                d['t'] = [tp.get() for _ in range(4)]
                (t1, bt1), (t2, bt2), (t3, bt3), (t4, bt4) = d['t']
                self.tt('dve', t1, v4(Vr), cE, ALU.mult, [bVr, b_E], [bt1])
                self.tt('dve', t2, v4(Vi), sE, ALU.mult, [bVi, b_E], [bt2])
                self.tt('dve', t3, v4(Vi), cE, ALU.mult, [bVi, b_E], [bt3])
                self.tt('dve', t4, v4(Vr), sE, ALU.mult, [bVr, b_E], [bt4])

            def s3():
                (t1, bt1), (t2, bt2), (t3, bt3), (t4, bt4) = d['t']
                d['X'] = (Xp.get(), Xp.get())
                (Xre, bXre), (Xim, bXim) = d['X']
                self.tt('pool', Xre, t1, t2, ALU.add, [bt1, bt2], [bXre])
                self.tt('pool', Xim, t3, t4, ALU.subtract, [bt3, bt4], [bXim])
                self.copy('pool', Zf[:, q0:q0 + 4, :, 0:1], Zf[:, q0:q0 + 4, :, N2:N2 + 1], [bZc[c4]], [bZc[c4]])

            def s4():
                (Xre, bXre), (Xim, bXim) = d['X']
                d['W'] = (Wp.get(), Wp.get())
                (Wre, bWre), (Wim, bWim) = d['W']
                for m in range(4):
                    q = q0 + m
                    rb = rho[:, q:q + 1].to_broadcast([128, N2])
                    self.P.add('dve', (lambda Wre, rb, Xre, q, m: lambda e: e.tensor_tensor_scan(out=Wre[:, m, :], data0=rb, data1=Xre[:, m, :], initial=Zf[:, q, 0, 0:1], op0=ALU.mult, op1=ALU.add))(Wre, rb, Xre, q, m),
                               reads=[b_rho, bXre, bZc[c4]], writes=[bWre])
                    self.P.add('dve', (lambda Wim, rb, Xim, q, m: lambda e: e.tensor_tensor_scan(out=Wim[:, m, :], data0=rb, data1=Xim[:, m, :], initial=Zf[:, q, 1, 0:1], op0=ALU.mult, op1=ALU.add))(Wim, rb, Xim, q, m),
                               reads=[b_rho, bXim, bZc[c4]], writes=[bWim])

            def s5():
                (Wre, bWre), (Wim, bWim) = d['W']
                d['u'] = [tp.get() for _ in range(4)]
                (u1, bu1), (u2, bu2), (u3, bu3), (u4, bu4) = d['u']
                self.tt('pool', u1, Wre, cE, ALU.mult, [bWre, b_E], [bu1])
                self.tt('pool', u2, Wim, sE, ALU.mult, [bWim, b_E], [bu2])
                self.tt('pool', u3, Wim, cE, ALU.mult, [bWim, b_E], [bu3])
                self.tt('pool', u4, Wre, sE, ALU.mult, [bWre, b_E], [bu4])

            def s6():
                (u1, bu1), (u2, bu2), (u3, bu3), (u4, bu4) = d['u']
                self.tt('dve', Zf[:, q0:q0 + 4, 0, 1:N2 + 1], u1, u2, ALU.subtract, [bu1, bu2], [bZc[c4]])
                self.tt('dve', Zf[:, q0:q0 + 4, 1, 1:N2 + 1], u3, u4, ALU.add, [bu3, bu4], [bZc[c4]])

            def s7():
                d['Zb'] = Zbp.get()
                Zb, bZb = d['Zb']
                for m in range(4):
                    self.copy('act', Zb[:, m], Zf[:, q0 + m, :, 0:N2], [bZc[c4]], [bZb])

            def s8():
                Zb, bZb = d['Zb']
                d['Y'] = pYs.get()
                Yp, bY = d['Y']
                for j in range(T):
                    ops_ = []
                    for i in range(j + 1):
                        ops_.append((Yp[:, j:TB:T], Kd[:, c4 * T + (j - i), :], ub[:, c4, i:TB:T]))
                    for m in range(4):
                        for ri in range(2):
                            ops_.append((Yp[64 * (m // 2):64 * (m // 2) + 64, j:TB:T], Cfv[:, q0 + m, j, ri, :], Zb[:, m, ri, :]))

                    def fn(e, ops_=ops_):
                        ins = None
                        for ii, (o_, l_, r_) in enumerate(ops_):
                            ins = e.matmul(o_, lhsT=l_, rhs=r_, start=(ii == 0), stop=(ii == len(ops_) - 1))
                        return ins
                    self.P.add('pe', fn, reads=[b_Kd, b_Cf, b_ub, bZb], writes=[bY])

            def s9():
                Yp, bY = d['Y']
                ys_, bys = ysum.get()
                self.stt(ys_, ub[:, c4, :], dsk[:, c4:c4 + 1], Yp, ALU.mult, ALU.add, [b_ub, b_par, bY], [bys])
                self.act(ysf[:, c4, :], ys_, AF.Gelu, [bys], [b_ysf[c4]])

            return [s1, s2, s3, s4, s5, s6, s7, s8, s9]

        def glu_item(b, c):
            sl = slice(b * TB, (b + 1) * TB)
            ysf, b_ysf = ysfs[b % 2], b_ysfs[b % 2]
            d = {}

            def g1():
                d['A'] = pA.get()
                d['B'] = pB.get()
                (Ap, bA), (Bp, bB) = d['A'], d['B']
                self.mm(Ap, [(Wa[:, c4, c * 128:(c + 1) * 128], ysf[:, c4, :]) for c4 in range(4)], b_ysf + rWa, [bA])
                self.mm(Bp, [(Wb[:, c4, c * 128:(c + 1) * 128], ysf[:, c4, :]) for c4 in range(4)], b_ysf + rWb, [bB])
                d['g1'] = g1p.get()
                g1_, bg1 = d['g1']
                self.dma('sp', g1_, self.GT[1024 + c * 128:1024 + (c + 1) * 128, sl], [self.dbuf('GT', b, 8 + c)], [bg1])

            def g3():
                (Bp, bB) = d['B']
                d['sg'] = sgp.get()
                sgb, bsg = d['sg']
                self.act(sgb, Bp, AF.Sigmoid, [bB], [bsg])
                (Ap, bA) = d['A']
                d['mt'] = mtp.get()
                mt, bmt = d['mt']
                self.tt('dve', mt, Ap, sgb, ALU.mult, [bA, bsg], [bmt])

            def g4():
                mt, bmt = d['mt']
                g1_, bg1 = d['g1']
                ms, bms = msp.get()
                self.tt('pool', ms, mt, g1_, ALU.mult, [bmt, bg1], [bms])
                self.dma('sp', self.MS[c * 128:(c + 1) * 128, sl], ms, [bms], [self.dbuf('MS', b, c)])

            return [g1, g3, g4]

        class Item:
            def __init__(self, stages, after):
                self.stages, self.after, self.pos, self.done = stages, after, 0, False

        seq = []
        chunks = {}
        for b in range(NB):
            glu_prev = []
            if b >= 1:
                glu_prev = [Item(glu_item(b - 1, c), [chunks[(b - 1, k)] for k in range(4)]) for c in range(8)]
            for c4 in range(4):
                it = Item(chunk_item(b, c4), [])
                chunks[(b, c4)] = it
                seq.append(it)
                seq.extend(glu_prev[2 * c4:2 * c4 + 2])
        seq.extend(Item(glu_item(NB - 1, c), [chunks[(NB - 1, k)] for k in range(4)]) for c in range(8))
        active = []
        started = set()
        WIN = 6
        while active or len(started) < len(seq):
            if len(active) < WIN:
                n_unstarted = 0
                for i_, it in enumerate(seq):
                    if i_ in started:
                        continue
                    n_unstarted += 1
                    if all(a_.done for a_ in it.after):
                        started.add(i_)
                        active.append(it)
                        break
                    if n_unstarted >= 4:
                        break
            for it in list(active):
                it.stages[it.pos]()
                it.pos += 1
                if it.pos == len(it.stages):
                    it.done = True
                    active.remove(it)

    def finish(self):
        P = self.P
        if self.dump:
            name, shape, dt = self.dump
            src = self.scratch[name]
            P.barrier()
            self.dma('sp', self.dbg, src, [], [Buf()])
        P.barrier()
        P.flush()
        P.close()
        return self.nc

    def build(self):
        self.declare()
        self.consts()
        nl = self.n_layers
        ph = self.phases
        for l in range(nl):
            src, sname = (self.I('xT'), 'xT') if l == 0 else (self.X, 'X')
            if l == 0 and (ph is None or 'B1' in ph):
                self.rope_tables()
            if ph is None or 'F1' in ph:
                self.ffn_phase(l, 0, src, sname, self.X, 'X')
            if ph is None or 'B1' in ph:
                self.b1_phase(l)
            if ph is None or 'R' in ph:
                self.r_phase(l)
            if ph is None or 'S' in ph:
                self.s_phase(l)
            if ph is None or 'M' in ph:
                self.m_phase(l)
            if ph is None or 'F2' in ph:
                last = (l == nl - 1)
                self.ffn_phase(l, 1, self.X, 'X', self.out if last else self.X, 'out' if last else 'X')
        return self.finish()


def host_constants():
    c = {}
    c['c_ident'] = np.eye(128, dtype=np.float32)
    m = np.arange(128)
    c['c_tri'] = (m[None, :] >= m[:, None]).astype(np.float32)
    lg = np.log1p(-np.exp2(-5.0 - np.arange(4, dtype=np.float64)))
    rel = (m[None, :] - m[:, None]).astype(np.float64)
    dec = np.zeros((128, 4, 128), np.float32)
    for h in range(4):
        dec[:, h, :] = np.where(rel >= 0, np.exp(np.maximum(rel, 0) * lg[h]), 0.0)
    c['c_dec'] = dec
    qd = np.zeros((128, 4, 128), np.float32)
    for h in range(4):
        qd[:, h, :] = np.exp((m + 1.0) * lg[h])[None, :]
    c['c_qdec'] = qd
    c['c_kdec'] = np.stack([np.exp((127.0 - m) * lg[h]) for h in range(4)], axis=1).astype(np.float32)
    fr = np.zeros((128, 2), np.float32)
    inv_r = (10000.0 ** (-np.arange(0, 128, 2, dtype=np.float32) / 128)).astype(np.float32)
    inv_m = (10000.0 ** (-np.arange(0, 64, 2, dtype=np.float32) / 64)).astype(np.float32)
    fr[:, 0] = np.concatenate([inv_r, inv_r])
    fr[:64, 1] = np.concatenate([inv_m, inv_m])
    c['c_freq'] = fr
    sg = np.ones((128, 2), np.float32)
    sg[:64, 0] = -1.0
    sg[:32, 1] = -1.0
    c['c_sgn'] = sg
    c['c_step'] = np.broadcast_to(np.arange(1, N2 + 1, dtype=np.float32)[None, :], (128, N2)).copy()
    return c


def host_layout(inp, b):
    m = {}
    m['xT'] = np.ascontiguousarray(inp['x'][b].T)
    m['pos'] = np.ascontiguousarray(inp['positions'][b:b + 1]).astype(np.int32)
    g = np.asarray(inp['norm_gains'], np.float32)
    m['gains'] = np.ascontiguousarray(g.reshape(DEPTH, 6, 8, 128).transpose(3, 0, 1, 2).reshape(128, DEPTH * 48))
    for k in ('ffn_w_gate', 'ffn_w_up', 'ffn_w_down', 'w_in', 'ret_w_o', 's5_glu_a', 's5_glu_b', 'mla_w_uq', 'mla_w_ukv', 'mla_w_o', 'w_out'):
        m[k] = np.ascontiguousarray(inp[k], dtype=np.float32)

    def pair(a):
        return np.ascontiguousarray(a.reshape(DEPTH, 16, 2, 64).transpose(0, 2, 3, 1).reshape(DEPTH, 128, 16))
    m['s5_are'] = pair(np.asarray(inp['s5_a_re']))
    m['s5_aim'] = pair(np.asarray(inp['s5_a_im']))
    m['s5_ldt'] = pair(np.repeat(np.asarray(inp['s5_log_dt'])[:, :, None], 64, axis=2))
    m['s5_bre'] = np.ascontiguousarray(np.asarray(inp['s5_b_re']).reshape(DEPTH, 16, 2, 64, 16).transpose(0, 2, 3, 1, 4).reshape(DEPTH, 128, 16, 16))
    m['s5_bim'] = np.ascontiguousarray(np.asarray(inp['s5_b_im']).reshape(DEPTH, 16, 2, 64, 16).transpose(0, 2, 3, 1, 4).reshape(DEPTH, 128, 16, 16))
    m['s5_cre'] = np.ascontiguousarray(np.asarray(inp['s5_c_re']).reshape(DEPTH, 16, 2, 16, 64).transpose(0, 2, 4, 1, 3).reshape(DEPTH, 128, 16, 16))
    m['s5_cim'] = np.ascontiguousarray(np.asarray(inp['s5_c_im']).reshape(DEPTH, 16, 2, 16, 64).transpose(0, 2, 4, 1, 3).reshape(DEPTH, 128, 16, 16))
    m['s5_d'] = np.ascontiguousarray(np.asarray(inp['s5_d']).reshape(DEPTH, 4, 128).transpose(0, 2, 1))
    m['mla_q_norm'] = np.ascontiguousarray(np.asarray(inp['mla_q_norm']).reshape(DEPTH, 2, 128).transpose(0, 2, 1))
    m['mla_kv_norm'] = np.ascontiguousarray(np.asarray(inp['mla_kv_norm']).reshape(DEPTH, 128, 1))
    return m


_CACHE = {}


def kernel(**inputs):
    if 'nc' not in _CACHE:
        _CACHE['nc'] = Builder().build()
    nc = _CACHE['nc']
    consts = host_constants()
    in_maps = []
    shared = None
    for b in range(8):
        m = host_layout(inputs, b)
        if shared is None:
            shared = {k: v for k, v in m.items() if k not in ('xT', 'pos')}
        else:
            for k in shared:
                m[k] = shared[k]
        m.update(consts)
        in_maps.append(m)
    res = run_bass_kernel_spmd(nc, in_maps, core_ids=list(range(8)))
    out = np.stack([np.ascontiguousarray(res.results[b]['outT'].T) for b in range(8)], axis=0)
    return out.astype(np.float32)
```

```python
import contextlib
import math
import numpy as np
import concourse.bass as bass
import concourse.mybir as mybir
from concourse.bass_utils import run_bass_kernel_spmd

F32 = mybir.dt.float32
BF16 = mybir.dt.bfloat16
I32 = mybir.dt.int32
AF = mybir.ActivationFunctionType
ALU = mybir.AluOpType

D = 1024
L = 4096
NB = 8
TB = 512
DFF = 2816
NFF = 22
INW = 6080
DEPTH = 4
NORM_EPS = 1e-6
GN_EPS = 1e-5
ST = 4
N2 = TB // ST

SAME_ENG_SYNC = True
DMA_SLOTS = {'sp': 24, 'act': 6, 'pool': 20}


class Buf:
    __slots__ = ('name', 'w', 'r')

    def __init__(self, name=''):
        self.name = name
        self.w = []
        self.r = []


class Op:
    __slots__ = ('eng', 'fn', 'deps', 'dma', 'slot', 'gen', 'sig', 'sigval', 'waits', 'idx')


class Prog:
    def __init__(self, nc):
        self.nc = nc
        self.ops = []
        self.emitted = 0
        self.stack = contextlib.ExitStack()
        self.esem = {}
        for e in ('pe', 'act', 'dve', 'pool', 'sp'):
            self.esem[e] = self.stack.enter_context(nc.semaphore('es_' + e))
        self.dsem = {}
        for q, k in DMA_SLOTS.items():
            self.dsem[q] = [self.stack.enter_context(nc.semaphore('ds_%s%d' % (q, i))) for i in range(k)]
        self.dcount = {q: 0 for q in DMA_SLOTS}
        self.dhist = {q: [] for q in DMA_SLOTS}
        self.clocks = {e: {} for e in self.esem}
        self.sigcnt = {e: 0 for e in self.esem}
        self.lastop = {}
        self.sb_off = 0
        self.SB_BYTES = 207 * 1024
        self.big = nc.alloc_sbuf_tensor('big', [128, self.SB_BYTES], mybir.dt.uint8)

    def sb_reset(self, off=0):
        self.sb_off = off

    def sb(self, shape, dtype, name=None, top=False):
        nbytes = int(np.prod(shape[1:])) * mybir.dt.size(dtype)
        if top:
            self.top_off = (getattr(self, 'top_off', 0) + nbytes + 63) // 64 * 64
            off = self.SB_BYTES - self.top_off
            assert off >= self.sb_off, ('SBUF overflow(top)', name)
        else:
            off = (self.sb_off + 63) // 64 * 64
            self.sb_off = off + nbytes
            assert self.sb_off <= self.SB_BYTES - getattr(self, 'top_off', 0), ('SBUF overflow', name, self.sb_off)
        v = self.big[:, off:off + nbytes].bitcast(dtype)
        if len(shape) == 3:
            v = v.rearrange("p (a b) -> p a b", a=shape[1])
        elif len(shape) == 4:
            v = v.rearrange("p (a b c) -> p a b c", a=shape[1], b=shape[2])
        if shape[0] < 128:
            v = v[0:shape[0]]
        return v

    def add(self, eng, fn, reads=(), writes=(), dma=False):
        op = Op()
        op.idx = len(self.ops)
        op.eng = eng
        op.fn = fn
        op.dma = dma
        op.sig = False
        op.sigval = None
        deps = set()
        for b in reads:
            deps.update(b.w)
            b.r.append(op.idx)
        for b in writes:
            deps.update(b.w)
            deps.update(b.r)
            b.w = [op.idx]
            b.r = []
        if dma:
            k = DMA_SLOTS[eng]
            n = self.dcount[eng]
            self.dcount[eng] = n + 1
            op.slot = n % k
            op.gen = n // k
            if n >= k:
                deps.add(self.dhist[eng][n - k])
            self.dhist[eng].append(op.idx)
        elif fn is not None:
            self.lastop[eng] = op.idx
        deps.discard(op.idx)
        op.deps = deps
        self.ops.append(op)
        return op.idx

    def barrier(self):
        deps = set(self.lastop.values())
        for q, k in DMA_SLOTS.items():
            deps.update(self.dhist[q][-k:])
        for e in self.esem:
            i = self.add(e, None)
            self.ops[i].deps.update(deps)

    def flush(self):
        nc = self.nc
        ops = self.ops
        new = ops[self.emitted:]
        for op in new:
            waits = []
            clk = self.clocks[op.eng]
            for d in sorted(op.deps):
                dop = ops[d]
                if dop.dma:
                    key = ('D', dop.eng, dop.slot)
                    val = dop.gen + 1
                else:
                    if dop.eng == op.eng and (op.eng in ('pe', 'sp') or not SAME_ENG_SYNC):
                        continue
                    key = ('E', dop.eng)
                    val = d
                if clk.get(key, -1) >= val:
                    continue
                clk[key] = val
                waits.append(d)
                if not dop.dma and d >= self.emitted:
                    dop.sig = True
            op.waits = waits
        last = {}
        for op in new:
            if not op.dma and op.fn is not None:
                last[op.eng] = op
        for op in last.values():
            op.sig = True
        for op in new:
            if not op.dma and op.sig:
                self.sigcnt[op.eng] += 1
                op.sigval = self.sigcnt[op.eng]
        per = {e: [] for e in self.esem}
        for op in new:
            per[op.eng].append(op)
        self.emitted = len(ops)

        def event(d):
            dop = ops[d]
            if dop.dma:
                return self.dsem[dop.eng][dop.slot], 16 * (dop.gen + 1)
            if dop.sigval is None:
                j = d
                while ops[j].eng != dop.eng or ops[j].dma or ops[j].sigval is None:
                    j += 1
                return self.esem[dop.eng], ops[j].sigval
            return self.esem[dop.eng], dop.sigval

        def runner(engname):
            def f(e):
                for op in per[engname]:
                    for d in op.waits:
                        s, v = event(d)
                        e.wait_ge(s, v)
                    if op.fn is None:
                        continue
                    ins = op.fn(e)
                    if op.dma:
                        ins.then_inc(self.dsem[op.eng][op.slot], 16)
                    elif op.sig:
                        ins.then_inc(self.esem[op.eng], 1)
            return f

        with nc.Block() as block:
            block.tensor(runner('pe'))
            block.scalar(runner('act'))
            block.vector(runner('dve'))
            block.gpsimd(runner('pool'))
            block.sync(runner('sp'))

    def close(self):
        self.stack.close()


class Pool_:
    def __init__(self, P, n, shape, dtype, name):
        self.tiles = [P.sb(shape, dtype, name) for _ in range(n)]
        self.bufs = [Buf('%s%d' % (name, i)) for i in range(n)]
        self.i = 0

    def get(self):
        k = self.i % len(self.tiles)
        self.i += 1
        assert not self.bufs[k].w or self.bufs[k].r, ('pool slot reused before its consumer was recorded', self.bufs[k].name)
        return self.tiles[k], self.bufs[k]


class PsumPool:
    def __init__(self, tiles, names):
        self.tiles = tiles
        self.bufs = [Buf(n) for n in names]
        self.i = 0

    def get(self):
        k = self.i % len(self.tiles)
        self.i += 1
        assert not self.bufs[k].w or self.bufs[k].r, ('psum slot reused before its consumer was recorded', self.bufs[k].name)
        return self.tiles[k], self.bufs[k]


class Builder:
    def __init__(self, n_layers=DEPTH, phases=None, dump=None):
        self.n_layers = n_layers
        self.phases = phases
        self.dump = dump
        nc = self.nc = bass.Bass("TRN2", target_bir_lowering=False)
        self.P = Prog(nc)
        self.inputs = {}
        self.scratch = {}
        self.dbufs = {}

    def dram_in(self, name, shape, dtype=F32):
        t = self.nc.dram_tensor(name, list(shape), dtype, kind="ExternalInput").ap()
        self.inputs[name] = t
        return t

    def dram_scratch(self, name, shape, dtype):
        t = self.nc.dram_tensor(name, list(shape), dtype).ap()
        self.scratch[name] = t
        return t

    def xb(self, name, b):
        return [self.dbuf(name, b, c) for c in range(8)]

    def dbuf(self, name, *idx):
        key = (name,) + idx
        b = self.dbufs.get(key)
        if b is None:
            b = self.dbufs[key] = Buf(str(key))
        return b

    def mm(self, out_ap, pairs, reads, writes):
        def fn(e):
            n = len(pairs)
            ins = None
            for i, (l, r) in enumerate(pairs):
                ins = e.matmul(out_ap, lhsT=l, rhs=r, start=(i == 0), stop=(i == n - 1))
            return ins
        self.P.add('pe', fn, reads=reads, writes=writes)

    def dma(self, q, out_ap, in_ap, reads, writes):
        self.P.add(q, lambda e: e.dma_start(out=out_ap, in_=in_ap), reads=reads, writes=writes, dma=True)

    def act(self, out_ap, in_ap, func, reads, writes, scale=1.0, bias=None):
        if bias is None:
            self.P.add('act', lambda e: e.activation(out=out_ap, in_=in_ap, func=func, scale=scale), reads=reads, writes=writes)
        else:
            self.P.add('act', lambda e: e.activation(out=out_ap, in_=in_ap, func=func, scale=scale, bias=bias), reads=reads, writes=writes)

    def tt(self, eng, out_ap, a, b, op, reads, writes):
        self.P.add(eng, lambda e: e.tensor_tensor(out=out_ap, in0=a, in1=b, op=op), reads=reads, writes=writes)

    def ts(self, eng, out_ap, a, s1, s2, op0, op1, reads, writes):
        if op1 is None:
            self.P.add(eng, lambda e: e.tensor_scalar(out=out_ap, in0=a, scalar1=s1, scalar2=None, op0=op0), reads=reads, writes=writes)
        else:
            self.P.add(eng, lambda e: e.tensor_scalar(out=out_ap, in0=a, scalar1=s1, scalar2=s2, op0=op0, op1=op1), reads=reads, writes=writes)

    def stt(self, out_ap, a, s, b, op0, op1, reads, writes):
        self.P.add('dve', lambda e: e.scalar_tensor_tensor(out=out_ap, in0=a, scalar=s, in1=b, op0=op0, op1=op1), reads=reads, writes=writes)

    def copy(self, eng, out_ap, in_ap, reads, writes):
        if eng == 'act':
            self.P.add('act', lambda e: e.activation(out=out_ap, in_=in_ap, func=AF.Copy), reads=reads, writes=writes)
        else:
            self.P.add(eng, lambda e: e.tensor_copy(out=out_ap, in_=in_ap), reads=reads, writes=writes)

    def memset(self, eng, ap, val, writes):
        self.P.add(eng, lambda e: e.memset(ap, val), writes=writes)

    IN_SHAPES = {
        'xT': ([D, L], F32), 'pos': ([1, L], I32), 'gains': ([128, DEPTH * 48], F32),
        'ffn_w_gate': ([DEPTH, 2, D, DFF], F32), 'ffn_w_up': ([DEPTH, 2, D, DFF], F32), 'ffn_w_down': ([DEPTH, 2, DFF, D], F32),
        'w_in': ([DEPTH, D, INW], F32), 'ret_w_o': ([DEPTH, 512, D], F32),
        's5_are': ([DEPTH, 128, 16], F32), 's5_aim': ([DEPTH, 128, 16], F32), 's5_ldt': ([DEPTH, 128, 16], F32),
        's5_bre': ([DEPTH, 128, 16, 16], F32), 's5_bim': ([DEPTH, 128, 16, 16], F32),
        's5_cre': ([DEPTH, 128, 16, 16], F32), 's5_cim': ([DEPTH, 128, 16, 16], F32), 's5_d': ([DEPTH, 128, 4], F32),
        's5_glu_a': ([DEPTH, 512, D], F32), 's5_glu_b': ([DEPTH, 512, D], F32),
        'mla_q_norm': ([DEPTH, 128, 2], F32), 'mla_kv_norm': ([DEPTH, 128, 1], F32),
        'mla_w_uq': ([DEPTH, 256, 768], F32), 'mla_w_ukv': ([DEPTH, 128, 1024], F32), 'mla_w_o': ([DEPTH, 512, D], F32),
        'w_out': ([DEPTH, D, D], F32),
        'c_ident': ([128, 128], F32), 'c_tri': ([128, 128], F32), 'c_dec': ([128, 4, 128], F32), 'c_qdec': ([128, 4, 128], F32),
        'c_kdec': ([128, 4], F32), 'c_freq': ([128, 2], F32), 'c_sgn': ([128, 2], F32), 'c_step': ([128, N2], F32),
    }

    def I(self, name):
        t = self.inputs.get(name)
        if t is None:
            shape, dt = self.IN_SHAPES[name]
            shape = list(shape)
            if shape[0] == DEPTH and len(shape) >= 3:
                shape[0] = self.n_layers
            t = self.dram_in(name, shape, dt)
        return t

    def declare(self):
        self.out = self.nc.dram_tensor('outT', [D, L], F32, kind="ExternalOutput").ap()
        ds = self.dram_scratch
        self.X = ds('X', [D, L], F32)
        self.CR = ds('CR', [128, L], F32)
        self.SR = ds('SR', [128, L], F32)
        self.CM = ds('CM', [64, L], F32)
        self.SM = ds('SM', [64, L], F32)
        self.QR = ds('QR', [512, L], BF16)
        self.KR = ds('KR', [512, L], BF16)
        self.VR = ds('VR', [L, 512], BF16)
        self.SG = ds('SG', [512, L], BF16)
        self.U = ds('U', [512, L], BF16)
        self.QN = ds('QN', [512, L], BF16)
        self.QP = ds('QP', [256, L], BF16)
        self.KN = ds('KN', [512, L], BF16)
        self.KP = ds('KP', [64, L], BF16)
        self.VM = ds('VM', [L, 512], BF16)
        self.GT = ds('GT', [3072, L], BF16)
        self.MR = ds('MR', [D, L], BF16)
        self.MS = ds('MS', [D, L], BF16)
        if self.dump:
            self.dbg = self.nc.dram_tensor('dbg', list(self.dump[1]), self.dump[2], kind="ExternalOutput").ap()

    def consts(self):
        P = self.P
        P.sb_reset(0)
        self.ones_bf = P.sb([128, 128], BF16, 'ones')
        self.b_ones = Buf('ones')
        self.G = P.sb([128, DEPTH * 48], F32, 'G')
        self.GH = P.sb([128, DEPTH * 48], F32, 'GH')
        self.b_G = Buf('G')
        self.ident = P.sb([128, 128], F32, 'ident')
        self.b_ident = Buf('ident')
        self.memset('dve', self.ones_bf, 1.0, [self.b_ones])
        self.dma('sp', self.G, self.I('gains'), [], [self.b_G])
        self.dma('sp', self.ident, self.I('c_ident'), [], [self.b_ident])
        self.ts('dve', self.GH, self.G, 0.5, None, ALU.mult, None, [self.b_G], [self.b_G])
        self.const_end = P.sb_off
        self.psum = [self.nc.alloc_psum_tensor('ps%d' % i, [128, 512], F32)[:, :] for i in range(8)]

    def gcol(self, l, n, c):
        k = (l * 6 + n) * 8 + c
        return self.G[:, k:k + 1]

    def ghcol(self, l, n, c):
        k = (l * 6 + n) * 8 + c
        return self.GH[:, k:k + 1]

    def load_weight_rows(self, dst, src, nrows_chunks, ncols, bufs):
        maxc = 2048
        nsplit = (ncols + maxc - 1) // maxc
        w = (ncols + nsplit - 1) // nsplit
        for c in range(nrows_chunks):
            for s in range(nsplit):
                c0 = s * w
                c1 = min(ncols, c0 + w)
                self.dma('pool', dst[:, c, c0:c1], src[c * 128:(c + 1) * 128, c0:c1], [], [bufs[c][s]])

    def wbufs(self, n, ncols):
        nsplit = (ncols + 2047) // 2048
        return [[Buf() for _ in range(nsplit)] for _ in range(n)]

    @staticmethod
    def flat(bl):
        return [b for row in bl for b in row]

    def rms_stats(self, chunks, bxs, width, ps_stat, b_ps, sqpool, inv_n, eps_ap, rstd, b_rstd, tmp, b_tmp, b_eps):
        n = len(chunks)
        for c in range(n):
            sq, bsq = sqpool.get()
            self.act(sq[:, 0:width], chunks[c], AF.Square, [bxs[c]], [bsq])
            self.P.add('pe', (lambda sq, c: lambda e: e.matmul(ps_stat[:, 0:width], lhsT=self.ones_bf, rhs=sq[:, 0:width], start=(c == 0), stop=(c == n - 1)))(sq, c),
                       reads=[bsq, self.b_ones], writes=[b_ps])
        self.act(tmp[:, 0:width], ps_stat[:, 0:width], AF.Sqrt, [b_ps, b_eps], [b_tmp], scale=inv_n, bias=eps_ap)
        self.P.add('dve', lambda e: e.reciprocal(out=rstd[:, 0:width], in_=tmp[:, 0:width]), reads=[b_tmp], writes=[b_rstd])

    def ffn_phase(self, l, j, src, srcname, dst, dstname):
        P = self.P
        P.barrier()
        P.sb_reset(self.const_end)
        FT = 256
        NBF = L // FT
        n_pre = 0 if j == 0 else 4
        n_post = 1 if j == 0 else 5
        Wg = P.sb([128, 8, DFF], BF16, 'Wg')
        Wu = P.sb([128, 8, DFF], BF16, 'Wu')
        Wd = P.sb([128, NFF, D], BF16, 'Wd')
        bWg = self.wbufs(8, DFF)
        bWu = self.wbufs(8, DFF)
        bWd = self.wbufs(NFF, D)
        xts = [P.sb([128, 8, FT], F32, 'x') for _ in range(2)]
        bxs = [Buf('x0'), Buf('x1')]
        hs = [P.sb([128, 8, FT], BF16, 'h') for _ in range(2)]
        bhs = [[Buf('h%d_%d' % (i, c)) for c in range(8)] for i in range(2)]
        actb = P.sb([128, NFF, FT], BF16, 'act')
        bact = [Buf('act%d' % f) for f in range(NFF)]
        y = P.sb([128, 8, FT], F32, 'y')
        by = [Buf('y%d' % c) for c in range(8)]
        sqpool = Pool_(P, 4, [128, FT], BF16, 'sq')
        silp = Pool_(P, 4, [128, FT], BF16, 'sil')
        rstd = P.sb([128, FT], F32, 'rstd')
        b_rstd = Buf('rstd')
        rstd2 = P.sb([128, FT], F32, 'rstd2')
        b_rstd2 = Buf('rstd2')
        tmp = P.sb([128, FT], F32, 'tmp')
        b_tmp = Buf('tmp')
        eps = P.sb([128, 1], F32, 'eps')
        b_eps = Buf('eps')
        self.memset('dve', eps, NORM_EPS, [b_eps])
        ps = self.psum
        pg = PsumPool(ps[0:2], ['pg0', 'pg1'])
        pu = PsumPool(ps[2:4], ['pu0', 'pu1'])
        pd = PsumPool(ps[4:6], ['pd0', 'pd1'])
        ps_st, b_st = ps[6], Buf('pst')
        ps_yst, b_yst = ps[7], Buf('pyst')

        self.load_weight_rows(Wg, self.I('ffn_w_gate')[l, j], 8, DFF, bWg)
        self.load_weight_rows(Wu, self.I('ffn_w_up')[l, j], 8, DFF, bWu)
        self.load_weight_rows(Wd, self.I('ffn_w_down')[l, j], NFF, D, bWd)
        rWg, rWu, rWd = self.flat(bWg), self.flat(bWu), self.flat(bWd)

        srcv = src.rearrange("(c p) t -> p c t", p=128)
        dstv = dst.rearrange("(c p) t -> p c t", p=128)

        def stage_a(b):
            xt, bx, h, bh = xts[b % 2], bxs[b % 2], hs[b % 2], bhs[b % 2]
            self.dma('sp', xt, srcv[:, :, b * FT:(b + 1) * FT], self.xb(srcname, b * FT // TB), [bx])
            self.rms_stats([xt[:, c, :] for c in range(8)], [bx] * 8, FT, ps_st, b_st, sqpool, 1.0 / D, eps[:, 0:1], rstd, b_rstd, tmp, b_tmp, b_eps)
            for c in range(8):
                self.stt(h[:, c, :], xt[:, c, :], self.gcol(l, n_pre, c), rstd, ALU.mult, ALU.mult, [bx, b_rstd, self.b_G], [bh[c]])

        def stage_b(b):
            h, bh = hs[b % 2], bhs[b % 2]
            for f in range(NFF):
                g_ps, bg = pg.get()
                u_ps, bu = pu.get()
                self.mm(g_ps[:, 0:FT], [(Wg[:, k, f * 128:(f + 1) * 128], h[:, k, :]) for k in range(8)], bh + rWg, [bg])
                self.mm(u_ps[:, 0:FT], [(Wu[:, k, f * 128:(f + 1) * 128], h[:, k, :]) for k in range(8)], bh + rWu, [bu])
                sl, bsl = silp.get()
                self.act(sl, g_ps[:, 0:FT], AF.Silu, [bg], [bsl])
                self.tt('dve', actb[:, f, :], sl, u_ps[:, 0:FT], ALU.mult, [bsl, bu], [bact[f]])

        def stage_c(b):
            pend = []
            for c in range(8):
                d_ps, bd = pd.get()
                self.mm(d_ps[:, 0:FT], [(Wd[:, f, c * 128:(c + 1) * 128], actb[:, f, :]) for f in range(NFF)], bact + rWd, [bd])
                self.copy('dve', y[:, c, :], d_ps[:, 0:FT], [bd], [by[c]])
                sq, bsq = sqpool.get()
                self.act(sq, y[:, c, :], AF.Square, [by[c]], [bsq])
                pend.append((sq, bsq, c))
                if len(pend) > 2:
                    sq_, bsq_, c_ = pend.pop(0)
                    self.P.add('pe', (lambda sq, c: lambda e: e.matmul(ps_yst[:, 0:FT], lhsT=self.ones_bf, rhs=sq, start=(c == 0), stop=(c == 7)))(sq_, c_),
                               reads=[bsq_, self.b_ones], writes=[b_yst])
            for sq_, bsq_, c_ in pend:
                self.P.add('pe', (lambda sq, c: lambda e: e.matmul(ps_yst[:, 0:FT], lhsT=self.ones_bf, rhs=sq, start=(c == 0), stop=(c == 7)))(sq_, c_),
                           reads=[bsq_, self.b_ones], writes=[b_yst])

        def stage_d(b):
            xt, bx = xts[b % 2], bxs[b % 2]
            self.act(tmp, ps_yst[:, 0:FT], AF.Sqrt, [b_yst, b_eps], [b_tmp], scale=1.0 / D, bias=eps[:, 0:1])
            self.P.add('dve', lambda e: e.reciprocal(out=rstd2, in_=tmp), reads=[b_tmp], writes=[b_rstd2])
            for c in range(8):
                self.tt('dve', y[:, c, :], y[:, c, :], rstd2, ALU.mult, [by[c], b_rstd2], [by[c]])
                self.stt(xt[:, c, :], y[:, c, :], self.ghcol(l, n_post, c), xt[:, c, :], ALU.mult, ALU.add, [by[c], bx, self.b_G], [bx])
            self.dma('sp', dstv[:, :, b * FT:(b + 1) * FT], xt, [bx], self.xb(dstname, b * FT // TB))

        import os
        dbg_st = os.environ.get('FFN_STAGES', 'abcd')
        dbg_nb = int(os.environ.get('FFN_NB', NBF))
        stage_a(0)
        for b in range(dbg_nb):
            if 'b' in dbg_st:
                stage_b(b)
            if b + 1 < dbg_nb:
                stage_a(b + 1)
            if 'c' in dbg_st:
                stage_c(b)
            if 'd' in dbg_st:
                stage_d(b)

    def rope_tables(self):
        P = self.P
        P.barrier()
        P.sb_reset(self.const_end)
        posi = P.sb([128, L], I32, 'posi')
        posf = P.sb([128, L], F32, 'posf')
        ang = P.sb([128, L], F32, 'ang')
        kk = P.sb([128, L], F32, 'kk')
        res = P.sb([128, L], F32, 'res')
        fr = P.sb([128, 2], F32, 'fr')
        sg = P.sb([128, 2], F32, 'sg')
        b_pos, b_ang, b_kk, b_res, b_c = Buf(), Buf(), Buf(), Buf(), Buf()
        self.dma('sp', posi, self.I('pos').partition_broadcast(128), [], [b_pos])
        self.dma('sp', fr, self.I('c_freq'), [], [b_c])
        self.dma('sp', sg, self.I('c_sgn'), [], [b_c])
        self.copy('dve', posf, posi, [b_pos], [b_pos])
        MAG = 12582912.0
        C1 = 6.28125
        C2 = 2.0 * math.pi - 6.28125
        for col, rows, dc, dsn in ((0, 128, self.CR, self.SR), (1, 64, self.CM, self.SM)):
            for is_cos in (False, True):
                a = ang[0:rows]
                k = kk[0:rows]
                r = res[0:rows]
                self.ts('dve', a, posf[0:rows], fr[0:rows, col:col + 1], (math.pi / 2 if is_cos else 0.0), ALU.mult, ALU.add, [b_pos, b_c], [b_ang])
                self.ts('dve', k, a, 1.0 / (2.0 * math.pi), MAG, ALU.mult, ALU.add, [b_ang], [b_kk])
                self.ts('dve', k, k, -MAG, None, ALU.add, None, [b_kk], [b_kk])
                self.stt(a, k, -C1, a, ALU.mult, ALU.add, [b_kk, b_ang], [b_ang])
                self.stt(a, k, -C2, a, ALU.mult, ALU.add, [b_kk, b_ang], [b_ang])
                self.ts('dve', a, a, 3.141592, -3.141592, ALU.min, ALU.max, [b_ang], [b_ang])
                self.act(r, a, AF.Sin, [b_ang], [b_res])
                if not is_cos:
                    self.ts('dve', r, r, sg[0:rows, col:col + 1], None, ALU.mult, None, [b_res, b_c], [b_res])
                self.dma('sp', dc if is_cos else dsn, r, [b_res], [self.dbuf('ropetab')])

    def rope_evac(self, ps, rows, Ct, St, b_tab, bps, scale, out_ap, b_out, t1p, t2p):
        hf = rows // 2
        t1, bt1 = t1p.get()
        t2, bt2 = t2p.get()
        self.stt(t1[0:rows], ps[0:rows], scale, Ct[0:rows], ALU.mult, ALU.mult, [bps, b_tab], [bt1])
        self.stt(t2[0:hf], ps[hf:rows], scale, St[0:hf], ALU.mult, ALU.mult, [bps, b_tab], [bt2])
        self.stt(t2[hf:rows], ps[0:hf], scale, St[hf:rows], ALU.mult, ALU.mult, [bps, b_tab, bt2], [bt2])
        self.tt('pool', out_ap, t1[0:rows], t2[0:rows], ALU.add, [bt1, bt2], [b_out])

    def b1_phase(self, l):
        P = self.P
        P.barrier()
        P.sb_reset(self.const_end)
        Win = P.sb([128, 8, INW], BF16, 'Win')
        Wuq = P.sb([128, 2, 768], BF16, 'Wuq')
        Wukv = P.sb([128, 1, 1024], BF16, 'Wukv')
        bWin = self.wbufs(8, INW)
        bWuq = self.wbufs(2, 768)
        bWukv = self.wbufs(1, 1024)
        gq = P.sb([128, 2], F32, 'gq')
        gkv = P.sb([128, 1], F32, 'gkv')
        eps = P.sb([128, 1], F32, 'eps')
        b_small = Buf('small')
        self.memset('dve', eps, NORM_EPS, [b_small])
        self.dma('sp', gq, self.I('mla_q_norm')[l], [], [b_small])
        self.dma('sp', gkv, self.I('mla_kv_norm')[l], [], [b_small])
        xt = P.sb([128, 8, TB], F32, 'x')
        bx = Buf('x')
        h = P.sb([128, 8, TB], BF16, 'h')
        bh = [Buf('h%d' % c) for c in range(8)]
        CRt = P.sb([128, TB], F32, 'CRt')
        SRt = P.sb([128, TB], F32, 'SRt')
        CMt = P.sb([64, TB], F32, 'CMt')
        SMt = P.sb([64, TB], F32, 'SMt')
        b_tab = Buf('tab')
        stq = P.sb([128, 4, TB], BF16, 'stq'); b_stq = [Buf() for _ in range(4)]
        stk = P.sb([128, 4, TB], BF16, 'stk'); b_stk = [Buf() for _ in range(4)]
        stsg = P.sb([128, 4, TB], BF16, 'stsg'); b_stsg = [Buf() for _ in range(4)]
        stu = P.sb([128, 4, TB], BF16, 'stu'); b_stu = [Buf() for _ in range(4)]
        stv = P.sb([128, 4, 512], BF16, 'stv'); b_stv = [Buf() for _ in range(4)]
        stqn = P.sb([128, 4, TB], BF16, 'stqn'); b_stqn = [Buf() for _ in range(4)]
        stqp = P.sb([64, 4, TB], BF16, 'stqp'); b_stqp = [Buf() for _ in range(4)]
        stkn = P.sb([128, 4, TB], BF16, 'stkn'); b_stkn = [Buf() for _ in range(4)]
        stvm = P.sb([128, 4, 512], BF16, 'stvm'); b_stvm = [Buf() for _ in range(4)]
        stkp = P.sb([64, TB], BF16, 'stkp'); b_stkp = Buf()
        gtp = Pool_(P, 4, [128, TB], BF16, 'gt')
        cq = P.sb([128, 2, TB], F32, 'cq'); b_cq = [Buf(), Buf()]
        cqn = P.sb([128, 2, TB], BF16, 'cqn'); b_cqn = [Buf(), Buf()]
        ckv = P.sb([128, TB], F32, 'ckv'); b_ckv = Buf()
        ckvn = P.sb([128, TB], BF16, 'ckvn'); b_ckvn = Buf()
        t1p = Pool_(P, 2, [128, TB], F32, 't1')
        t2p = Pool_(P, 2, [128, TB], F32, 't2')
        sqpool = Pool_(P, 4, [128, TB], BF16, 'sq')
        rstd = P.sb([128, TB], F32, 'rstd'); b_rstd = Buf()
        tmp = P.sb([128, TB], F32, 'tmp'); b_tmp = Buf()
        ps = self.psum
        pp = PsumPool(ps[0:6], ['pp%d' % i for i in range(6)])
        ps_st, b_st = ps[6], Buf('pst')
        ps_st2, b_st2 = ps[7], Buf('pst2')

        self.load_weight_rows(Win, self.I('w_in')[l], 8, INW, bWin)
        self.load_weight_rows(Wuq, self.I('mla_w_uq')[l], 2, 768, bWuq)
        self.load_weight_rows(Wukv, self.I('mla_w_ukv')[l], 1, 1024, bWukv)
        rWin, rWuq, rWukv = self.flat(bWin), self.flat(bWuq), self.flat(bWukv)
        Xv = self.X.rearrange("(c p) t -> p c t", p=128)
        RSC = 128 ** -0.5
        MSC = 192 ** -0.5

        def proj(col0, ncols):
            pt, bp = pp.get()
            self.mm(pt[0:ncols, :], [(Win[:, k, col0:col0 + ncols], h[:, k, :]) for k in range(8)], bh + rWin, [bp])
            return pt, bp

        for b in range(NB):
            sl = slice(b * TB, (b + 1) * TB)
            self.dma('sp', xt, Xv[:, :, sl], self.xb('X', b), [bx])
            self.dma('sp', CRt, self.CR[:, sl], [self.dbuf('ropetab')], [b_tab])
            self.dma('sp', SRt, self.SR[:, sl], [self.dbuf('ropetab')], [b_tab])
            self.dma('sp', CMt, self.CM[:, sl], [self.dbuf('ropetab')], [b_tab])
            self.dma('sp', SMt, self.SM[:, sl], [self.dbuf('ropetab')], [b_tab])
            self.rms_stats([xt[:, c, :] for c in range(8)], [bx] * 8, TB, ps_st, b_st, sqpool, 1.0 / D, eps[:, 0:1], rstd, b_rstd, tmp, b_tmp, b_small)
            for c in range(8):
                self.stt(h[:, c, :], xt[:, c, :], self.gcol(l, 2, c), rstd, ALU.mult, ALU.mult, [bx, b_rstd, self.b_G], [bh[c]])
            for hh in range(4):
                pt, bp = proj(hh * 128, 128)
                self.rope_evac(pt, 128, CRt, SRt, b_tab, bp, 1.0, stq[:, hh, :], b_stq[hh], t1p, t2p)
                pt, bp = proj(512 + hh * 128, 128)
                self.rope_evac(pt, 128, CRt, SRt, b_tab, bp, RSC, stk[:, hh, :], b_stk[hh], t1p, t2p)
            self.dma('sp', self.QR.rearrange("(h p) t -> p h t", p=128)[:, :, sl], stq, b_stq, [self.dbuf('QR', b)])
            self.dma('sp', self.KR.rearrange("(h p) t -> p h t", p=128)[:, :, sl], stk, b_stk, [self.dbuf('KR', b)])
            for tt_ in range(4):
                pt, bp = pp.get()
                self.mm(pt, [(h[:, k, tt_ * 128:(tt_ + 1) * 128], Win[:, k, 1024:1536]) for k in range(8)], bh + rWin, [bp])
                self.copy('act', stv[:, tt_, :], pt, [bp], [b_stv[tt_]])
            self.dma('sp', self.VR[sl, :].rearrange("(t p) c -> p t c", p=128), stv, b_stv, [self.dbuf('VR', b)])
            for hh in range(4):
                pt, bp = proj(1536 + hh * 128, 128)
                self.act(stsg[:, hh, :], pt, AF.Silu, [bp], [b_stsg[hh]])
            self.dma('sp', self.SG.rearrange("(h p) t -> p h t", p=128)[:, :, sl], stsg, b_stsg, [self.dbuf('SG', b)])
            for hh in range(4):
                pt, bp = proj(2048 + hh * 128, 128)
                self.copy('act', stu[:, hh, :], pt, [bp], [b_stu[hh]])
            self.dma('sp', self.U.rearrange("(h p) t -> p h t", p=128)[:, :, sl], stu, b_stu, [self.dbuf('U', b)])
            for c2 in range(2):
                pt, bp = proj(2560 + c2 * 128, 128)
                self.copy('dve', cq[:, c2, :], pt, [bp], [b_cq[c2]])
            self.rms_stats([cq[:, 0, :], cq[:, 1, :]], b_cq, TB, ps_st2, b_st2, sqpool, 1.0 / 256, eps[:, 0:1], rstd, b_rstd, tmp, b_tmp, b_small)
            for c2 in range(2):
                self.stt(cqn[:, c2, :], cq[:, c2, :], gq[:, c2:c2 + 1], rstd, ALU.mult, ALU.mult, [b_cq[c2], b_rstd, b_small], [b_cqn[c2]])
            for hh in range(4):
                pt, bp = pp.get()
                self.mm(pt, [(Wuq[:, k2, hh * 192:hh * 192 + 128], cqn[:, k2, :]) for k2 in range(2)], b_cqn + rWuq, [bp])
                self.act(stqn[:, hh, :], pt, AF.Copy, [bp], [b_stqn[hh]], scale=MSC)
                pt, bp = pp.get()
                self.mm(pt[0:64, :], [(Wuq[:, k2, hh * 192 + 128:hh * 192 + 192], cqn[:, k2, :]) for k2 in range(2)], b_cqn + rWuq, [bp])
                self.rope_evac(pt, 64, CMt, SMt, b_tab, bp, MSC, stqp[:, hh, :], b_stqp[hh], t1p, t2p)
            self.dma('sp', self.QN.rearrange("(h p) t -> p h t", p=128)[:, :, sl], stqn, b_stqn, [self.dbuf('QN', b)])
            self.dma('sp', self.QP.rearrange("(h p) t -> p h t", p=64)[:, :, sl], stqp, b_stqp, [self.dbuf('QP', b)])
            pt, bp = proj(2816, 128)
            self.copy('dve', ckv, pt, [bp], [b_ckv])
            self.rms_stats([ckv], [b_ckv], TB, ps_st2, b_st2, sqpool, 1.0 / 128, eps[:, 0:1], rstd, b_rstd, tmp, b_tmp, b_small)
            self.stt(ckvn, ckv, gkv[:, 0:1], rstd, ALU.mult, ALU.mult, [b_ckv, b_rstd, b_small], [b_ckvn])
            for hh in range(4):
                pt, bp = pp.get()
                self.mm(pt, [(Wukv[:, 0, hh * 256:hh * 256 + 128], ckvn)], [b_ckvn] + rWukv, [bp])
                self.copy('act', stkn[:, hh, :], pt, [bp], [b_stkn[hh]])
            self.dma('sp', self.KN.rearrange("(h p) t -> p h t", p=128)[:, :, sl], stkn, b_stkn, [self.dbuf('KN', b)])
            wv = Wukv[:, 0, :].rearrange("p (h c) -> p h c", h=4)[:, :, 128:256]
            for tt_ in range(4):
                pt, bp = pp.get()
                self.mm(pt.rearrange("p (h c) -> p h c", h=4), [(ckvn[:, tt_ * 128:(tt_ + 1) * 128], wv)], [b_ckvn] + rWukv, [bp])
                self.copy('act', stvm[:, tt_, :], pt, [bp], [b_stvm[tt_]])
            self.dma('sp', self.VM[sl, :].rearrange("(t p) c -> p t c", p=128), stvm, b_stvm, [self.dbuf('VM', b)])
            pt, bp = proj(2944, 64)
            self.rope_evac(pt, 64, CMt, SMt, b_tab, bp, 1.0, stkp, b_stkp, t1p, t2p)
            self.dma('sp', self.KP[:, sl], stkp, [b_stkp], [self.dbuf('KP', b)])
            for jg in range(24):
                pt, bp = proj(3008 + jg * 128, 128)
                gt_, bgt = gtp.get()
                self.act(gt_, pt, AF.Sigmoid, [bp], [bgt])
                self.dma('sp', self.GT[jg * 128:(jg + 1) * 128, sl], gt_, [bgt], [self.dbuf('GT', b, jg)])

    def r_phase(self, l):
        P = self.P
        P.barrier()
        P.sb_reset(self.const_end)
        Wo = P.sb([128, 4, D], BF16, 'Wo')
        bWo = self.wbufs(4, D)
        self.load_weight_rows(Wo, self.I('ret_w_o')[l], 4, D, bWo)
        rWo = self.flat(bWo)
        DEC = P.sb([128, 4, 128], F32, 'DEC')
        QDEC = P.sb([128, 4, 128], F32, 'QDEC')
        KDEC = P.sb([128, 4], F32, 'KDEC')
        identb = P.sb([128, 128], BF16, 'identb')
        onesf = P.sb([128, 128], F32, 'onesf')
        epsg = P.sb([128, 1], F32, 'epsg')
        b_c = Buf('rc')
        self.dma('sp', DEC, self.I('c_dec'), [], [b_c])
        self.dma('sp', QDEC, self.I('c_qdec'), [], [b_c])
        self.dma('sp', KDEC, self.I('c_kdec'), [], [b_c])
        self.copy('dve', identb, self.ident, [self.b_ident], [b_c])
        self.memset('dve', onesf, 1.0 / 128, [b_c])
        self.memset('dve', epsg, GN_EPS, [b_c])
        S = P.sb([128, 4, 128], F32, 'S')
        Sb = P.sb([128, 4, 128], BF16, 'Sb')
        bS = [Buf('S%d' % i) for i in range(4)]
        bSb = [Buf('Sb%d' % i) for i in range(4)]
        for hh in range(4):
            self.memset('dve', S[:, hh, :], 0.0, [bS[hh]])
            self.memset('pool', Sb[:, hh, :], 0.0, [bSb[hh]])
        qt = P.sb([128, 4, TB], BF16, 'qt'); b_qt = Buf()
        kt = P.sb([128, 4, TB], BF16, 'kt'); b_kt = Buf()
        vt = P.sb([128, 4, 512], BF16, 'vt'); b_vt = Buf()
        sgt = P.sb([128, 4, TB], BF16, 'sgt'); b_sgt = Buf()
        GO = P.sb([128, 4, TB], BF16, 'GO'); bGO = [Buf() for _ in range(4)]
        ptp = Pool_(P, 8, [128, 128], BF16, 'PT')
        kdp = Pool_(P, 8, [128, 128], BF16, 'kd')
        qdp = Pool_(P, 8, [128, 128], BF16, 'qd')
        osb = [P.sb([128, TB], F32, 'osb') for _ in range(4)]; b_osb = [Buf() for _ in range(4)]
        osq = [P.sb([128, TB], F32, 'osq') for _ in range(4)]; b_osq = [Buf() for _ in range(4)]
        mean = [P.sb([128, TB], F32, 'mean') for _ in range(4)]; b_mean = [Buf() for _ in range(4)]
        var = [P.sb([128, TB], F32, 'var') for _ in range(4)]; b_var = [Buf() for _ in range(4)]
        rstd = [P.sb([128, TB], F32, 'rstd') for _ in range(4)]; b_rstd = [Buf() for _ in range(4)]
        gp = Pool_(P, 3, [128, TB], BF16, 'g0')
        mp = Pool_(P, 3, [128, TB], BF16, 'mr')
        ps = self.psum
        psc = PsumPool(ps[0:1], ['sc0'])
        pkt = PsumPool(ps[1:2], ['kt0'])
        psn = PsumPool(ps[2:3], ['sn'])
        pOs = [(ps[3 + i], Buf('O%d' % i)) for i in range(4)]
        pst = PsumPool([ps[7], ps[0], ps[1], ps[2]], ['st7', 'sc0', 'kt0', 'sn'])
        pst.bufs = [Buf('st7'), psc.bufs[0], pkt.bufs[0], psn.bufs[0]]
        G128 = [float(np.exp(128.0 * np.log1p(-np.exp2(-5.0 - hh)))) for hh in range(4)]

        for b in range(NB):
            sl = slice(b * TB, (b + 1) * TB)
            self.dma('sp', qt, self.QR.rearrange("(h p) t -> p h t", p=128)[:, :, sl], [self.dbuf('QR', b)], [b_qt])
            self.dma('sp', kt, self.KR.rearrange("(h p) t -> p h t", p=128)[:, :, sl], [self.dbuf('KR', b)], [b_kt])
            self.dma('sp', vt, self.VR[sl, :].rearrange("(t p) c -> p t c", p=128), [self.dbuf('VR', b)], [b_vt])
            self.dma('sp', sgt, self.SG.rearrange("(h p) t -> p h t", p=128)[:, :, sl], [self.dbuf('SG', b)], [b_sgt])
            def front(n, hh):
                cs = slice(n * 128, (n + 1) * 128)
                sc, bsc = psc.get()
                self.mm(sc[:, 0:128], [(kt[:, hh, cs], qt[:, hh, cs])], [b_kt, b_qt], [bsc])
                PT, bPT = ptp.get()
                self.tt('dve', PT, sc[:, 0:128], DEC[:, hh, :], ALU.mult, [bsc, b_c], [bPT])
                ktp, bktp = pkt.get()
                self.mm(ktp[:, 0:128], [(kt[:, hh, cs], identb)], [b_kt, b_c], [bktp])
                kd, bkd = kdp.get()
                self.act(kd, ktp[:, 0:128], AF.Copy, [bktp, b_c], [bkd], scale=KDEC[:, hh:hh + 1])
                qd, bqd = qdp.get()
                self.tt('pool', qd, qt[:, hh, cs], QDEC[:, hh, :], ALU.mult, [b_qt, b_c], [bqd])
                return (n, hh, PT, bPT, kd, bkd, qd, bqd)

            def back(fr):
                n, hh, PT, bPT, kd, bkd, qd, bqd = fr
                cs = slice(n * 128, (n + 1) * 128)
                O_ps, bO = pOs[hh]
                vs = vt[:, n, hh * 128:(hh + 1) * 128]
                self.mm(O_ps[:, cs], [(vs, PT), (Sb[:, hh, :], qd)], [b_vt, bPT, bSb[hh], bqd], [bO])
                sn, bsn = psn.get()
                self.mm(sn[:, 0:128], [(kd, vs)], [bkd, b_vt], [bsn])
                self.stt(S[:, hh, :], S[:, hh, :], G128[hh], sn[:, 0:128], ALU.mult, ALU.add, [bS[hh], bsn], [bS[hh]])
                self.copy('pool', Sb[:, hh, :], S[:, hh, :], [bS[hh]], [bSb[hh]])

            seq = [(n, hh) for n in range(4) for hh in range(4)]
            frs = [front(*seq[0]), front(*seq[1])]
            for i in range(len(seq)):
                if i + 2 < len(seq):
                    frs.append(front(*seq[i + 2]))
                back(frs.pop(0))
            H4 = range(4)
            for hh in H4:
                self.copy('act', osb[hh], pOs[hh][0], [pOs[hh][1]], [b_osb[hh]])
            for hh in H4:
                self.act(osq[hh], osb[hh], AF.Square, [b_osb[hh]], [b_osq[hh]])
            mpss, qpss = [], []
            for hh in H4:
                mps, bmps = pst.get()
                self.mm(mps, [(onesf, osb[hh])], [b_c, b_osb[hh]], [bmps])
                self.copy('act', mean[hh], mps, [bmps], [b_mean[hh]])
            for hh in H4:
                qps, bqps = pst.get()
                self.mm(qps, [(onesf, osq[hh])], [b_c, b_osq[hh]], [bqps])
                self.copy('act', var[hh], qps, [bqps], [b_var[hh]])
            for hh in H4:
                self.act(osq[hh], mean[hh], AF.Square, [b_mean[hh]], [b_osq[hh]])
            for hh in H4:
                self.tt('dve', var[hh], var[hh], osq[hh], ALU.subtract, [b_var[hh], b_osq[hh]], [b_var[hh]])
            for hh in H4:
                self.act(var[hh], var[hh], AF.Sqrt, [b_var[hh], b_c], [b_var[hh]], bias=epsg[:, 0:1])
            for hh in H4:
                self.P.add('dve', (lambda hh: lambda e: e.reciprocal(out=rstd[hh], in_=var[hh]))(hh), reads=[b_var[hh]], writes=[b_rstd[hh]])
            for hh in H4:
                self.tt('dve', osb[hh], osb[hh], mean[hh], ALU.subtract, [b_osb[hh], b_mean[hh]], [b_osb[hh]])
            for hh in H4:
                self.tt('pool', osb[hh], osb[hh], rstd[hh], ALU.mult, [b_osb[hh], b_rstd[hh]], [b_osb[hh]])
            for hh in H4:
                self.tt('pool', GO[:, hh, :], osb[hh], sgt[:, hh, :], ALU.mult, [b_osb[hh], b_sgt], [bGO[hh]])
            for c in range(8):
                yps, byps = pst.get()
                self.mm(yps, [(Wo[:, hh, c * 128:(c + 1) * 128], GO[:, hh, :]) for hh in range(4)], bGO + rWo, [byps])
                g0, bg0 = gp.get()
                self.dma('sp', g0, self.GT[c * 128:(c + 1) * 128, sl], [self.dbuf('GT', b, c)], [bg0])
                mr, bmr = mp.get()
                self.tt('dve', mr, yps, g0, ALU.mult, [byps, bg0], [bmr])
                self.dma('sp', self.MR[c * 128:(c + 1) * 128, sl], mr, [bmr], [self.dbuf('MR', b, c)])

    def m_phase(self, l):
        P = self.P
        P.barrier()
        P.sb_reset(self.const_end)
        KNs = P.sb([128, 4, L], BF16, 'KNs')
        KPs = P.sb([64, L], BF16, 'KPs')
        VMs = P.sb([128, 32, 512], BF16, 'VMs')
        bKV = [Buf('kv%d' % b) for b in range(NB)]
        Wmo = P.sb([128, 4, D], BF16, 'Wmo')
        Wout = P.sb([128, 8, D], BF16, 'Wout')
        bWmo = self.wbufs(4, D)
        bWout = self.wbufs(8, D)
        self.load_weight_rows(Wmo, self.I('mla_w_o')[l], 4, D, bWmo)
        self.load_weight_rows(Wout, self.I('w_out')[l], 8, D, bWout)
        rWmo, rWout = self.flat(bWmo), self.flat(bWout)
        trif = P.sb([128, 128], F32, 'trif')
        tri = P.sb([128, 128], BF16, 'tri')
        eps = P.sb([128, 1], F32, 'eps')
        b_c = Buf('mc')
        self.dma('sp', trif, self.I('c_tri'), [], [b_c])
        self.copy('dve', tri, trif, [b_c], [b_c])
        self.memset('dve', eps, NORM_EPS, [b_c])
        qn = P.sb([128, 4, TB], BF16, 'qn'); b_qn = Buf()
        qp = P.sb([64, 4, TB], BF16, 'qp'); b_qp = Buf()
        ptp = Pool_(P, 6, [128, TB], BF16, 'PT')
        omlas = [P.sb([128, 4, TB], BF16, 'omla') for _ in range(2)]; b_oms = [[Buf() for _ in range(4)] for _ in range(2)]
        rden = P.sb([128, TB], F32, 'rden'); b_rden = Buf()
        merged = P.sb([128, 8, TB], BF16, 'merged'); b_mg = [Buf() for _ in range(8)]
        g2p = Pool_(P, 3, [128, TB], BF16, 'g2')
        mrp = Pool_(P, 3, [128, TB], BF16, 'mrl')
        msp = Pool_(P, 3, [128, TB], BF16, 'msl')
        accp = Pool_(P, 2, [128, TB], F32, 'acc')
        y = P.sb([128, 8, TB], F32, 'y'); by = [Buf() for _ in range(8)]
        xcp = Pool_(P, 3, [128, TB], F32, 'xc')
        sqpool = Pool_(P, 4, [128, TB], BF16, 'sq')
        rstd = P.sb([128, TB], F32, 'rstd'); b_rstd = Buf()
        tmp = P.sb([128, TB], F32, 'tmp'); b_tmp = Buf()
        ps = self.psum
        pS = PsumPool(ps[0:2], ['S0', 'S1'])
        pO = PsumPool(ps[2:4], ['O0', 'O1'])
        pDn = PsumPool(ps[4:6], ['D0', 'D1'])
        pY = PsumPool(ps[6:7], ['Y0'])
        st_ps, b_st = ps[7], Buf('mst')

        def make_tail(b, omla, b_om):
            sl = slice(b * TB, (b + 1) * TB)
            steps = []

            def merge_step(c):
                yps, byps = pY.get()
                self.mm(yps, [(Wmo[:, hh, c * 128:(c + 1) * 128], omla[:, hh, :]) for hh in range(4)], b_om + rWmo, [byps])
                g2, bg2 = g2p.get()
                self.dma('sp', g2, self.GT[2048 + c * 128:2048 + (c + 1) * 128, sl], [self.dbuf('GT', b, 16 + c)], [bg2])
                mr, bmr = mrp.get()
                self.dma('sp', mr, self.MR[c * 128:(c + 1) * 128, sl], [self.dbuf('MR', b, c)], [bmr])
                ms, bms = msp.get()
                self.dma('sp', ms, self.MS[c * 128:(c + 1) * 128, sl], [self.dbuf('MS', b, c)], [bms])
                acc, bacc = accp.get()
                self.tt('dve', acc, yps, g2, ALU.mult, [byps, bg2], [bacc])
                self.tt('pool', acc, acc, mr, ALU.add, [bacc, bmr], [bacc])
                self.tt('pool', merged[:, c, :], acc, ms, ALU.add, [bacc, bms], [b_mg[c]])

            def wout_step(c2):
                yps, byps = pY.get()
                self.mm(yps, [(Wout[:, c, c2 * 128:(c2 + 1) * 128], merged[:, c, :]) for c in range(8)], b_mg + rWout, [byps])
                self.copy('dve', y[:, c2, :], yps, [byps], [by[c2]])
                sq, bsq = sqpool.get()
                self.act(sq, y[:, c2, :], AF.Square, [by[c2]], [bsq])
                self.P.add('pe', (lambda sq, c2: lambda e: e.matmul(st_ps, lhsT=self.ones_bf, rhs=sq, start=(c2 == 0), stop=(c2 == 7)))(sq, c2),
                           reads=[bsq, self.b_ones], writes=[b_st])

            def norm_step():
                self.act(tmp, st_ps, AF.Sqrt, [b_st, b_c], [b_tmp], scale=1.0 / D, bias=eps[:, 0:1])
                self.P.add('dve', lambda e: e.reciprocal(out=rstd, in_=tmp), reads=[b_tmp], writes=[b_rstd])

            def res_step(c2):
                xc, bxc = xcp.get()
                self.dma('sp', xc, self.X[c2 * 128:(c2 + 1) * 128, sl], [self.dbuf('X', b, c2)], [bxc])
                self.tt('dve', y[:, c2, :], y[:, c2, :], rstd, ALU.mult, [by[c2], b_rstd], [by[c2]])
                self.stt(xc, y[:, c2, :], self.gcol(l, 3, c2), xc, ALU.mult, ALU.add, [by[c2], bxc, self.b_G], [bxc])
                self.dma('sp', self.X[c2 * 128:(c2 + 1) * 128, sl], xc, [bxc], [self.dbuf('X', b, c2)])

            for c in range(8):
                steps.append((lambda c: lambda: merge_step(c))(c))
            for c2 in range(8):
                steps.append((lambda c2: lambda: wout_step(c2))(c2))
            steps.append(norm_step)
            for c2 in range(8):
                steps.append((lambda c2: lambda: res_step(c2))(c2))
            return steps

        pending_tail = []
        for b in range(NB):
            sl = slice(b * TB, (b + 1) * TB)
            self.dma('sp', KNs[:, :, sl], self.KN.rearrange("(h p) t -> p h t", p=128)[:, :, sl], [self.dbuf('KN', b)], [bKV[b]])
            self.dma('sp', KPs[:, sl], self.KP[:, sl], [self.dbuf('KP', b)], [bKV[b]])
            self.dma('sp', VMs[:, 4 * b:4 * b + 4, :], self.VM[sl, :].rearrange("(t p) c -> p t c", p=128), [self.dbuf('VM', b)], [bKV[b]])
            self.dma('sp', qn, self.QN.rearrange("(h p) t -> p h t", p=128)[:, :, sl], [self.dbuf('QN', b)], [b_qn])
            self.dma('sp', qp, self.QP.rearrange("(h p) t -> p h t", p=64)[:, :, sl], [self.dbuf('QP', b)], [b_qp])
            nkt = 4 * (b + 1)
            omla, b_om = omlas[b % 2], b_oms[b % 2]
            for hh in range(4):
                O_ps, bO = pO.get()
                Dn, bDn = pDn.get()
                def emit_S(kt, hh=hh):
                    c0 = 128 * (kt - 4 * b) if kt >= 4 * b else 0
                    ks = slice(kt * 128, (kt + 1) * 128)
                    kvb = bKV[kt // 4]
                    S_ps, bS_ = pS.get()
                    self.mm(S_ps[:, c0:TB], [(KNs[:, hh, ks], qn[:, hh, c0:TB]), (KPs[0:64, ks], qp[0:64, hh, c0:TB])], [kvb, b_qn, b_qp], [bS_])
                    return S_ps, bS_, c0, kvb
                nxt = emit_S(0)
                for kt in range(nkt):
                    S_ps, bS_, c0, kvb = nxt
                    if kt + 1 < nkt:
                        nxt = emit_S(kt + 1)
                    PT, bPT = ptp.get()
                    self.act(PT[:, c0:TB], S_ps[:, c0:TB], AF.Exp, [bS_], [bPT])
                    if kt >= 4 * b:
                        self.tt('pool', PT[:, c0:c0 + 128], PT[:, c0:c0 + 128], tri, ALU.mult, [bPT, b_c], [bPT])
                    first, lastk = (kt == 0), (kt == nkt - 1)
                    self.P.add('pe', (lambda O_ps, PT, c0, kt, hh, first, lastk: lambda e: e.matmul(
                        O_ps[:, c0:TB], lhsT=VMs[:, kt, hh * 128:(hh + 1) * 128], rhs=PT[:, c0:TB], start=first, stop=lastk))(O_ps, PT, c0, kt, hh, first, lastk),
                        reads=[kvb, bPT], writes=[bO])
                    self.P.add('pe', (lambda Dn, PT, c0, first, lastk: lambda e: e.matmul(
                        Dn[:, c0:TB], lhsT=self.ones_bf, rhs=PT[:, c0:TB], start=first, stop=lastk))(Dn, PT, c0, first, lastk),
                        reads=[self.b_ones, bPT], writes=[bDn])
                    if pending_tail:
                        pending_tail.pop(0)()
                self.P.add('dve', (lambda Dn: lambda e: e.reciprocal(out=rden, in_=Dn))(Dn), reads=[bDn], writes=[b_rden])
                self.tt('dve', omla[:, hh, :], O_ps, rden, ALU.mult, [bO, b_rden], [b_om[hh]])
            while pending_tail:
                pending_tail.pop(0)()
            pending_tail.extend(make_tail(b, omla, b_om))
        while pending_tail:
            pending_tail.pop(0)()

    def trig(self, ang, b_ang, n, out_sin, out_cos, b_out, wk):
        MAG = 12582912.0
        C1 = 6.28125
        C2 = 2.0 * math.pi - 6.28125
        a2, k, b_w = wk
        for dst, shift in ((out_sin, 0.0), (out_cos, math.pi / 2)):
            self.ts('dve', a2[:, 0:n], ang, shift, None, ALU.add, None, [b_ang], [b_w])
            self.ts('dve', k[:, 0:n], a2[:, 0:n], 1.0 / (2.0 * math.pi), MAG, ALU.mult, ALU.add, [b_w], [b_w])
            self.ts('dve', k[:, 0:n], k[:, 0:n], -MAG, None, ALU.add, None, [b_w], [b_w])
            self.stt(a2[:, 0:n], k[:, 0:n], -C1, a2[:, 0:n], ALU.mult, ALU.add, [b_w], [b_w])
            self.stt(a2[:, 0:n], k[:, 0:n], -C2, a2[:, 0:n], ALU.mult, ALU.add, [b_w], [b_w])
            self.ts('dve', a2[:, 0:n], a2[:, 0:n], 3.141592, -3.141592, ALU.min, ALU.max, [b_w], [b_w])
            self.act(dst, a2[:, 0:n], AF.Sin, [b_w], [b_out])

    def s_phase(self, l):
        P = self.P
        P.barrier()
        P.sb_reset(self.const_end)
        T = ST
        ps = self.psum
        Wa = P.sb([128, 4, D], BF16, 'Wa')
        Wb = P.sb([128, 4, D], BF16, 'Wb')
        bWa = self.wbufs(4, D)
        bWb = self.wbufs(4, D)
        self.load_weight_rows(Wa, self.I('s5_glu_a')[l], 4, D, bWa)
        self.load_weight_rows(Wb, self.I('s5_glu_b')[l], 4, D, bWb)
        rWa, rWb = self.flat(bWa), self.flat(bWb)
        are = P.sb([128, 16], F32); aim = P.sb([128, 16], F32); ldt = P.sb([128, 16], F32)
        bre = P.sb([128, 16, 16], F32); bim = P.sb([128, 16, 16], F32)
        cre = P.sb([128, 16, 16], F32); cim = P.sb([128, 16, 16], F32)
        dsk = P.sb([128, 4], F32)
        step = P.sb([128, N2], F32)
        b_par = Buf('s5par')
        for t_, nm in ((are, 's5_are'), (aim, 's5_aim'), (ldt, 's5_ldt'), (bre, 's5_bre'), (bim, 's5_bim'), (cre, 's5_cre'), (cim, 's5_cim'), (dsk, 's5_d')):
            self.dma('sp', t_, self.I(nm)[l], [], [b_par])
        self.dma('sp', step, self.I('c_step'), [], [b_par])
        adt = P.sb([128, 16], F32); th = P.sb([128, 16], F32); dtt = P.sb([128, 16], F32)
        b_adt = Buf('adt')
        self.act(dtt, ldt, AF.Exp, [b_par], [b_adt])
        self.tt('dve', adt, are, dtt, ALU.mult, [b_par, b_adt], [b_adt])
        self.tt('dve', th, aim, dtt, ALU.mult, [b_par, b_adt], [b_adt])
        wk = (P.sb([128, 4 * N2], F32, 'wk_a2'), P.sb([128, 4 * N2], F32, 'wk_k'), Buf('wk'))
        angs = P.sb([128, 16], F32); b_angs = Buf()
        mag = P.sb([128, 16], F32); b_mag = Buf()
        sn = P.sb([128, 16], F32); cs = P.sb([128, 16], F32); b_sc = Buf()

        def lam_pow(n, lr, li, nli, b_l):
            self.act(mag, adt, AF.Exp, [b_adt], [b_mag], scale=float(n))
            self.ts('dve', angs, th, float(n), None, ALU.mult, None, [b_adt], [b_angs])
            self.trig(angs, b_angs, 16, sn, cs, b_sc, wk)
            self.tt('dve', lr, mag, cs, ALU.mult, [b_mag, b_sc], [b_l])
            self.tt('dve', li, mag, sn, ALU.mult, [b_mag, b_sc], [b_l])
            self.ts('dve', nli, li, -1.0, None, ALU.mult, None, [b_l], [b_l])

        lr = P.sb([128, 16], F32); li = P.sb([128, 16], F32); nli = P.sb([128, 16], F32); b_l = Buf('lam')
        lam_pow(1, lr, li, nli, b_l)
        fre = P.sb([128, 16], F32); fim = P.sb([128, 16], F32); b_f = Buf('f')
        den = P.sb([128, 16], F32); t16 = P.sb([128, 16], F32); nr = P.sb([128, 16], F32)
        self.tt('dve', den, are, are, ALU.mult, [b_par], [b_f])
        self.tt('dve', t16, aim, aim, ALU.mult, [b_par], [b_f])
        self.tt('dve', den, den, t16, ALU.add, [b_f], [b_f])
        self.P.add('dve', lambda e: e.reciprocal(out=den, in_=den), reads=[b_f], writes=[b_f])
        self.ts('dve', nr, lr, -1.0, None, ALU.add, None, [b_l], [b_f])
        self.tt('dve', fre, nr, are, ALU.mult, [b_f, b_par], [b_f])
        self.tt('dve', t16, li, aim, ALU.mult, [b_l, b_par], [b_f])
        self.tt('dve', fre, fre, t16, ALU.add, [b_f], [b_f])
        self.tt('dve', fre, fre, den, ALU.mult, [b_f], [b_f])
        self.tt('dve', fim, li, are, ALU.mult, [b_l, b_par], [b_f])
        self.tt('dve', t16, nr, aim, ALU.mult, [b_f, b_par], [b_f])
        self.tt('dve', fim, fim, t16, ALU.subtract, [b_f], [b_f])
        self.tt('dve', fim, fim, den, ALU.mult, [b_f], [b_f])

        def bc(a):
            return a.unsqueeze(2).to_broadcast([128, 16, 16])

        def cmul(out_re, out_im, sr, si, xr, xi, reads, b_o, t_a, neg_im=False):
            self.tt('dve', out_re, xr, bc(sr), ALU.mult, reads, [b_o])
            self.tt('dve', t_a, xi, bc(si), ALU.mult, reads, [b_o])
            self.tt('dve', out_re, out_re, t_a, ALU.subtract, [b_o], [b_o])
            self.tt('dve', out_im, xi, bc(sr), ALU.mult, reads, [b_o])
            self.tt('dve', t_a, xr, bc(si), ALU.mult, reads, [b_o])
            self.tt('dve', out_im, out_im, t_a, ALU.add, [b_o], [b_o])
            if neg_im:
                self.ts('dve', out_im, out_im, -1.0, None, ALU.mult, None, [b_o], [b_o])

        bbre = P.sb([128, 16, 16], F32); bbim = P.sb([128, 16, 16], F32); b_bb = Buf('bb')
        t256 = P.sb([128, 16, 16], F32)
        cmul(bbre, bbim, fre, fim, bre, bim, [b_f, b_par], b_bb, t256)
        NatB = [P.sb([128, 16, 128], F32, 'NatBre', top=True), P.sb([128, 16, 128], F32, 'NatBim', top=True)]
        NatT = [P.sb([128, 16, 128], F32, 'NatTre', top=True), P.sb([128, 16, 128], F32, 'NatTim', top=True)]
        b_natB = [[[Buf() for _ in range(2)] for _ in range(4)] for _ in range(2)]
        b_natT = [[[Buf() for _ in range(2)] for _ in range(4)] for _ in range(2)]

        def allb(bb):
            return [x for r_ in bb for x in r_]
        for i_, t_ in enumerate(NatB):
            self.memset('pool', t_, 0.0, allb(b_natB[i_]))
        for i_, t_ in enumerate(NatT):
            self.memset('pool', t_, 0.0, allb(b_natT[i_]))

        def scatter(nat, src, b_src, b_nat):
            for m in range(4):
                for g2 in range(2):
                    rows = slice(64 * g2, 64 * g2 + 64)
                    dst = nat.rearrange("p (c m) x -> p c m x", m=4)[rows, :, m, m * 32 + g2 * 16:m * 32 + g2 * 16 + 16]
                    srcv = src.rearrange("p (c m) h -> p c m h", m=4)[rows, :, m, :]
                    self.copy('pool', dst, srcv, [b_src], [b_nat[m][g2]])

        scatter(NatB[0], bbre, b_bb, b_natB[0])
        scatter(NatB[1], bbim, b_bb, b_natB[1])
        identf = self.ident
        BfT = P.sb([128, 16 * T * 2, 128], BF16, 'BfT'); b_BfT = [Buf() for _ in range(16 * T * 2)]
        xr = P.sb([128, 16, 16], F32); xi = P.sb([128, 16, 16], F32); b_x = Buf('x')
        psetup = PsumPool(ps[0:4], ['su%d' % i for i in range(4)])
        for j in range(T):
            if j == T - 1:
                natre, natim, b_nat = NatB[0], NatB[1], b_natB
            else:
                lam_pow(T - 1 - j, lr, li, nli, b_l)
                cmul(xr, xi, lr, li, bbre, bbim, [b_l, b_bb], b_x, t256)
                scatter(NatT[0], xr, b_x, b_natT[0])
                scatter(NatT[1], xi, b_x, b_natT[1])
                natre, natim, b_nat = NatT[0], NatT[1], b_natT
            for ri, nat in ((0, natre), (1, natim)):
                for q in range(16):
                    pt, bp = psetup.get()
                    self.mm(pt[:, 0:128], [(nat[:, q, :], identf)], b_nat[ri][q % 4] + [self.b_ident], [bp])
                    self.copy('act', BfT[:, (q * T + j) * 2 + ri, :], pt[:, 0:128], [bp], [b_BfT[(q * T + j) * 2 + ri]])
        Cf = P.sb([128, 16 * T * 2, 64], BF16, 'Cf'); b_Cf = [Buf() for _ in range(T * 2 * 4)]
        self.memset('pool', Cf, 0.0, b_Cf)
        Kd = P.sb([128, 4 * T, 128], BF16, 'Kd'); b_Kd = [Buf() for _ in range(4 * T)]
        Cfv = Cf.rearrange("p (q j r) x -> p q j r x", q=16, j=T)
        for d in range(T + 1):
            if d == 0:
                self.copy('dve', xr, cre, [b_par], [b_x])
                self.ts('dve', xi, cim, -1.0, None, ALU.mult, None, [b_par], [b_x])
            else:
                lam_pow(d, lr, li, nli, b_l)
                cmul(xr, xi, lr, li, cre, cim, [b_l, b_par], b_x, t256, neg_im=True)
            if d >= 1:
                j = d - 1
                for ri, src in ((0, xr), (1, xi)):
                    for g2 in range(2):
                        rows = slice(64 * g2, 64 * g2 + 64)
                        for mm_ in range(2):
                            co = mm_ * 32 + g2 * 16
                            dstv = Cfv.rearrange("p (a b) j r x -> p a b j r x", b=2)[rows, :, mm_, j, ri, co:co + 16]
                            srcv = src.rearrange("p (a b) h -> p a b h", b=2)[rows, :, mm_, :]
                            self.copy('pool', dstv, srcv, [b_x], [b_Cf[((j * 2 + ri) * 2 + g2) * 2 + mm_]])
            if d <= T - 1:
                scatter(NatT[0], xr, b_x, b_natT[0])
                scatter(NatT[1], xi, b_x, b_natT[1])
                for c4 in range(4):
                    pt, bp = psetup.get()
                    pairs = []
                    for m in range(4):
                        pairs.append((NatB[0][:, 4 * c4 + m, :], NatT[0][:, 4 * c4 + m, :]))
                        pairs.append((NatB[1][:, 4 * c4 + m, :], NatT[1][:, 4 * c4 + m, :]))
                    self.mm(pt[:, 0:128], pairs, allb(b_natB[0]) + allb(b_natB[1]) + allb(b_natT[0]) + allb(b_natT[1]), [bp])
                    self.copy('act', Kd[:, c4 * T + d, :], pt[:, 0:128], [bp], [b_Kd[c4 * T + d]])
        cosE = P.sb([128, 16, N2], F32, 'cosE'); sinE = P.sb([128, 16, N2], F32, 'sinE'); b_E = Buf('E')
        angE = P.sb([128, 16, N2], F32, 'angE', top=True); b_angE = Buf('angE')
        thT = P.sb([128, 16], F32)
        rho = P.sb([128, 16], F32); b_rho = Buf('rho')
        self.ts('dve', thT, th, float(T), None, ALU.mult, None, [b_adt], [b_rho])
        self.act(rho, adt, AF.Exp, [b_adt], [b_rho], scale=float(T))
        for q in range(16):
            self.ts('dve', angE[:, q, :], step, thT[:, q:q + 1], None, ALU.mult, None, [b_par, b_rho], [b_angE])
        for qq in range(4):
            self.trig(angE[:, 4 * qq:4 * qq + 4, :].rearrange("p q k -> p (q k)"), b_angE, 4 * N2, sinE[:, 4 * qq:4 * qq + 4, :].rearrange("p q k -> p (q k)"),
                      cosE[:, 4 * qq:4 * qq + 4, :].rearrange("p q k -> p (q k)"), b_E, wk)
        Zf = P.sb([128, 16, 2, N2 + 1], F32, 'Zf'); bZ = [Buf('Z%d' % q) for q in range(16)]
        bZc = [Buf('Zc%d' % c4) for c4 in range(4)]
        self.memset('pool', Zf, 0.0, bZc)
        P.barrier()
        P.top_off = 0
        ubs = [P.sb([128, 4, TB], BF16, 'ub') for _ in range(2)]; b_ubs = [Buf('ub0'), Buf('ub1')]
        tp = Pool_(P, 12, [128, 4, N2], F32, 'tS')
        Xp = Pool_(P, 4, [128, 4, N2], F32, 'XS')
        Wp = Pool_(P, 4, [128, 4, N2], F32, 'WS')
        Zbp = Pool_(P, 3, [128, 4, 2, N2], BF16, 'Zb')
        ysfs = [P.sb([128, 4, TB], BF16, 'ysf') for _ in range(2)]; b_ysfs = [[Buf() for _ in range(4)] for _ in range(2)]
        ysum = Pool_(P, 2, [128, TB], F32, 'ysum')
        sgp = Pool_(P, 2, [128, TB], F32, 'sgb')
        g1p = Pool_(P, 4, [128, TB], BF16, 'g1')
        mtp = Pool_(P, 2, [128, TB], F32, 'mt')
        msp = Pool_(P, 3, [128, TB], BF16, 'ms')
        pVr = PsumPool(ps[0:2], ['Vr0', 'Vr1'])
        pVi = PsumPool(ps[2:4], ['Vi0', 'Vi1'])
        pYs = PsumPool(ps[4:6], ['Ys0', 'Ys1'])
        pA = PsumPool(ps[6:7], ['A0'])
        pB = PsumPool(ps[7:8], ['B0'])

        def v4(t):
            return t.rearrange("p (m k) -> p m k", m=4)

        def chunk_item(b, c4):
            sl = slice(b * TB, (b + 1) * TB)
            ub, b_ub = ubs[b % 2], b_ubs[b % 2]
            ysf, b_ysf = ysfs[b % 2], b_ysfs[b % 2]
            q0 = 4 * c4
            cE, sE = cosE[:, q0:q0 + 4, :], sinE[:, q0:q0 + 4, :]
            d = {}

            def s1():
                if c4 == 0:
                    self.dma('sp', ub, self.U.rearrange("(h p) t -> p h t", p=128)[:, :, sl], [self.dbuf('U', b)], [b_ub])
                d['Vr'] = pVr.get()
                d['Vi'] = pVi.get()
                for m in range(4):
                    for ri, (Vt, bVt) in enumerate((d['Vr'], d['Vi'])):
                        prs = [(BfT[:, ((q0 + m) * T + j) * 2 + ri, :], ub[:, c4, j:TB:T]) for j in range(T)]
                        self.mm(Vt[:, m * N2:(m + 1) * N2], prs, [b_BfT[((q0 + m) * T + j) * 2 + ri] for j in range(T)] + [b_ub], [bVt])

            def s2():
                (Vr, bVr), (Vi, bVi) = d['Vr'], d['Vi']
                d['t'] = [tp.get() for _ in range(4)]
                (t1, bt1), (t2, bt2), (t3, bt3), (t4, bt4) = d['t']
                self.tt('dve', t1, v4(Vr), cE, ALU.mult, [bVr, b_E], [bt1])
                self.tt('dve', t2, v4(Vi), sE, ALU.mult, [bVi, b_E], [bt2])
                self.tt('dve', t3, v4(Vi), cE, ALU.mult, [bVi, b_E], [bt3])
                self.tt('dve', t4, v4(Vr), sE, ALU.mult, [bVr, b_E], [bt4])

            def s3():
                (t1, bt1), (t2, bt2), (t3, bt3), (t4, bt4) = d['t']
                d['X'] = (Xp.get(), Xp.get())
                (Xre, bXre), (Xim, bXim) = d['X']
                self.tt('pool', Xre, t1, t2, ALU.add, [bt1, bt2], [bXre])
                self.tt('pool', Xim, t3, t4, ALU.subtract, [bt3, bt4], [bXim])
                self.copy('pool', Zf[:, q0:q0 + 4, :, 0:1], Zf[:, q0:q0 + 4, :, N2:N2 + 1], [bZc[c4]], [bZc[c4]])

            def s4():
                (Xre, bXre), (Xim, bXim) = d['X']
                d['W'] = (Wp.get(), Wp.get())
                (Wre, bWre), (Wim, bWim) = d['W']
                for m in range(4):
                    q = q0 + m
                    rb = rho[:, q:q + 1].to_broadcast([128, N2])
                    self.P.add('dve', (lambda Wre, rb, Xre, q, m: lambda e: e.tensor_tensor_scan(out=Wre[:, m, :], data0=rb, data1=Xre[:, m, :], initial=Zf[:, q, 0, 0:1], op0=ALU.mult, op1=ALU.add))(Wre, rb, Xre, q, m),
                               reads=[b_rho, bXre, bZc[c4]], writes=[bWre])
                    self.P.add('dve', (lambda Wim, rb, Xim, q, m: lambda e: e.tensor_tensor_scan(out=Wim[:, m, :], data0=rb, data1=Xim[:, m, :], initial=Zf[:, q, 1, 0:1], op0=ALU.mult, op1=ALU.add))(Wim, rb, Xim, q, m),
                               reads=[b_rho, bXim, bZc[c4]], writes=[bWim])

            def s5():
                (Wre, bWre), (Wim, bWim) = d['W']
                d['u'] = [tp.get() for _ in range(4)]
                (u1, bu1), (u2, bu2), (u3, bu3), (u4, bu4) = d['u']
                self.tt('pool', u1, Wre, cE, ALU.mult, [bWre, b_E], [bu1])
                self.tt('pool', u2, Wim, sE, ALU.mult, [bWim, b_E], [bu2])
                self.tt('pool', u3, Wim, cE, ALU.mult, [bWim, b_E], [bu3])
                self.tt('pool', u4, Wre, sE, ALU.mult, [bWre, b_E], [bu4])

            def s6():
                (u1, bu1), (u2, bu2), (u3, bu3), (u4, bu4) = d['u']
                self.tt('dve', Zf[:, q0:q0 + 4, 0, 1:N2 + 1], u1, u2, ALU.subtract, [bu1, bu2], [bZc[c4]])
                self.tt('dve', Zf[:, q0:q0 + 4, 1, 1:N2 + 1], u3, u4, ALU.add, [bu3, bu4], [bZc[c4]])

            def s7():
                d['Zb'] = Zbp.get()
                Zb, bZb = d['Zb']
                for m in range(4):
                    self.copy('act', Zb[:, m], Zf[:, q0 + m, :, 0:N2], [bZc[c4]], [bZb])

            def s8():
                Zb, bZb = d['Zb']
                d['Y'] = pYs.get()
                Yp, bY = d['Y']
                for j in range(T):
                    ops_ = []
                    for i in range(j + 1):
                        ops_.append((Yp[:, j:TB:T], Kd[:, c4 * T + (j - i), :], ub[:, c4, i:TB:T]))
                    for m in range(4):
                        for ri in range(2):
                            ops_.append((Yp[64 * (m // 2):64 * (m // 2) + 64, j:TB:T], Cfv[:, q0 + m, j, ri, :], Zb[:, m, ri, :]))

                    def fn(e, ops_=ops_):
                        ins = None
                        for ii, (o_, l_, r_) in enumerate(ops_):
                            ins = e.matmul(o_, lhsT=l_, rhs=r_, start=(ii == 0), stop=(ii == len(ops_) - 1))
                        return ins
                    self.P.add('pe', fn, reads=b_Kd[c4 * T:(c4 + 1) * T] + b_Cf + [b_ub, bZb], writes=[bY])

            def s9():
                Yp, bY = d['Y']
                ys_, bys = ysum.get()
                self.stt(ys_, ub[:, c4, :], dsk[:, c4:c4 + 1], Yp, ALU.mult, ALU.add, [b_ub, b_par, bY], [bys])
                self.act(ysf[:, c4, :], ys_, AF.Gelu, [bys], [b_ysf[c4]])

            return [s1, s2, s3, s4, s5, s6, s7, s8, s9]

        def glu_item(b, c):
            sl = slice(b * TB, (b + 1) * TB)
            ysf, b_ysf = ysfs[b % 2], b_ysfs[b % 2]
            d = {}

            def g1():
                d['A'] = pA.get()
                d['B'] = pB.get()
                (Ap, bA), (Bp, bB) = d['A'], d['B']
                self.mm(Ap, [(Wa[:, c4, c * 128:(c + 1) * 128], ysf[:, c4, :]) for c4 in range(4)], b_ysf + rWa, [bA])
                self.mm(Bp, [(Wb[:, c4, c * 128:(c + 1) * 128], ysf[:, c4, :]) for c4 in range(4)], b_ysf + rWb, [bB])
                d['g1'] = g1p.get()
                g1_, bg1 = d['g1']
                self.dma('sp', g1_, self.GT[1024 + c * 128:1024 + (c + 1) * 128, sl], [self.dbuf('GT', b, 8 + c)], [bg1])

            def g3():
                (Bp, bB) = d['B']
                d['sg'] = sgp.get()
                sgb, bsg = d['sg']
                self.act(sgb, Bp, AF.Sigmoid, [bB], [bsg])
                (Ap, bA) = d['A']
                d['mt'] = mtp.get()
                mt, bmt = d['mt']
                self.tt('dve', mt, Ap, sgb, ALU.mult, [bA, bsg], [bmt])

            def g4():
                mt, bmt = d['mt']
                g1_, bg1 = d['g1']
                ms, bms = msp.get()
                self.tt('pool', ms, mt, g1_, ALU.mult, [bmt, bg1], [bms])
                self.dma('sp', self.MS[c * 128:(c + 1) * 128, sl], ms, [bms], [self.dbuf('MS', b, c)])

            return [g1, g3, g4]

        class Item:
            def __init__(self, stages, after):
                self.stages, self.after, self.pos, self.done = stages, after, 0, False

        seq = []
        chunks = {}
        for b in range(NB):
            glu_prev = []
            if b >= 1:
                glu_prev = [Item(glu_item(b - 1, c), [chunks[(b - 1, k)] for k in range(4)]) for c in range(8)]
            for c4 in range(4):
                it = Item(chunk_item(b, c4), [])
                chunks[(b, c4)] = it
                seq.append(it)
                seq.extend(glu_prev[2 * c4:2 * c4 + 2])
        seq.extend(Item(glu_item(NB - 1, c), [chunks[(NB - 1, k)] for k in range(4)]) for c in range(8))
        active = []
        started = set()
        WIN = 6
        while active or len(started) < len(seq):
            if len(active) < WIN:
                n_unstarted = 0
                for i_, it in enumerate(seq):
                    if i_ in started:
                        continue
                    n_unstarted += 1
                    if all(a_.done for a_ in it.after):
                        started.add(i_)
                        active.append(it)
                        break
                    if n_unstarted >= 4:
                        break
            for it in list(active):
                it.stages[it.pos]()
                it.pos += 1
                if it.pos == len(it.stages):
                    it.done = True
                    active.remove(it)

    def finish(self):
        P = self.P
        if self.dump:
            name, shape, dt = self.dump
            src = self.scratch[name]
            P.barrier()
            self.dma('sp', self.dbg, src, [], [Buf()])
        P.barrier()
        P.flush()
        P.close()
        return self.nc

    def build(self):
        self.declare()
        self.consts()
        nl = self.n_layers
        ph = self.phases
        for l in range(nl):
            src, sname = (self.I('xT'), 'xT') if l == 0 else (self.X, 'X')
            if l == 0 and (ph is None or 'B1' in ph):
                self.rope_tables()
            if ph is None or 'F1' in ph:
                self.ffn_phase(l, 0, src, sname, self.X, 'X')
            if ph is None or 'B1' in ph:
                self.b1_phase(l)
            if ph is None or 'R' in ph:
                self.r_phase(l)
            if ph is None or 'S' in ph:
                self.s_phase(l)
            if ph is None or 'M' in ph:
                self.m_phase(l)
            if ph is None or 'F2' in ph:
                last = (l == nl - 1)
                self.ffn_phase(l, 1, self.X, 'X', self.out if last else self.X, 'out' if last else 'X')
        return self.finish()


def host_constants():
    c = {}
    c['c_ident'] = np.eye(128, dtype=np.float32)
    m = np.arange(128)
    c['c_tri'] = (m[None, :] >= m[:, None]).astype(np.float32)
    lg = np.log1p(-np.exp2(-5.0 - np.arange(4, dtype=np.float64)))
    rel = (m[None, :] - m[:, None]).astype(np.float64)
    dec = np.zeros((128, 4, 128), np.float32)
    for h in range(4):
        dec[:, h, :] = np.where(rel >= 0, np.exp(np.maximum(rel, 0) * lg[h]), 0.0)
    c['c_dec'] = dec
    qd = np.zeros((128, 4, 128), np.float32)
    for h in range(4):
        qd[:, h, :] = np.exp((m + 1.0) * lg[h])[None, :]
    c['c_qdec'] = qd
    c['c_kdec'] = np.stack([np.exp((127.0 - m) * lg[h]) for h in range(4)], axis=1).astype(np.float32)
    fr = np.zeros((128, 2), np.float32)
    inv_r = (10000.0 ** (-np.arange(0, 128, 2, dtype=np.float32) / 128)).astype(np.float32)
    inv_m = (10000.0 ** (-np.arange(0, 64, 2, dtype=np.float32) / 64)).astype(np.float32)
    fr[:, 0] = np.concatenate([inv_r, inv_r])
    fr[:64, 1] = np.concatenate([inv_m, inv_m])
    c['c_freq'] = fr
    sg = np.ones((128, 2), np.float32)
    sg[:64, 0] = -1.0
    sg[:32, 1] = -1.0
    c['c_sgn'] = sg
    c['c_step'] = np.broadcast_to(np.arange(1, N2 + 1, dtype=np.float32)[None, :], (128, N2)).copy()
    return c


def host_layout(inp, b):
    m = {}
    m['xT'] = np.ascontiguousarray(inp['x'][b].T)
    m['pos'] = np.ascontiguousarray(inp['positions'][b:b + 1]).astype(np.int32)
    g = np.asarray(inp['norm_gains'], np.float32)
    m['gains'] = np.ascontiguousarray(g.reshape(DEPTH, 6, 8, 128).transpose(3, 0, 1, 2).reshape(128, DEPTH * 48))
    for k in ('ffn_w_gate', 'ffn_w_up', 'ffn_w_down', 'w_in', 'ret_w_o', 's5_glu_a', 's5_glu_b', 'mla_w_uq', 'mla_w_ukv', 'mla_w_o', 'w_out'):
        m[k] = np.ascontiguousarray(inp[k], dtype=np.float32)

    def pair(a):
        return np.ascontiguousarray(a.reshape(DEPTH, 16, 2, 64).transpose(0, 2, 3, 1).reshape(DEPTH, 128, 16))
    m['s5_are'] = pair(np.asarray(inp['s5_a_re']))
    m['s5_aim'] = pair(np.asarray(inp['s5_a_im']))
    m['s5_ldt'] = pair(np.repeat(np.asarray(inp['s5_log_dt'])[:, :, None], 64, axis=2))
    m['s5_bre'] = np.ascontiguousarray(np.asarray(inp['s5_b_re']).reshape(DEPTH, 16, 2, 64, 16).transpose(0, 2, 3, 1, 4).reshape(DEPTH, 128, 16, 16))
    m['s5_bim'] = np.ascontiguousarray(np.asarray(inp['s5_b_im']).reshape(DEPTH, 16, 2, 64, 16).transpose(0, 2, 3, 1, 4).reshape(DEPTH, 128, 16, 16))
    m['s5_cre'] = np.ascontiguousarray(np.asarray(inp['s5_c_re']).reshape(DEPTH, 16, 2, 16, 64).transpose(0, 2, 4, 1, 3).reshape(DEPTH, 128, 16, 16))
    m['s5_cim'] = np.ascontiguousarray(np.asarray(inp['s5_c_im']).reshape(DEPTH, 16, 2, 16, 64).transpose(0, 2, 4, 1, 3).reshape(DEPTH, 128, 16, 16))
    m['s5_d'] = np.ascontiguousarray(np.asarray(inp['s5_d']).reshape(DEPTH, 4, 128).transpose(0, 2, 1))
    m['mla_q_norm'] = np.ascontiguousarray(np.asarray(inp['mla_q_norm']).reshape(DEPTH, 2, 128).transpose(0, 2, 1))
    m['mla_kv_norm'] = np.ascontiguousarray(np.asarray(inp['mla_kv_norm']).reshape(DEPTH, 128, 1))
    return m


_CACHE = {}


def kernel(**inputs):
    if 'nc' not in _CACHE:
        _CACHE['nc'] = Builder().build()
    nc = _CACHE['nc']
    consts = host_constants()
    in_maps = []
    shared = None
    for b in range(8):
        m = host_layout(inputs, b)
        if shared is None:
            shared = {k: v for k, v in m.items() if k not in ('xT', 'pos')}
        else:
            for k in shared:
                m[k] = shared[k]
        m.update(consts)
        in_maps.append(m)
    res = run_bass_kernel_spmd(nc, in_maps, core_ids=list(range(8)))
    out = np.stack([np.ascontiguousarray(res.results[b]['outT'].T) for b in range(8)], axis=0)
    return out.astype(np.float32)
```
